# Optimizing a Trainium2 kernel written in Bass

```python
import math
import jax, jax.numpy as jnp
from jax import lax
import numpy as np

D_MODEL = 1024
BATCH = 4
SEQ = 8192
DEPTH = 1

D_MIX = D_MODEL
N_DIFF_HEADS = 8
DIFF_HEAD_DIM = 32
DIFF_V_DIM = 2 * DIFF_HEAD_DIM
D_ATTN = N_DIFF_HEADS * DIFF_V_DIM
D_SSM = D_MIX - D_ATTN
SSM_GROUP = 16
N_SSM_GROUPS = D_SSM // SSM_GROUP
SSM_STATE = 64
D_IN_PROJ = 3 * D_ATTN + D_SSM
N_EXPERTS = 32
TOP_K = 4
D_FF = D_MODEL
SWIGLU_LIMIT = 7.0
SWIGLU_ALPHA = 1.702
PLE_DIM = 256
Q_BLOCK = 128
MOE_BLOCK = 128
LN_EPS = 1e-5
RMS_EPS = 1e-5
DEEPNORM_ALPHA = (2.0 * DEPTH) ** 0.25
DEEPNORM_BETA = (8.0 * DEPTH) ** -0.25

kernel_name = "hymba_diffattn_s5_moe_deepnorm"


def layer_norm(x, g, b):
    xf = x.astype(jnp.float32)
    mu = jnp.mean(xf, axis=-1, keepdims=True)
    var = jnp.mean(jnp.square(xf - mu), axis=-1, keepdims=True)
    return ((xf - mu) * lax.rsqrt(var + LN_EPS) * g + b).astype(x.dtype)


def rms_norm(x, g):
    xf = x.astype(jnp.float32)
    return (xf * lax.rsqrt(jnp.mean(jnp.square(xf), axis=-1, keepdims=True) + RMS_EPS) * g).astype(x.dtype)


def alibi_slopes(n_heads):
    return jnp.exp2(-(jnp.arange(n_heads, dtype=jnp.float32) + 1.0) * (8.0 / n_heads))


def diff_attention(q, k, v, lam, subln_g, lambda_init):
    Bsz, H, L, _ = v.shape
    nb = L // Q_BLOCK
    scale = DIFF_HEAD_DIM ** -0.5
    slopes = alibi_slopes(H)
    kpos = jnp.arange(L, dtype=jnp.int32)
    qb = q.reshape(Bsz, H, nb, Q_BLOCK, 2, DIFF_HEAD_DIM).transpose(2, 0, 1, 3, 4, 5)

    def block(args):
        i, qi = args
        qpos = i * Q_BLOCK + jnp.arange(Q_BLOCK, dtype=jnp.int32)
        s = jnp.einsum('bhqmd,bhkmd->bhmqk', qi, k,
                       preferred_element_type=jnp.float32) * scale
        dist = (qpos[:, None] - kpos[None, :]).astype(jnp.float32)
        bias = jnp.where(dist >= 0, -slopes[:, None, None] * dist, -jnp.inf)
        probs = jax.nn.softmax(s + bias[None, :, None], axis=-1)
        w = probs[:, :, 0] - lam * probs[:, :, 1]
        return jnp.einsum('bhqk,bhkd->bhqd', w.astype(v.dtype), v)

    o = lax.map(block, (jnp.arange(nb, dtype=jnp.int32), qb))
    o = o.transpose(1, 2, 0, 3, 4).reshape(Bsz, H, L, DIFF_V_DIM)
    o = rms_norm(o, subln_g) * (1.0 - lambda_init)
    return o.transpose(0, 2, 1, 3).reshape(Bsz, L, H * DIFF_V_DIM)


def _ssm_combine(e1, e2):
    a1, b1 = e1
    a2, b2 = e2
    return (a2 * a1, a2 * b1 + b2)


def s5_ssm(u, a_re, a_im, log_dt, b_re, b_im, c_re, c_im, d_skip):
    Bsz, L, _ = u.shape
    f32 = jnp.float32
    A = lax.complex(a_re.astype(f32), a_im.astype(f32))
    dt = jnp.exp(log_dt.astype(f32))[:, None]
    A_bar = jnp.exp(A * dt)
    Bm = lax.complex(b_re.astype(f32), b_im.astype(f32))
    B_bar = ((A_bar - 1.0) / A)[..., None] * Bm
    Cm = lax.complex(c_re.astype(f32), c_im.astype(f32))
    uf = u.astype(f32)
    ug = uf.reshape(Bsz, L, N_SSM_GROUPS, SSM_GROUP)

    def one(u_b):
        bu = jnp.einsum('lgc,gpc->lgp', u_b, B_bar)
        a = jnp.broadcast_to(A_bar, bu.shape)
        _, states = lax.associative_scan(_ssm_combine, (a, bu), axis=0)
        return jnp.einsum('lgp,gcp->lgc', states, Cm).real

    y = lax.map(one, ug).reshape(Bsz, L, D_SSM)
    y = y + d_skip.astype(f32) * uf
    return y.astype(u.dtype)


def moe(x, w_router, b_router, w_gate_up, b_gate_up, w_down, b_down):
    Bsz, L, D = x.shape
    T = Bsz * L
    xf = x.reshape(T, D)
    logits = (xf @ w_router + b_router).astype(jnp.float32)
    top_vals, top_idx = lax.top_k(logits, TOP_K)
    gates = jax.nn.softmax(top_vals, axis=-1)
    n_assign = T * TOP_K
    flat_e = top_idx.reshape(-1).astype(jnp.int32)
    flat_tok = jnp.arange(n_assign, dtype=jnp.int32) // TOP_K
    flat_gate = gates.reshape(-1)
    order = jnp.argsort(flat_e)
    sorted_e = flat_e[order]
    counts = jnp.bincount(flat_e, length=N_EXPERTS).astype(jnp.int32)
    padded = ((counts + MOE_BLOCK - 1) // MOE_BLOCK) * MOE_BLOCK
    pad_end = jnp.cumsum(padded)
    pad_start = pad_end - padded
    start = jnp.cumsum(counts) - counts
    rank = jnp.arange(n_assign, dtype=jnp.int32) - start[sorted_e]
    dest = pad_start[sorted_e] + rank
    n_pad = n_assign + N_EXPERTS * MOE_BLOCK
    n_blocks = n_pad // MOE_BLOCK
    slot_tok = jnp.zeros((n_pad,), jnp.int32).at[dest].set(flat_tok[order])
    slot_gate = jnp.zeros((n_pad,), jnp.float32).at[dest].set(flat_gate[order])
    block_e = jnp.minimum(
        jnp.searchsorted(pad_end, jnp.arange(n_blocks, dtype=jnp.int32) * MOE_BLOCK, side='right'),
        N_EXPERTS - 1).astype(jnp.int32)

    def expert_block(args):
        e, tok = args
        xb = xf[tok]
        h = xb @ w_gate_up[e] + b_gate_up[e]
        x_glu = jnp.minimum(h[:, ::2], SWIGLU_LIMIT)
        x_lin = jnp.clip(h[:, 1::2], -SWIGLU_LIMIT, SWIGLU_LIMIT)
        act = x_glu * jax.nn.sigmoid(SWIGLU_ALPHA * x_glu) * (x_lin + 1.0)
        return act @ w_down[e] + b_down[e]

    out = lax.map(expert_block, (block_e, slot_tok.reshape(n_blocks, MOE_BLOCK)))
    out = out.reshape(n_pad, D) * slot_gate[:, None].astype(x.dtype)
    y = jnp.zeros_like(xf).at[slot_tok].add(out)
    return y.reshape(Bsz, L, D)


def setup_inputs(seed: int = 0) -> dict:
    key = jax.random.key(seed)
    ks = jax.random.split(key, 40)
    f32 = jnp.float32

    def nrm(k, shape, s):
        return s * jax.random.normal(k, shape, f32)

    n_idx = jnp.arange(SSM_STATE, dtype=f32)
    return {
        "x": nrm(ks[0], (BATCH, SEQ, D_MODEL), 1.0),
        "p": nrm(ks[1], (DEPTH, BATCH, SEQ, PLE_DIM), 1.0),
        "w_in": nrm(ks[2], (DEPTH, D_MODEL, D_IN_PROJ), D_MODEL ** -0.5),
        "lambda_q1": nrm(ks[3], (DEPTH, DIFF_HEAD_DIM), 0.1),
        "lambda_k1": nrm(ks[4], (DEPTH, DIFF_HEAD_DIM), 0.1),
        "lambda_q2": nrm(ks[5], (DEPTH, DIFF_HEAD_DIM), 0.1),
        "lambda_k2": nrm(ks[6], (DEPTH, DIFF_HEAD_DIM), 0.1),
        "subln_g": 1.0 + nrm(ks[7], (DEPTH, DIFF_V_DIM), 0.02),
        "ssm_a_re": -0.5 + nrm(ks[8], (DEPTH, N_SSM_GROUPS, SSM_STATE), 0.01),
        "ssm_a_im": jnp.pi * n_idx[None, None, :] + nrm(ks[9], (DEPTH, N_SSM_GROUPS, SSM_STATE), 0.01),
        "ssm_log_dt": jax.random.uniform(ks[10], (DEPTH, N_SSM_GROUPS), f32,
                                         math.log(1e-3), math.log(1e-1)),
        "ssm_b_re": nrm(ks[11], (DEPTH, N_SSM_GROUPS, SSM_STATE, SSM_GROUP), (0.5 / SSM_GROUP) ** 0.5),
        "ssm_b_im": nrm(ks[12], (DEPTH, N_SSM_GROUPS, SSM_STATE, SSM_GROUP), (0.5 / SSM_GROUP) ** 0.5),
        "ssm_c_re": nrm(ks[13], (DEPTH, N_SSM_GROUPS, SSM_GROUP, SSM_STATE), (0.5 / SSM_STATE) ** 0.5),
        "ssm_c_im": nrm(ks[14], (DEPTH, N_SSM_GROUPS, SSM_GROUP, SSM_STATE), (0.5 / SSM_STATE) ** 0.5),
        "ssm_d": nrm(ks[15], (DEPTH, D_SSM), 1.0),
        "w_glu": nrm(ks[16], (DEPTH, D_SSM, 2 * D_SSM), D_SSM ** -0.5),
        "ssm_norm_g": 1.0 + nrm(ks[17], (DEPTH, D_SSM), 0.02),
        "w_out": nrm(ks[18], (DEPTH, D_MIX, D_MODEL), D_MIX ** -0.5 * DEEPNORM_BETA),
        "ln1_g": 1.0 + nrm(ks[19], (DEPTH, D_MODEL), 0.02),
        "ln1_b": nrm(ks[20], (DEPTH, D_MODEL), 0.01),
        "w_router": nrm(ks[21], (DEPTH, D_MODEL, N_EXPERTS), D_MODEL ** -0.5),
        "b_router": nrm(ks[22], (DEPTH, N_EXPERTS), 0.01),
        "w_gate_up": nrm(ks[23], (DEPTH, N_EXPERTS, D_MODEL, 2 * D_FF), D_MODEL ** -0.5),
        "b_gate_up": nrm(ks[24], (DEPTH, N_EXPERTS, 2 * D_FF), 0.01),
        "w_down": nrm(ks[25], (DEPTH, N_EXPERTS, D_FF, D_MODEL), D_FF ** -0.5 * DEEPNORM_BETA),
        "b_down": nrm(ks[26], (DEPTH, N_EXPERTS, D_MODEL), 0.01),
        "w_ple_gate": nrm(ks[27], (DEPTH, D_MODEL, D_MODEL), D_MODEL ** -0.5),
        "w_ple_proj": nrm(ks[28], (DEPTH, PLE_DIM, D_MODEL), PLE_DIM ** -0.5 * DEEPNORM_BETA),
        "ln2_g": 1.0 + nrm(ks[29], (DEPTH, D_MODEL), 0.02),
        "ln2_b": nrm(ks[30], (DEPTH, D_MODEL), 0.01),
    }


def reference(x, p, w_in, lambda_q1, lambda_k1, lambda_q2, lambda_k2, subln_g,
              ssm_a_re, ssm_a_im, ssm_log_dt, ssm_b_re, ssm_b_im, ssm_c_re, ssm_c_im,
              ssm_d, w_glu, ssm_norm_g, w_out, ln1_g, ln1_b,
              w_router, b_router, w_gate_up, b_gate_up, w_down, b_down,
              w_ple_gate, w_ple_proj, ln2_g, ln2_b):
    Bsz, L, _ = x.shape
    for i in range(DEPTH):
        lambda_init = 0.8 - 0.6 * math.exp(-0.3 * i)
        h = x @ w_in[i]
        q, k, v, u = jnp.split(h, [D_ATTN, 2 * D_ATTN, 3 * D_ATTN], axis=-1)
        q = q.reshape(Bsz, L, N_DIFF_HEADS, 2, DIFF_HEAD_DIM).transpose(0, 2, 1, 3, 4)
        k = k.reshape(Bsz, L, N_DIFF_HEADS, 2, DIFF_HEAD_DIM).transpose(0, 2, 1, 3, 4)
        v = v.reshape(Bsz, L, N_DIFF_HEADS, DIFF_V_DIM).transpose(0, 2, 1, 3)
        lam = (jnp.exp(jnp.sum(lambda_q1[i].astype(jnp.float32) * lambda_k1[i].astype(jnp.float32)))
               - jnp.exp(jnp.sum(lambda_q2[i].astype(jnp.float32) * lambda_k2[i].astype(jnp.float32)))
               + lambda_init)
        attn_out = diff_attention(q, k, v, lam, subln_g[i], lambda_init)
        y = s5_ssm(u, ssm_a_re[i], ssm_a_im[i], ssm_log_dt[i], ssm_b_re[i], ssm_b_im[i],
                   ssm_c_re[i], ssm_c_im[i], ssm_d[i])
        y = jax.nn.gelu(y)
        g = y @ w_glu[i]
        y = g[..., :D_SSM] * jax.nn.sigmoid(g[..., D_SSM:])
        ssm_out = rms_norm(y, ssm_norm_g[i])
        mix = jnp.concatenate([attn_out, ssm_out], axis=-1) @ w_out[i]
        x = layer_norm(DEEPNORM_ALPHA * x + mix, ln1_g[i], ln1_b[i])
        r = DEEPNORM_ALPHA * x + moe(x, w_router[i], b_router[i], w_gate_up[i], b_gate_up[i],
                                     w_down[i], b_down[i])
        gate = jax.nn.sigmoid(r @ w_ple_gate[i])
        x = layer_norm(r + gate * (p[i] @ w_ple_proj[i]), ln2_g[i], ln2_b[i])
    return x
```

```python
import math
import os
from contextlib import ExitStack

import numpy as np
import concourse.bass as bass
import concourse.mybir as mybir
from concourse.bass_utils import run_bass_kernel_spmd

F32 = mybir.dt.float32
BF16 = mybir.dt.bfloat16
AF = mybir.ActivationFunctionType
ALU = mybir.AluOpType
AX = mybir.AxisListType

ENGINES = ("tensor", "vector", "scalar", "gpsimd", "sync")
SAME_ENGINE_SYNC = True
KDEBUG = os.environ.get("KDEBUG", "")

L = 8192
LO = 4096
DM = 1024
ALPHA = 2.0 ** 0.25
LAMBDA_INIT = 0.2
ATT_TH = 64.0
NEG = -30000.0
NE = int(os.environ.get('KNE', '32'))
NCH = int(os.environ.get('KNC', '4'))


KEYMAP = {
    "pb0": ["B0"], "pb1": ["B1"], "pbC": ["B2"], "pbu_re": ["B2"], "pbu_im": ["B3"], "py0": ["B4"], "py1": ["B5"], "pms": ["B6"],
    "sc0": ["B0", "B1"], "sc1": ["B2", "B3"], "Ooff": ["B4", "B5"], "Odg": ["B6", "B7"],
    "pm": ["B0", "B1"], "ptr": ["B2", "B3"], "pl": ["B4"],
    "pa": ["B0", "B1"], "pg": ["B6"], "pgl0": ["B0"], "pgl0l": ["B1"], "pgl1": ["B2"], "pgl1l": ["B3"],
    "pdn0": ["B4"], "pdn1": ["B5"], "pdn2": ["B6"], "pdn3": ["B7"],
    "ptr5": ["B4", "B5"], "ptp": ["B6"], "pgt": ["B0", "B1"], "ppp": ["B2", "B3"],
}


def _mapkeys(ks):
    out = []
    for k in ks:
        out.extend(KEYMAP.get(k, [k]))
    return out


class Prog:
    def __init__(self, nc, n_dma_sems=24):
        self.nc = nc
        self.ops = []
        self.last_w = {}
        self.readers = {}
        self.n_dma_sems = n_dma_sems
        self.last_of = {e: None for e in ENGINES}
        self.recent_dma = {e: [] for e in ENGINES}

    def op(self, eng, fn, r=(), w=(), dma=False, extra=()):
        idx = len(self.ops)
        deps = set(extra)
        r = _mapkeys(r); w = _mapkeys(w)
        for k in r:
            if k in self.last_w:
                deps.add(self.last_w[k])
        for k in w:
            if k in self.last_w:
                deps.add(self.last_w[k])
            for x in self.readers.get(k, ()):
                deps.add(x)
        for k in w:
            self.last_w[k] = idx
            self.readers[k] = []
        for k in r:
            if k not in w:
                self.readers.setdefault(k, []).append(idx)
        deps.discard(idx)
        self.ops.append(dict(eng=eng, fn=fn, deps=deps, dma=dma, idx=idx))
        self.last_of[eng] = idx
        if dma:
            self.recent_dma[eng].append(idx)
            self.recent_dma[eng] = self.recent_dma[eng][-self.n_dma_sems:]
        return idx

    def pe(self, fn, r=(), w=()): return self.op("tensor", fn, r, w)
    def dve(self, fn, r=(), w=()): return self.op("vector", fn, r, w)
    def act(self, fn, r=(), w=()): return self.op("scalar", fn, r, w)
    def pool(self, fn, r=(), w=()): return self.op("gpsimd", fn, r, w)
    def dma(self, fn, r=(), w=(), q="sync"): return self.op(q, fn, r, w, dma=True)

    def barrier(self):
        deps = set()
        for e in ENGINES:
            if self.last_of[e] is not None:
                deps.add(self.last_of[e])
            deps.update(self.recent_dma[e])
        for e in ENGINES:
            self.op(e, None, extra=tuple(deps))
        self.last_w = {}
        self.readers = {}

    def emit(self, stack):
        nc = self.nc
        ops = self.ops
        needed = set()
        for o in ops:
            for d in o["deps"]:
                needed.add(d)
        for e in ENGINES:
            if self.last_of[e] is not None:
                needed.add(self.last_of[e])
        csem = {e: stack.enter_context(nc.semaphore(f"c_{e}")) for e in ENGINES}
        ccount = {e: 0 for e in ENGINES}
        dsems, dcount = {}, {}
        dnext = {e: 0 for e in ENGINES}
        for e in ("sync", "gpsimd", "scalar"):
            dsems[e] = [stack.enter_context(nc.semaphore(f"d_{e}_{i}")) for i in range(self.n_dma_sems)]
            dcount[e] = [0] * self.n_dma_sems
        for o in ops:
            e = o["eng"]
            if o["fn"] is None:
                o["sig"] = None
            elif o["dma"]:
                j = dnext[e]
                dnext[e] = (j + 1) % self.n_dma_sems
                o["dma_prev"] = (dsems[e][j], dcount[e][j])
                dcount[e][j] += 16
                o["sig"] = (dsems[e][j], dcount[e][j])
            elif o["idx"] in needed:
                ccount[e] += 1
                o["sig"] = (csem[e], ccount[e])
            else:
                o["sig"] = None
        def resolve(d, seen):
            od = ops[d]
            if od["fn"] is not None:
                return [d]
            out = []
            for dd in od["deps"]:
                if dd not in seen:
                    seen.add(dd)
                    out.extend(resolve(dd, seen))
            return out
        waited = {e: {} for e in ENGINES}
        per_eng = {e: [o for o in ops if o["eng"] == e] for e in ENGINES}
        block = stack.enter_context(nc.Block())

        def make(e):
            def body(eng):
                wd = waited[e]

                def wait(sem, val):
                    if val <= 0:
                        return
                    key = id(sem)
                    if wd.get(key, 0) >= val:
                        return
                    eng.wait_ge(sem, val)
                    wd[key] = val
                for o in per_eng[e]:
                    alld = []
                    seen = set()
                    for d in sorted(o["deps"]):
                        alld.extend(resolve(d, seen))
                    for d in sorted(set(alld)):
                        od = ops[d]
                        if od["sig"] is None:
                            continue
                        if (not od["dma"]) and od["eng"] == e and not SAME_ENGINE_SYNC:
                            continue
                        wait(*od["sig"])
                    if o["fn"] is None:
                        continue
                    if o["dma"]:
                        wait(*o["dma_prev"])
                    ins = o["fn"](eng)
                    if o["sig"] is not None:
                        ins.then_inc(o["sig"][0], 16 if o["dma"] else 1)
                if e == "sync":
                    for q in dsems:
                        for j in range(self.n_dma_sems):
                            wait(dsems[q][j], dcount[q][j])
                    for e2 in ENGINES:
                        if e2 != "sync":
                            wait(csem[e2], ccount[e2])
            return body

        block.tensor(make("tensor"))
        block.vector(make("vector"))
        block.scalar(make("scalar"))
        block.gpsimd(make("gpsimd"))
        block.sync(make("sync"))


def I(name, *a, **k):
    return lambda e: getattr(e, name)(*a, **k)


class Arena:
    def __init__(self, ap_f32, nbytes):
        self.ap = ap_f32
        self.n = nbytes
        self.off = 0

    def mark(self): return self.off
    def reset(self, m): self.off = m

    def alloc(self, shape, dt=F32, parts=128):
        esz = 4 if dt == F32 else 2
        n = int(np.prod(shape[1:])) * esz
        n4 = (n + 3) // 4
        assert self.off + n4 * 4 <= self.n, f"arena overflow {self.off + n4 * 4} > {self.n}"
        a = self.ap[0:shape[0], self.off // 4:self.off // 4 + n4]
        self.off += n4 * 4
        if dt != F32:
            a = a.bitcast(dt)
        if len(shape) == 3:
            a = a.rearrange("p (a b) -> p a b", a=shape[1])
        elif len(shape) == 4:
            a = a.rearrange("p (a b c) -> p a b c", a=shape[1], b=shape[2])
        return a


def _qkey_lo(q0, slope):
    kmin = q0 - ATT_TH / slope
    return max(0, int(math.floor(kmin / 128.0)))


def build_program(dbg=""):
    nc = bass.Bass("TRN2", target_bir_lowering=False)
    D = {}

    def din(name, shape, dt=F32):
        D[name] = nc.dram_tensor(name, list(shape), dt, kind="ExternalInput").ap()
        return D[name]

    def dscr(name, shape, dt):
        kind = "ExternalOutput" if name in dbg.split(",") else "Internal"
        D[name] = nc.dram_tensor(name, list(shape), dt, kind=kind).ap()
        return D[name]

    xc = din("xc", [L, DM]); pc = din("pc", [LO, 256]); pref = din("pref", [128, 1])
    w_in = din("w_in", [1024, 2048])
    lq1 = din("lambda_q1", [1, 32]); lk1 = din("lambda_k1", [1, 32]); lq2 = din("lambda_q2", [1, 32]); lk2 = din("lambda_k2", [1, 32])
    subln_g = din("subln_g", [64, 1])
    a_re = din("ssm_a_re", [32, 64]); a_im = din("ssm_a_im", [32, 64]); log_dt = din("ssm_log_dt", [1, 32])
    b_re = din("ssm_b_re", [32, 64, 16]); b_im = din("ssm_b_im", [32, 64, 16])
    c_re = din("ssm_c_re", [32, 16, 64]); c_im = din("ssm_c_im", [32, 16, 64])
    ssm_d = din("ssm_d", [512]); w_glu = din("w_glu", [512, 1024]); ssm_norm_g = din("ssm_norm_g", [512])
    w_out = din("w_out", [1024, 1024]); ln1_g = din("ln1_g", [1, 1024]); ln1_b = din("ln1_b", [1, 1024])
    w_router = din("w_router", [1024, 32]); b_router = din("b_router", [1, 32])
    phases = os.environ.get("KPHASES", "12345")
    if "4" in phases:
        w_gu = din("w_gate_up", [NE, 1024, 2048]); b_gu = din("b_gate_up", [32, 2048])
        w_dn = din("w_down", [NE, 1024, 1024]); b_dn = din("b_down", [32, 1024])
    w_pg = din("w_ple_gate", [1024, 1024]); w_pp = din("w_ple_proj", [256, 1024])
    ln2_g = din("ln2_g", [1, 1024]); ln2_b = din("ln2_b", [1, 1024])
    out = nc.dram_tensor("out", [LO, DM], F32, kind="ExternalOutput").ap()

    KT = dscr("KT", [4, 128, L], BF16); QT = dscr("QT", [4, 128, LO], BF16)
    VV = dscr("VV", [64, 128, 520], BF16)
    MIXT = dscr("MIXT", [8, 128, LO], BF16)
    X1 = dscr("X1", [LO, DM], F32); X1T = dscr("X1T", [8, 128, LO], BF16)
    RR = dscr("RR", [LO, DM], F32)

    with ExitStack() as st:
        st.enter_context(nc.allow_non_contiguous_dma(reason="layout"))
        ARENA_B = 200 * 1024
        arena_t = st.enter_context(nc.sbuf_tensor("arena", [128, ARENA_B // 4], F32))
        psum_t = st.enter_context(nc.psum_tensor("psum", [128, 4096], F32))
        AR = Arena(arena_t, ARENA_B)
        P = Prog(nc)

        def bank(i, n=1):
            return psum_t[:, 512 * i:512 * (i + n)]

        ident = AR.alloc([128, 128]); ones = AR.alloc([128, 128])
        P.pool(I("memset", ident, 1.0), w=["ident"])
        P.pool(I("affine_select", out=ident, in_=ident, pattern=[[-1, 128]], compare_op=ALU.is_equal,
                                         fill=0.0, base=0, channel_multiplier=1), r=["ident"], w=["ident"])
        P.pool(I("memset", ones, 1.0), w=["ones"])
        gmark = AR.mark()

        if "1" in phases:
            win = AR.alloc([128, 8, 2048], BF16)
            for kt in range(8):
                P.dma(I("dma_start", out=win[:, kt, :], in_=w_in[kt * 128:(kt + 1) * 128, :]), w=["win"], q="gpsimd")
            wglu = AR.alloc([128, 4, 1024], BF16)
            for kt in range(4):
                P.dma(I("dma_start", out=wglu[:, kt, :], in_=w_glu[kt * 128:(kt + 1) * 128, :]), w=["wglu"], q="gpsimd")
            sm = lambda: AR.alloc([128, 16])
            are, aim, dtt, rho, th, cth, sth, Are, Aim, t0, t1, t2, Fre, Fim, d2 = [sm() for _ in range(15)]
            P.dma(I("dma_start", out=are, in_=a_re.rearrange("(gp g2) p -> (g2 p) gp", g2=2)), w=["are"])
            P.dma(I("dma_start", out=aim, in_=a_im.rearrange("(gp g2) p -> (g2 p) gp", g2=2)), w=["aim"])
            ldv = log_dt.rearrange("o (gp g2) -> o g2 gp", g2=2)
            for g2 in range(2):
                P.dma(I("dma_start", out=dtt[64 * g2:64 * g2 + 64, :], in_=ldv[:, g2, :].to_broadcast([64, 16])), w=["dtt"])
            P.act(I("activation", out=dtt, in_=dtt, func=AF.Exp), r=["dtt"], w=["dtt"])
            P.dve(I("tensor_tensor", out=t0, in0=are, in1=dtt, op=ALU.mult), r=["are", "dtt"], w=["t0"])
            P.act(I("activation", out=rho, in_=t0, func=AF.Exp), r=["t0"], w=["rho"])
            P.dve(I("tensor_tensor", out=th, in0=aim, in1=dtt, op=ALU.mult), r=["aim", "dtt"], w=["th"])
            P.dve(I("memset", t1, 0.0), w=["t1"])
            for kk in range(6):
                thr = (2 * kk + 1) * math.pi
                P.dve(I("tensor_scalar", out=t2, in0=th, scalar1=-thr, scalar2=1e6, op0=ALU.add, op1=ALU.mult), r=["th"], w=["t2"])
                P.dve(I("tensor_scalar", out=t2, in0=t2, scalar1=0.0, scalar2=1.0, op0=ALU.max, op1=ALU.min), r=["t2"], w=["t2"])
                P.dve(I("tensor_tensor", out=t1, in0=t1, in1=t2, op=ALU.add), r=["t1", "t2"], w=["t1"])
            P.dve(I("scalar_tensor_tensor", out=th, in0=t1, scalar=-2.0 * math.pi, in1=th, op0=ALU.mult, op1=ALU.add), r=["t1", "th"], w=["th"])
            P.act(I("activation", out=sth, in_=th, func=AF.Sin), r=["th"], w=["sth"])
            P.dve(I("tensor_scalar", out=t2, in0=th, scalar1=-1.0, scalar2=None, op0=ALU.mult), r=["th"], w=["t2"])
            P.dve(I("tensor_tensor", out=t2, in0=t2, in1=th, op=ALU.max), r=["t2", "th"], w=["t2"])
            P.dve(I("tensor_scalar", out=t2, in0=t2, scalar1=-1.0, scalar2=math.pi / 2, op0=ALU.mult, op1=ALU.add), r=["t2"], w=["t2"])
            P.act(I("activation", out=cth, in_=t2, func=AF.Sin), r=["t2"], w=["cth"])
            P.dve(I("tensor_tensor", out=Are, in0=rho, in1=cth, op=ALU.mult), r=["rho", "cth"], w=["Are"])
            P.dve(I("tensor_tensor", out=Aim, in0=rho, in1=sth, op=ALU.mult), r=["rho", "sth"], w=["Aim"])
            P.dve(I("tensor_scalar", out=t0, in0=Are, scalar1=-1.0, scalar2=None, op0=ALU.add), r=["Are"], w=["t0"])
            P.dve(I("tensor_tensor", out=d2, in0=are, in1=are, op=ALU.mult), r=["are"], w=["d2"])
            P.dve(I("tensor_tensor", out=t1, in0=aim, in1=aim, op=ALU.mult), r=["aim"], w=["t1"])
            P.dve(I("tensor_tensor", out=d2, in0=d2, in1=t1, op=ALU.add), r=["d2", "t1"], w=["d2"])
            P.dve(I("reciprocal", out=d2, in_=d2), r=["d2"], w=["d2"])
            P.dve(I("tensor_tensor", out=t1, in0=t0, in1=are, op=ALU.mult), r=["t0", "are"], w=["t1"])
            P.dve(I("tensor_tensor", out=t2, in0=Aim, in1=aim, op=ALU.mult), r=["Aim", "aim"], w=["t2"])
            P.dve(I("tensor_tensor", out=t1, in0=t1, in1=t2, op=ALU.add), r=["t1", "t2"], w=["t1"])
            P.dve(I("tensor_tensor", out=Fre, in0=t1, in1=d2, op=ALU.mult), r=["t1", "d2"], w=["Fre"])
            P.dve(I("tensor_tensor", out=t1, in0=Aim, in1=are, op=ALU.mult), r=["Aim", "are"], w=["t1"])
            P.dve(I("tensor_tensor", out=t2, in0=t0, in1=aim, op=ALU.mult), r=["t0", "aim"], w=["t2"])
            P.dve(I("tensor_tensor", out=t1, in0=t1, in1=t2, op=ALU.subtract), r=["t1", "t2"], w=["t1"])
            P.dve(I("tensor_tensor", out=Fim, in0=t1, in1=d2, op=ALU.mult), r=["t1", "d2"], w=["Fim"])
            Bre = AR.alloc([128, 16, 16]); Bim = AR.alloc([128, 16, 16]); Bbr = AR.alloc([128, 16, 16]); Bbi = AR.alloc([128, 16, 16]); Bt = AR.alloc([128, 16, 16])
            P.dma(I("dma_start", out=Bre, in_=b_re.rearrange("(gp g2) p c -> (g2 p) gp c", g2=2)), w=["Bre"])
            P.dma(I("dma_start", out=Bim, in_=b_im.rearrange("(gp g2) p c -> (g2 p) gp c", g2=2)), w=["Bim"])
            bc = lambda a: a.unsqueeze(2).to_broadcast([128, 16, 16])
            P.dve(I("tensor_tensor", out=Bbr, in0=Bre, in1=bc(Fre), op=ALU.mult), r=["Bre", "Fre"], w=["Bbr"])
            P.dve(I("tensor_tensor", out=Bt, in0=Bim, in1=bc(Fim), op=ALU.mult), r=["Bim", "Fim"], w=["Bt"])
            P.dve(I("tensor_tensor", out=Bbr, in0=Bbr, in1=Bt, op=ALU.subtract), r=["Bbr", "Bt"], w=["Bbr"])
            P.dve(I("tensor_tensor", out=Bbi, in0=Bim, in1=bc(Fre), op=ALU.mult), r=["Bim", "Fre"], w=["Bbi"])
            P.dve(I("tensor_tensor", out=Bt, in0=Bre, in1=bc(Fim), op=ALU.mult), r=["Bre", "Fim"], w=["Bt"])
            P.dve(I("tensor_tensor", out=Bbi, in0=Bbi, in1=Bt, op=ALU.add), r=["Bbi", "Bt"], w=["Bbi"])
            Bp = [AR.alloc([128, 16, 128], BF16), AR.alloc([128, 16, 128], BF16)]
            m1 = AR.mark()
            Lx = AR.alloc([128, 16, 128])
            for ri, Bb in enumerate((Bbr, Bbi)):
                P.dve(I("memset", Lx, 0.0), w=["Lx"])
                Lv = Lx.rearrange("q gp (l g c) -> q gp l g c", l=4, g=2)
                for g2 in range(2):
                    P.dve(I("tensor_copy",
                        out=Lv[64 * g2:64 * g2 + 64, :, :, g2, :],
                        in_=Bb[64 * g2:64 * g2 + 64].unsqueeze(2).to_broadcast([64, 16, 4, 16])), r=["Bbr", "Bbi"], w=["Lx"])
                for g4 in range(4):
                    pb = bank(g4 % 2).rearrange("p (a b) -> p a b", a=4)
                    for j in range(4):
                        gp = g4 * 4 + j
                        P.pe(I("transpose", out=pb[:, j, :], in_=Lx[:, gp, :], identity=ident), r=["Lx", "ident"], w=["pb%d" % (g4 % 2)])
                    P.dve(I("tensor_copy", out=Bp[ri][:, g4 * 4:g4 * 4 + 4, :], in_=pb), r=["pb%d" % (g4 % 2)], w=["Bp"])
            AR.reset(m1)
            Cr = AR.alloc([128, 16, 32]); Cni = AR.alloc([128, 16, 32])
            m1 = AR.mark()
            Cx = AR.alloc([128, 16, 128])
            for ri, csrc in enumerate((c_re, c_im)):
                P.dve(I("memset", Cx[0:32], 0.0), w=["Cx"])
                cv = csrc.rearrange("(gp g2) c p -> g2 c gp p", g2=2)
                for g2 in range(2):
                    P.dma(I("dma_start", out=Cx[16 * g2:16 * g2 + 16, :, 64 * g2:64 * g2 + 64], in_=cv[g2]), r=["Cx"], w=["Cx"])
                pb = bank(2).rearrange("p (a b) -> p a b", a=16)
                for gp in range(16):
                    P.pe(I("transpose", out=pb[:, gp, :], in_=Cx[0:32, gp, :], identity=ident[0:32, 0:32]), r=["Cx", "ident"], w=["pbC"])
                if ri == 0:
                    P.dve(I("tensor_copy", out=Cr, in_=pb), r=["pbC"], w=["Cr"])
                else:
                    P.dve(I("tensor_scalar", out=Cni, in0=pb, scalar1=-1.0, scalar2=None, op0=ALU.mult), r=["pbC"], w=["Cni"])
            AR.reset(m1)
            cn = AR.alloc([128, 16, 128]); sn = AR.alloc([128, 16, 128]); tA = AR.alloc([128, 16, 64]); tB = AR.alloc([128, 16, 64])
            P.dve(I("tensor_copy", out=cn[:, :, 0:1], in_=cth.unsqueeze(2)), r=["cth"], w=["cn"])
            P.dve(I("tensor_copy", out=sn[:, :, 0:1], in_=sth.unsqueeze(2)), r=["sth"], w=["sn"])
            m = 1
            while m < 128:
                cm = cn[:, :, m - 1:m].to_broadcast([128, 16, m]); smm = sn[:, :, m - 1:m].to_broadcast([128, 16, m])
                ta = tA[:, :, 0:m]; tb = tB[:, :, 0:m]
                P.dve(I("tensor_tensor", out=ta, in0=cn[:, :, 0:m], in1=cm, op=ALU.mult), r=["cn"], w=["tA"])
                P.dve(I("tensor_tensor", out=tb, in0=sn[:, :, 0:m], in1=smm, op=ALU.mult), r=["sn"], w=["tB"])
                P.dve(I("tensor_tensor", out=ta, in0=ta, in1=tb, op=ALU.subtract), r=["tA", "tB"], w=["tA"])
                P.dve(I("tensor_tensor", out=tb, in0=cn[:, :, 0:m], in1=smm, op=ALU.mult), r=["cn", "sn"], w=["tB"])
                P.dve(I("tensor_copy", out=cn[:, :, m:2 * m], in_=ta), r=["tA"], w=["cn"])
                P.dve(I("tensor_tensor", out=ta, in0=sn[:, :, 0:m], in1=cm, op=ALU.mult), r=["sn", "cn"], w=["tA"])
                P.dve(I("tensor_tensor", out=sn[:, :, m:2 * m], in0=ta, in1=tb, op=ALU.add), r=["tA", "tB"], w=["sn"])
                m *= 2
            dcol = AR.alloc([128, 4]); gncol = AR.alloc([128, 4])
            P.dma(I("dma_start", out=dcol, in_=ssm_d.rearrange("(ut r) -> r ut", r=128)), w=["dcol"])
            P.dma(I("dma_start", out=gncol, in_=ssm_norm_g.rearrange("(ut r) -> r ut", r=128)), w=["gncol"])
            Sre = AR.alloc([128, 16]); Sim = AR.alloc([128, 16])
            P.dve(I("memset", Sre, 0.0), w=["Sre"]); P.dve(I("memset", Sim, 0.0), w=["Sim"])
            xin = [AR.alloc([128, 1024]) for _ in range(2)]
            xT = [AR.alloc([128, 8, 512], BF16) for _ in range(2)]
            kst = AR.alloc([128, 4, 512], BF16); qst = AR.alloc([128, 4, 512], BF16)
            vst = [AR.alloc([128, 8, 65], BF16) for _ in range(2)]
            for b_ in range(2):
                P.dve(I("memset", vst[b_], 1.0), w=["vst%d" % b_])
            uT = AR.alloc([128, 4, 512], BF16)
            vre = AR.alloc([128, 4, 128]); vim = AR.alloc([128, 4, 128]); e1 = AR.alloc([128, 4, 128]); e2 = AR.alloc([128, 4, 128])
            wre = AR.alloc([128, 4, 128]); wim = AR.alloc([128, 4, 128]); sre = AR.alloc([128, 4, 128]); sim = AR.alloc([128, 4, 128])
            yd = AR.alloc([128, 512]); g1 = AR.alloc([128, 512]); g2t = AR.alloc([128, 512])
            glT = AR.alloc([128, 4, 512], BF16)
            y2 = AR.alloc([128, 4, 512]); sq = AR.alloc([128, 512]); sig = AR.alloc([128, 512]); rstd = AR.alloc([128, 512])
            soT = AR.alloc([128, 4, 512], BF16)
            pbu_re = bank(2).rearrange("p (a b) -> p a b", a=4); pbu_im = bank(3).rearrange("p (a b) -> p a b", a=4)
            for s in range(16):
                own = s >= 8
                xt = xT[s % 2]; xk = "xT%d" % (s % 2)
                for ti in range(4):
                    xb = xin[ti % 2]; xbk = "xin%d" % (ti % 2)
                    r0 = s * 512 + ti * 128
                    P.dma(I("dma_start", out=xb, in_=xc[r0:r0 + 128, :]), w=[xbk])
                    for half in range(2):
                        pb = bank(half).rearrange("p (a b) -> p a b", a=4)
                        for j in range(4):
                            kt = half * 4 + j
                            P.pe(I("transpose", out=pb[:, j, :], in_=xb[:, kt * 128:(kt + 1) * 128], identity=ident), r=[xbk, "ident"], w=["pb%d" % half])
                        if half == 0:
                            P.dve(I("tensor_copy", out=xt[:, 0:4, ti * 128:(ti + 1) * 128], in_=pb), r=["pb0"], w=[xk])
                        else:
                            P.act(I("activation", out=xt[:, 4:8, ti * 128:(ti + 1) * 128], in_=pb, func=AF.Copy), r=["pb1"], w=[xk])
                def proj_fm(col0, dst, dk, n_m=4):
                    for mt in range(n_m):
                        pb = bank(mt % 2); pk = "pb%d" % (mt % 2)
                        for kt in range(8):
                            P.pe(I("matmul", pb, lhsT=win[:, kt, col0 + mt * 128:col0 + (mt + 1) * 128], rhs=xt[:, kt, :], start=(kt == 0), stop=(kt == 7)), r=["win", xk], w=[pk])
                        if mt % 2 == 0:
                            P.dve(I("tensor_copy", out=dst[:, mt, :], in_=pb), r=[pk], w=[dk])
                        else:
                            P.act(I("activation", out=dst[:, mt, :], in_=pb, func=AF.Copy), r=[pk], w=[dk])
                proj_fm(512, kst, "kst")
                P.dma(I("dma_start", out=KT[:, :, s * 512:(s + 1) * 512].rearrange("h p t -> p h t"), in_=kst), r=["kst"], w=["KT"])
                if own:
                    proj_fm(0, qst, "qst")
                    P.dma(I("dma_start", out=QT[:, :, (s - 8) * 512:(s - 7) * 512].rearrange("h p t -> p h t"), in_=qst), r=["qst"], w=["QT"])
                for ti in range(4):
                    pb = bank(ti % 2); pk = "pb%d" % (ti % 2)
                    vb = vst[ti % 2]; vk = "vst%d" % (ti % 2)
                    for kt in range(8):
                        P.pe(I("matmul", pb, lhsT=xt[:, kt, ti * 128:(ti + 1) * 128], rhs=win[:, kt, 1024:1536], start=(kt == 0), stop=(kt == 7)), r=["win", xk], w=[pk])
                    P.dve(I("tensor_copy", out=vb[:, :, 0:64], in_=pb.rearrange("p (h d) -> p h d", h=8)), r=[pk], w=[vk])
                    P.dma(I("dma_start", out=VV[s * 4 + ti], in_=vb.rearrange("p h d -> p (h d)")), r=[vk], w=["VV"])
                proj_fm(1536, uT, "uT")
                for ut in range(4):
                    py = bank(4 + ut % 2); pyk = "py%d" % (ut % 2)
                    for un in range(4):
                        tsl = slice(un * 128, (un + 1) * 128)
                        for gl in range(4):
                            gp = ut * 4 + gl
                            P.pe(I("matmul", pbu_re[:, gl, :], lhsT=Bp[0][32 * gl:32 * gl + 32, gp, :], rhs=uT[32 * gl:32 * gl + 32, ut, tsl], start=True, stop=True, tile_position=(32 * gl, 0)), r=["Bp", "uT"], w=["pbu_re"])
                            P.pe(I("matmul", pbu_im[:, gl, :], lhsT=Bp[1][32 * gl:32 * gl + 32, gp, :], rhs=uT[32 * gl:32 * gl + 32, ut, tsl], start=True, stop=True, tile_position=(32 * gl, 0)), r=["Bp", "uT"], w=["pbu_im"])
                        g4 = slice(ut * 4, ut * 4 + 4)
                        cnv = cn[:, g4, :]; snv = sn[:, g4, :]
                        P.dve(I("tensor_tensor", out=vre, in0=pbu_re, in1=cnv, op=ALU.mult), r=["pbu_re", "cn"], w=["vre"])
                        P.dve(I("tensor_tensor", out=e1, in0=pbu_im, in1=snv, op=ALU.mult), r=["pbu_im", "sn"], w=["e1"])
                        P.dve(I("tensor_tensor", out=vim, in0=pbu_im, in1=cnv, op=ALU.mult), r=["pbu_im", "cn"], w=["vim"])
                        P.dve(I("tensor_tensor", out=e2, in0=pbu_re, in1=snv, op=ALU.mult), r=["pbu_re", "sn"], w=["e2"])
                        P.pool(I("tensor_tensor", out=vre, in0=vre, in1=e1, op=ALU.add), r=["vre", "e1"], w=["vre"])
                        P.pool(I("tensor_tensor", out=vim, in0=vim, in1=e2, op=ALU.subtract), r=["vim", "e2"], w=["vim"])
                        for gl in range(4):
                            gp = ut * 4 + gl
                            P.dve(I("tensor_tensor_scan", out=wre[:, gl, :], data0=rho[:, gp:gp + 1].to_broadcast([128, 128]), data1=vre[:, gl, :], initial=Sre[:, gp:gp + 1], op0=ALU.mult, op1=ALU.add), r=["vre", "rho", "Sre"], w=["wre"])
                            P.dve(I("tensor_tensor_scan", out=wim[:, gl, :], data0=rho[:, gp:gp + 1].to_broadcast([128, 128]), data1=vim[:, gl, :], initial=Sim[:, gp:gp + 1], op0=ALU.mult, op1=ALU.add), r=["vim", "rho", "Sim"], w=["wim"])
                        if own:
                            cs, ws = slice(0, 128), slice(0, 4)
                        else:
                            cs, ws = slice(127, 128), slice(0, 4)
                        P.pool(I("tensor_tensor", out=sre[:, :, cs], in0=wre[:, :, cs], in1=cnv[:, :, cs], op=ALU.mult), r=["wre", "cn"], w=["sre"])
                        P.pool(I("tensor_tensor", out=e1[:, :, cs], in0=wim[:, :, cs], in1=snv[:, :, cs], op=ALU.mult), r=["wim", "sn"], w=["e1"])
                        P.pool(I("tensor_tensor", out=sre[:, :, cs], in0=sre[:, :, cs], in1=e1[:, :, cs], op=ALU.subtract), r=["sre", "e1"], w=["sre"])
                        P.pool(I("tensor_tensor", out=sim[:, :, cs], in0=wim[:, :, cs], in1=cnv[:, :, cs], op=ALU.mult), r=["wim", "cn"], w=["sim"])
                        P.pool(I("tensor_tensor", out=e2[:, :, cs], in0=wre[:, :, cs], in1=snv[:, :, cs], op=ALU.mult), r=["wre", "sn"], w=["e2"])
                        P.pool(I("tensor_tensor", out=sim[:, :, cs], in0=sim[:, :, cs], in1=e2[:, :, cs], op=ALU.add), r=["sim", "e2"], w=["sim"])
                        P.dve(I("tensor_copy", out=Sre[:, g4], in_=sre[:, :, 127]), r=["sre"], w=["Sre"])
                        P.dve(I("tensor_copy", out=Sim[:, g4], in_=sim[:, :, 127]), r=["sim"], w=["Sim"])
                        if own:
                            for gl in range(4):
                                gp = ut * 4 + gl
                                P.pe(I("matmul", py[32 * gl:32 * gl + 32, tsl], lhsT=Cr[:, gp, :], rhs=sre[:, gl, :], start=True, stop=False, tile_position=(0, 32 * gl)), r=["Cr", "sre"], w=[pyk])
                                P.pe(I("matmul", py[32 * gl:32 * gl + 32, tsl], lhsT=Cni[:, gp, :], rhs=sim[:, gl, :], start=False, stop=True, tile_position=(0, 32 * gl)), r=["Cni", "sim"], w=[pyk])
                    if own:
                        P.dve(I("scalar_tensor_tensor", out=yd, in0=uT[:, ut, :], scalar=dcol[:, ut:ut + 1], in1=py, op0=ALU.mult, op1=ALU.add), r=["uT", "dcol", pyk], w=["yd"])
                        P.pool(I("tensor_tensor", out=g1, in0=yd, in1=yd, op=ALU.mult), r=["yd"], w=["g1"])
                        P.pool(I("tensor_scalar", out=g1, in0=g1, scalar1=0.044715, scalar2=1.0, op0=ALU.mult, op1=ALU.add), r=["g1"], w=["g1"])
                        P.pool(I("tensor_tensor", out=g1, in0=g1, in1=yd, op=ALU.mult), r=["g1", "yd"], w=["g1"])
                        P.act(I("activation", out=g2t, in_=g1, func=AF.Sigmoid, scale=2.0 * math.sqrt(2.0 / math.pi)), r=["g1"], w=["g2t"])
                        P.pool(I("tensor_tensor", out=glT[:, ut, :], in0=yd, in1=g2t, op=ALU.mult), r=["yd", "g2t"], w=["glT"])
                if own:
                    for mo in range(4):
                        plo = bank(0); phi = bank(1)
                        for kt in range(4):
                            P.pe(I("matmul", plo, lhsT=wglu[:, kt, mo * 128:(mo + 1) * 128], rhs=glT[:, kt, :], start=(kt == 0), stop=(kt == 3)), r=["wglu", "glT"], w=["pb0"])
                        for kt in range(4):
                            P.pe(I("matmul", phi, lhsT=wglu[:, kt, 512 + mo * 128:512 + (mo + 1) * 128], rhs=glT[:, kt, :], start=(kt == 0), stop=(kt == 3)), r=["wglu", "glT"], w=["pb1"])
                        P.act(I("activation", out=sig, in_=phi, func=AF.Sigmoid), r=["pb1"], w=["sig"])
                        P.dve(I("tensor_tensor", out=y2[:, mo, :], in0=plo, in1=sig, op=ALU.mult), r=["pb0", "sig"], w=["y2"])
                    pms = bank(6)
                    for mo in range(4):
                        P.pool(I("tensor_tensor", out=sq, in0=y2[:, mo, :], in1=y2[:, mo, :], op=ALU.mult), r=["y2"], w=["sq"])
                        P.pe(I("matmul", pms, lhsT=ones, rhs=sq, start=(mo == 0), stop=(mo == 3)), r=["ones", "sq"], w=["pms"])
                    P.act(I("activation", out=rstd, in_=pms, func=AF.Ln, scale=1.0 / 512.0, bias=1e-5), r=["pms"], w=["rstd"])
                    P.act(I("activation", out=rstd, in_=rstd, func=AF.Exp, scale=-0.5), r=["rstd"], w=["rstd"])
                    for mo in range(4):
                        P.dve(I("scalar_tensor_tensor", out=soT[:, mo, :], in0=y2[:, mo, :], scalar=gncol[:, mo:mo + 1], in1=rstd, op0=ALU.mult, op1=ALU.mult), r=["y2", "gncol", "rstd"], w=["soT"])
                    P.dma(I("dma_start", out=MIXT[4:8, :, (s - 8) * 512:(s - 7) * 512].rearrange("h p t -> p h t"), in_=soT), r=["soT"], w=["MIXT"])
            P.barrier()
            AR.reset(gmark)


        G = AR.alloc([128, 32, 32])
        gmark = AR.mark()
        SC = 1.0 / math.sqrt(32.0)

        def layer_norm_tile(pre, gb, bb, dst, tg):
            stt = AR_t["st"]; mv = AR_t["mv"]; rs = AR_t["rs"]
            P.dve(I("bn_stats", out=stt[:, 0:6], in_=pre[:, 0:512]), r=[tg], w=["lnst"])
            P.dve(I("bn_stats", out=stt[:, 6:12], in_=pre[:, 512:1024]), r=[tg], w=["lnst2"])
            P.dve(I("bn_aggr", out=mv, in_=stt), r=["lnst", "lnst2"], w=["lnmv"])
            P.act(I("activation", out=rs, in_=mv[:, 1:2], func=AF.Ln, bias=1e-5), r=["lnmv"], w=["lnrs"])
            P.act(I("activation", out=rs, in_=rs, func=AF.Exp, scale=-0.5), r=["lnrs"], w=["lnrs"])
            P.dve(I("tensor_scalar", out=dst, in0=pre, scalar1=mv[:, 0:1], scalar2=rs[:, 0:1], op0=ALU.subtract, op1=ALU.mult), r=[tg, "lnmv", "lnrs"], w=[tg + "o"])
            P.pool(I("tensor_tensor", out=dst, in0=dst, in1=gb, op=ALU.mult), r=[tg + "o", "lng"], w=[tg + "o"])
            P.pool(I("tensor_tensor", out=dst, in0=dst, in1=bb, op=ALU.add), r=[tg + "o", "lng"], w=[tg + "o"])
        AR_t = {}

        if "2" in phases:
            slopes = [2.0 ** (-(h + 1)) for h in range(8)]
            tri = AR.alloc([128, 128]); trib = AR.alloc([128, 128], BF16); U = AR.alloc([128, 128]); kl = AR.alloc([128, 1])
            P.pool(I("memset", tri, 1.0), w=["tri"])
            P.pool(I("affine_select", out=tri, in_=tri, pattern=[[1, 128]], compare_op=ALU.is_ge, fill=0.0, base=0, channel_multiplier=-1), r=["tri"], w=["tri"])
            P.dve(I("tensor_copy", out=trib, in_=tri), r=["tri"], w=["trib"])
            P.pool(I("memset", U, 1.0), w=["U"])
            P.pool(I("affine_select", out=U, in_=U, pattern=[[1, 128]], compare_op=ALU.is_gt, fill=0.0, base=0, channel_multiplier=-1), r=["U"], w=["U"])
            P.pe(I("matmul", bank(0)[:, 0:1], lhsT=U, rhs=ones[:, 0:1], start=True, stop=True), r=["U", "ones"], w=["pb0"])
            P.dve(I("tensor_copy", out=kl, in_=bank(0)[:, 0:1]), r=["pb0"], w=["kl"])
            ND = 68
            bt = AR.alloc([128, 8, ND]); btp = AR.alloc([128, 8, ND]); prefc = AR.alloc([128, 1])
            P.dma(I("dma_start", out=prefc, in_=pref), w=["prefc"])
            for h in range(8):
                for Dd in range(ND):
                    fn = P.dve if (Dd % 2 == 0) else P.pool
                    fn(I("tensor_scalar", out=bt[:, h, Dd:Dd + 1], in0=kl, scalar1=slopes[h], scalar2=-slopes[h] * 128.0 * Dd, op0=ALU.mult, op1=ALU.add), r=["kl"], w=["bt%d" % (Dd % 2)])
            P.dve(I("tensor_scalar", out=btp, in0=bt, scalar1=prefc[:, 0:1], scalar2=None, op0=ALU.add), r=["bt0", "bt1", "prefc"], w=["btp"])
            lv = [AR.alloc([64, 32]) for _ in range(4)]; lt = AR.alloc([64, 32]); l1 = AR.alloc([64, 1]); l2 = AR.alloc([64, 1]); neglam = AR.alloc([64, 1]); gcol = AR.alloc([64, 1])
            for i_, src in enumerate((lq1, lk1, lq2, lk2)):
                P.dma(I("dma_start", out=lv[i_], in_=src.to_broadcast([64, 32])), w=["lv%d" % i_])
            P.dve(I("tensor_tensor", out=lt, in0=lv[0], in1=lv[1], op=ALU.mult), r=["lv0", "lv1"], w=["lt"])
            P.dve(I("reduce_sum", out=l1, in_=lt, axis=AX.X), r=["lt"], w=["l1"])
            P.dve(I("tensor_tensor", out=lt, in0=lv[2], in1=lv[3], op=ALU.mult), r=["lv2", "lv3", "l1"], w=["lt"])
            P.dve(I("reduce_sum", out=l2, in_=lt, axis=AX.X), r=["lt"], w=["l2"])
            P.act(I("activation", out=l1, in_=l1, func=AF.Exp), r=["l1"], w=["l1"])
            P.act(I("activation", out=l2, in_=l2, func=AF.Exp), r=["l2"], w=["l2"])
            P.dve(I("tensor_tensor", out=neglam, in0=l2, in1=l1, op=ALU.subtract), r=["l1", "l2"], w=["neglam"])
            P.dve(I("tensor_scalar", out=neglam, in0=neglam, scalar1=-LAMBDA_INIT, scalar2=None, op0=ALU.add), r=["neglam"], w=["neglam"])
            P.dma(I("dma_start", out=gcol, in_=subln_g), w=["gcol"])
            P.dve(I("tensor_scalar", out=gcol, in0=gcol, scalar1=1.0 - LAMBDA_INIT, scalar2=None, op0=ALU.mult), r=["gcol"], w=["gcol"])
            sel = AR.alloc([65, 64])
            P.dve(I("memset", sel, 0.0), w=["sel"])
            P.dve(I("memset", sel[64:65, :], 1.0), r=["sel"], w=["sel"])
            KTh = AR.alloc([128, L], BF16); QTh = AR.alloc([128, LO], BF16); Vh = AR.alloc([128, 64, 130], BF16)
            Pt = [AR.alloc([128, 2, 512], BF16) for _ in range(2)]
            Osb = AR.alloc([65, 2, 512]); Od = AR.alloc([65, 2, 512]); rL = AR.alloc([64, 2, 512])
            a1 = AR.alloc([64, 512]); a2 = AR.alloc([64, 512]); dif = AR.alloc([64, 512]); sq2 = AR.alloc([64, 512]); rs2 = AR.alloc([64, 512])
            oT = AR.alloc([64, 512], BF16)
            scb = [bank(0, 2).rearrange("p (a b) -> p a b", a=2), bank(2, 2).rearrange("p (a b) -> p a b", a=2)]
            Ooff = bank(4, 2).rearrange("p (a b) -> p a b", a=2); Odg = bank(6, 2).rearrange("p (a b) -> p a b", a=2)
            it = 0
            for hp in range(4):
                P.dma(I("dma_start", out=KTh, in_=KT[hp]), r=["KT"], w=["KTh"])
                P.dma(I("dma_start", out=QTh, in_=QT[hp]), r=["QT"], w=["QTh"])
                P.dma(I("dma_start", out=Vh, in_=VV[:, :, hp * 130:(hp + 1) * 130].rearrange("b p c -> p b c")), r=["VV"], w=["Vh"])
                for j in range(8):
                    for hl in range(2):
                        h = 2 * hp + hl; sl = slopes[h]
                        q0 = LO + 512 * j; kd0 = q0 // 128; klo = _qkey_lo(q0, sl)
                        offs = list(range(klo, kd0))
                        for ii, kb in enumerate(offs):
                            sc = scb[it % 2]; sk = "sc%d" % (it % 2); pt = Pt[it % 2]; pk = "Pt%d" % (it % 2); it += 1
                            for m_ in range(2):
                                pr = 32 * (2 * hl + m_)
                                P.pe(I("matmul", sc[:, m_, :], lhsT=KTh[pr:pr + 32, kb * 128:(kb + 1) * 128], rhs=QTh[pr:pr + 32, j * 512:(j + 1) * 512], start=True, stop=True, tile_position=(pr, 0)), r=["KTh", "QTh"], w=[sk])
                            btab = btp if kb < 32 else bt
                            P.act(I("activation", out=pt, in_=sc, func=AF.Exp, bias=btab[:, h, kd0 - kb:kd0 - kb + 1], scale=SC), r=[sk, "btp", "bt0", "bt1"], w=[pk])
                            for m_ in range(2):
                                P.pe(I("matmul", Ooff[0:65, m_, :], lhsT=Vh[:, kb, hl * 65:(hl + 1) * 65], rhs=pt[:, m_, :], start=(ii == 0), stop=(ii == len(offs) - 1)), r=["Vh", pk], w=["Ooff"])
                        for r_ in range(4):
                            kb = kd0 + r_
                            sc = scb[it % 2]; sk = "sc%d" % (it % 2); pt = Pt[it % 2]; pk = "Pt%d" % (it % 2); it += 1
                            c0 = 128 * r_
                            for m_ in range(2):
                                pr = 32 * (2 * hl + m_)
                                P.pe(I("matmul", sc[:, m_, c0:512], lhsT=KTh[pr:pr + 32, kb * 128:(kb + 1) * 128], rhs=QTh[pr:pr + 32, j * 512 + c0:(j + 1) * 512], start=True, stop=True, tile_position=(pr, 0)), r=["KTh", "QTh"], w=[sk])
                            for qs in range(r_, 4):
                                P.act(I("activation", out=pt[:, :, 128 * qs:128 * qs + 128], in_=sc[:, :, 128 * qs:128 * qs + 128], func=AF.Exp, bias=bt[:, h, qs - r_:qs - r_ + 1], scale=SC), r=[sk, "bt0", "bt1"], w=[pk])
                            P.pool(I("tensor_tensor", out=pt[:, :, c0:c0 + 128], in0=pt[:, :, c0:c0 + 128], in1=trib.unsqueeze(1).to_broadcast([128, 2, 128]), op=ALU.mult), r=[pk, "trib"], w=[pk])
                            for m_ in range(2):
                                P.pe(I("matmul", Odg[0:65, m_, c0:512], lhsT=Vh[:, kb, hl * 65:(hl + 1) * 65], rhs=pt[:, m_, c0:512], start=(r_ == 0), stop=(r_ == 3)), r=["Vh", pk], w=["Odg"])
                        P.act(I("activation", out=Od, in_=Odg[0:65], func=AF.Copy), r=["Odg"], w=["Od"])
                        for qs in range(4):
                            f = math.exp(-sl * 128.0 * qs)
                            cs = slice(128 * qs, 128 * qs + 128)
                            P.dve(I("scalar_tensor_tensor", out=Osb[:, :, cs], in0=Ooff[0:65, :, cs], scalar=f, in1=Od[:, :, cs], op0=ALU.mult, op1=ALU.add), r=["Ooff", "Od"], w=["Osb"])
                        for m_ in range(2):
                            P.pe(I("matmul", Odg[0:64, m_, :], lhsT=sel, rhs=Osb[:, m_, :], start=True, stop=True), r=["sel", "Osb", "Od"], w=["Odg"])
                        P.dve(I("reciprocal", out=rL, in_=Odg[0:64]), r=["Odg"], w=["rL"])
                        P.pool(I("tensor_tensor", out=a1, in0=Osb[0:64, 0, :], in1=rL[:, 0, :], op=ALU.mult), r=["Osb", "rL"], w=["a1"])
                        P.pool(I("tensor_tensor", out=a2, in0=Osb[0:64, 1, :], in1=rL[:, 1, :], op=ALU.mult), r=["Osb", "rL"], w=["a2"])
                        P.dve(I("scalar_tensor_tensor", out=dif, in0=a2, scalar=neglam[:, 0:1], in1=a1, op0=ALU.mult, op1=ALU.add), r=["a1", "a2", "neglam"], w=["dif"])
                        P.pool(I("tensor_tensor", out=sq2, in0=dif, in1=dif, op=ALU.mult), r=["dif"], w=["sq2"])
                        P.pe(I("matmul", Odg[0:64, 0, :], lhsT=ones[0:64, 0:64], rhs=sq2, start=True, stop=True), r=["ones", "sq2", "rL"], w=["Odg"])
                        P.act(I("activation", out=rs2, in_=Odg[0:64, 0, :], func=AF.Ln, scale=1.0 / 64.0, bias=1e-5), r=["Odg"], w=["rs2"])
                        P.act(I("activation", out=rs2, in_=rs2, func=AF.Exp, scale=-0.5), r=["rs2"], w=["rs2"])
                        P.dve(I("scalar_tensor_tensor", out=oT, in0=dif, scalar=gcol[:, 0:1], in1=rs2, op0=ALU.mult, op1=ALU.mult), r=["dif", "gcol", "rs2"], w=["oT"])
                        P.dma(I("dma_start", out=MIXT[h // 2, (h % 2) * 64:(h % 2) * 64 + 64, j * 512:(j + 1) * 512], in_=oT), r=["oT"], w=["MIXT"])
            P.barrier()
            AR.reset(gmark)

        def bcast_row(dst, src, n, key):
            P.dma(I("dma_start", out=dst, in_=src.to_broadcast([128, n])), w=[key])

        if "3" in phases:
            if os.environ.get('KZERO'):
                zt = AR.alloc([128, 8, 512], BF16)
                P.dve(I("memset", zt, 0.0), w=["zt"])
                for cc in range(8):
                    P.dma(I("dma_start", out=MIXT[:, :, cc * 512:(cc + 1) * 512].rearrange("k p t -> p k t"), in_=zt), r=["zt"], w=["MIXT"])
            wout = AR.alloc([128, 8, 1024], BF16)
            for kt in range(8):
                P.dma(I("dma_start", out=wout[:, kt, :], in_=w_out[kt * 128:(kt + 1) * 128, :]), w=["wout"], q="gpsimd")
            wr = AR.alloc([128, 8, 32])
            P.dma(I("dma_start", out=wr, in_=w_router.rearrange("(k p) e -> p k e", p=128)), w=["wr"])
            g1b = AR.alloc([128, 1024]); b1b = AR.alloc([128, 1024]); brb = AR.alloc([128, 32])
            bcast_row(g1b, ln1_g, 1024, "lng"); bcast_row(b1b, ln1_b, 1024, "lng"); bcast_row(brb, b_router, 32, "brb")
            AR_t["st"] = AR.alloc([128, 12]); AR_t["mv"] = AR.alloc([128, 2]); AR_t["rs"] = AR.alloc([128, 1])
            mixT = [AR.alloc([128, 8, 128], BF16) for _ in range(2)]
            xt_ = [AR.alloc([128, 1024]) for _ in range(2)]
            pre = AR.alloc([128, 1024]); x1 = [AR.alloc([128, 1024]) for _ in range(2)]
            x1Tf = AR.alloc([128, 8, 128]); x1Tb = [AR.alloc([128, 8, 128], BF16) for _ in range(2)]
            lg = AR.alloc([128, 32]); mx8 = AR.alloc([128, 8]); msk = AR.alloc([128, 32]); ex = AR.alloc([128, 32]); nmx = AR.alloc([128, 1]); ssum = AR.alloc([128, 1])
            CUT = int(os.environ.get('KCUT', '99'))
            for t in range(int(os.environ.get('KNT3', '32'))):
                b_ = t % 2
                P.dma(I("dma_start", out=mixT[b_], in_=MIXT[:, :, t * 128:(t + 1) * 128].rearrange("k p t -> p k t")), r=["MIXT"], w=["mixT%d" % b_])
                P.dma(I("dma_start", out=xt_[b_], in_=xc[LO + t * 128:LO + (t + 1) * 128, :]), w=["xt%d" % b_])
                if CUT < 2: continue
                pm = bank(0, 2)
                for dh in range(2):
                    for kt in range(8):
                        P.pe(I("matmul", pm[:, dh * 512:(dh + 1) * 512], lhsT=mixT[b_][:, kt, :], rhs=wout[:, kt, dh * 512:(dh + 1) * 512], start=(kt == 0), stop=(kt == 7)), r=["mixT%d" % b_, "wout"], w=["pm"])
                P.dve(I("scalar_tensor_tensor", out=pre, in0=xt_[b_], scalar=ALPHA, in1=pm, op0=ALU.mult, op1=ALU.add), r=["xt%d" % b_, "pm"], w=["pre"])
                if CUT < 3: continue
                layer_norm_tile(pre, g1b, b1b, x1[b_], "pre")
                if CUT < 4: continue
                P.dma(I("dma_start", out=X1[t * 128:(t + 1) * 128, :], in_=x1[b_]), r=["preo"], w=["X1"])
                if CUT < 5: continue
                ptr = bank(2, 2).rearrange("p (a b) -> p a b", a=8)
                for kt in range(8):
                    P.pe(I("transpose", out=ptr[:, kt, :], in_=x1[b_][:, kt * 128:(kt + 1) * 128], identity=ident), r=["preo", "ident"], w=["ptr"])
                KS = os.environ.get('KSUB', 'abc')
                if 'a' in KS:
                    P.act(I("activation", out=x1Tf, in_=ptr, func=AF.Copy), r=["ptr"], w=["x1Tf"])
                if 'b' in KS:
                    P.act(I("activation", out=x1Tb[b_], in_=ptr, func=AF.Copy), r=["ptr"], w=["x1Tb%d" % b_])
                if 'c' in KS:
                    P.dma(I("dma_start", out=X1T[:, :, t * 128:(t + 1) * 128].rearrange("k p t -> p k t"), in_=x1Tb[b_]), r=["x1Tb%d" % b_], w=["X1T"])
                if CUT < 6: continue
                pl = bank(4)[:, 0:32]
                for kt in range(8):
                    P.pe(I("matmul", pl, lhsT=x1Tf[:, kt, :], rhs=wr[:, kt, :], start=(kt == 0), stop=(kt == 7)), r=["x1Tf", "wr"], w=["pl"])
                if CUT < 7: continue
                P.dve(I("tensor_tensor", out=lg, in0=pl, in1=brb, op=ALU.add), r=["pl", "brb"], w=["lg"])
                P.dve(I("max", out=mx8, in_=lg), r=["lg"], w=["mx8"])
                P.dve(I("tensor_scalar", out=msk, in0=lg, scalar1=mx8[:, 3:4], scalar2=1e-7, op0=ALU.subtract, op1=ALU.add), r=["lg", "mx8"], w=["msk"])
                P.dve(I("tensor_scalar", out=msk, in0=msk, scalar1=1e10, scalar2=0.0, op0=ALU.mult, op1=ALU.max), r=["msk"], w=["msk"])
                P.dve(I("tensor_scalar", out=msk, in0=msk, scalar1=1.0, scalar2=None, op0=ALU.min), r=["msk"], w=["msk"])
                P.dve(I("tensor_scalar", out=nmx, in0=mx8[:, 0:1], scalar1=-1.0, scalar2=None, op0=ALU.mult), r=["mx8"], w=["nmx"])
                P.act(I("activation", out=ex, in_=lg, func=AF.Exp, bias=nmx[:, 0:1]), r=["lg", "nmx"], w=["ex"])
                P.dve(I("tensor_tensor", out=ex, in0=ex, in1=msk, op=ALU.mult), r=["ex", "msk"], w=["ex"])
                P.dve(I("reduce_sum", out=ssum, in_=ex, axis=AX.X), r=["ex"], w=["ssum"])
                P.dve(I("reciprocal", out=ssum, in_=ssum), r=["ssum"], w=["ssum"])
                P.dve(I("tensor_scalar", out=G[:, t, :], in0=ex, scalar1=ssum[:, 0:1], scalar2=None, op0=ALU.mult), r=["ex", "ssum"], w=["G"])
            P.barrier()
            AR.reset(gmark)

        if "4" in phases:
            Wgu = [AR.alloc([128, 8, 2, 1024], BF16) for _ in range(2)]
            Wd = [AR.alloc([128, 8, 1024], BF16) for _ in range(2)]
            stage = [AR.alloc([128, 2048]) for _ in range(2)]
            X1Tc = AR.alloc([128, 8, 1024], BF16); acc = AR.alloc([128, 8, 1024]); actT = AR.alloc([128, 8, 512], BF16)
            tg = AR.alloc([128, 512]); tsg = AR.alloc([128, 512]); tl = AR.alloc([128, 512])
            BGU = AR.alloc([128, 8, 2, 32]); bd = AR.alloc([32, 1024]); GTc = AR.alloc([32, 8, 128])
            bgn = AR.alloc([32, 2048])
            P.dma(I("dma_start", out=bgn, in_=b_gu), w=["bgn"])
            bgv = bgn.rearrange("e (ft p two) -> e ft two p", p=128, two=2)
            pbg = bank(7).rearrange("p (a b c) -> p a b c", a=8, b=2)
            for ft in range(8):
                for two in range(2):
                    P.pe(I("transpose", out=pbg[:, ft, two, :], in_=bgv[:, ft, two, :], identity=ident[0:32, 0:32]), r=["bgn", "ident"], w=["B7"])
            P.dve(I("tensor_copy", out=BGU, in_=pbg), r=["B7"], w=["BGU"])
            P.dma(I("dma_start", out=bd, in_=b_dn), w=["bd"])
            stn = [0]

            def load_expert(e):
                b_ = e % 2
                for kt in range(8):
                    sb_ = stn[0] % 2; stn[0] += 1
                    P.dma(I("dma_start", out=stage[sb_], in_=w_gu[e, kt * 128:(kt + 1) * 128, :]), w=["stage%d" % sb_])
                    src = stage[sb_].rearrange("p (f two) -> p two f", two=2)
                    if kt % 2 == 0:
                        P.act(I("activation", out=Wgu[b_][:, kt, :, :], in_=src, func=AF.Copy), r=["stage%d" % sb_], w=["Wgu%d" % b_])
                    else:
                        P.pool(I("tensor_copy", out=Wgu[b_][:, kt, :, :], in_=src), r=["stage%d" % sb_], w=["Wgu%d" % b_])
                for k2 in range(4):
                    sb_ = stn[0] % 2; stn[0] += 1
                    P.dma(I("dma_start", out=stage[sb_].rearrange("p (k f) -> p k f", k=2), in_=w_dn[e, k2 * 256:(k2 + 1) * 256, :].rearrange("(k p) f -> p k f", p=128)), w=["stage%d" % sb_])
                    src = stage[sb_].rearrange("p (k f) -> p k f", k=2)
                    if k2 % 2 == 0:
                        P.act(I("activation", out=Wd[b_][:, 2 * k2:2 * k2 + 2, :], in_=src, func=AF.Copy), r=["stage%d" % sb_], w=["Wd%d" % b_])
                    else:
                        P.pool(I("tensor_copy", out=Wd[b_][:, 2 * k2:2 * k2 + 2, :], in_=src), r=["stage%d" % sb_], w=["Wd%d" % b_])

            for c in range(NCH):
                P.dma(I("dma_start", out=X1Tc, in_=X1T[:, :, c * 1024:(c + 1) * 1024].rearrange("k p t -> p k t")), r=["X1T"], w=["X1Tc"])
                pg = bank(6)[0:32, :].rearrange("p (a b) -> p a b", a=4)
                for half in range(2):
                    for i_ in range(4):
                        tl_ = half * 4 + i_
                        P.pe(I("transpose", out=pg[:, i_, :], in_=G[:, c * 8 + tl_, :], identity=ident), r=["G", "ident"], w=["pg"])
                    P.dve(I("tensor_copy", out=GTc[:, half * 4:half * 4 + 4, :], in_=pg), r=["pg"], w=["GTc"])
                for tl_ in range(8):
                    pa = bank(0, 2)
                    for dh in range(2):
                        P.pe(I("matmul", pa[:, dh * 512:(dh + 1) * 512], lhsT=GTc[:, tl_, :], rhs=bd[:, dh * 512:(dh + 1) * 512], start=True, stop=True), r=["GTc", "bd"], w=["pa"])
                    P.dve(I("tensor_copy", out=acc[:, tl_, :], in_=pa), r=["pa"], w=["acc"])
                load_expert(0)
                for e in range(NE):
                    if e + 1 < NE:
                        load_expert(e + 1)
                    b_ = e % 2
                    for tc in range(2):
                        for ft in range(8):
                            pgl = bank((ft % 2) * 2); pln = bank((ft % 2) * 2 + 1); gk = "pgl%d" % (ft % 2)
                            for kt in range(8):
                                P.pe(I("matmul", pgl, lhsT=Wgu[b_][:, kt, 0, ft * 128:(ft + 1) * 128], rhs=X1Tc[:, kt, tc * 512:(tc + 1) * 512], start=(kt == 0), stop=(kt == 7)), r=["Wgu%d" % b_, "X1Tc"], w=[gk])
                            for kt in range(8):
                                P.pe(I("matmul", pln, lhsT=Wgu[b_][:, kt, 1, ft * 128:(ft + 1) * 128], rhs=X1Tc[:, kt, tc * 512:(tc + 1) * 512], start=(kt == 0), stop=(kt == 7)), r=["Wgu%d" % b_, "X1Tc"], w=[gk + "l"])
                            P.dve(I("tensor_scalar", out=tg, in0=pgl, scalar1=BGU[:, ft, 0, e:e + 1], scalar2=7.0, op0=ALU.add, op1=ALU.min), r=[gk, "BGU"], w=["tg"])
                            P.act(I("activation", out=tsg, in_=tg, func=AF.Sigmoid, scale=1.702), r=["tg"], w=["tsg"])
                            P.dve(I("tensor_scalar", out=tl, in0=pln, scalar1=BGU[:, ft, 1, e:e + 1], scalar2=7.0, op0=ALU.add, op1=ALU.min), r=[gk + "l", "BGU"], w=["tl"])
                            P.pool(I("tensor_scalar", out=tl, in0=tl, scalar1=-7.0, scalar2=1.0, op0=ALU.max, op1=ALU.add), r=["tl"], w=["tl"])
                            P.pool(I("tensor_tensor", out=tg, in0=tg, in1=tsg, op=ALU.mult), r=["tg", "tsg"], w=["tg"])
                            P.pool(I("tensor_tensor", out=actT[:, ft, :], in0=tg, in1=tl, op=ALU.mult), r=["tg", "tl"], w=["actT"])
                        for ti in range(4):
                            tl_ = tc * 4 + ti
                            for dh in range(2):
                                pdn = bank(4 + (ti * 2 + dh) % 4); dk = "pdn%d" % ((ti * 2 + dh) % 4)
                                for ft in range(8):
                                    P.pe(I("matmul", pdn, lhsT=actT[:, ft, ti * 128:(ti + 1) * 128], rhs=Wd[b_][:, ft, dh * 512:(dh + 1) * 512], start=(ft == 0), stop=(ft == 7)), r=["actT", "Wd%d" % b_], w=[dk])
                                P.dve(I("scalar_tensor_tensor", out=acc[:, tl_, dh * 512:(dh + 1) * 512], in0=pdn, scalar=G[:, c * 8 + tl_, e:e + 1], in1=acc[:, tl_, dh * 512:(dh + 1) * 512], op0=ALU.mult, op1=ALU.add), r=[dk, "G", "acc"], w=["acc"])
                for tl_ in range(8):
                    t = c * 8 + tl_
                    sb_ = stn[0] % 2; stn[0] += 1
                    xs = stage[sb_][:, 0:1024]
                    P.dma(I("dma_start", out=xs, in_=X1[t * 128:(t + 1) * 128, :]), r=["X1"], w=["stage%d" % sb_])
                    P.dve(I("scalar_tensor_tensor", out=xs, in0=xs, scalar=ALPHA, in1=acc[:, tl_, :], op0=ALU.mult, op1=ALU.add), r=["stage%d" % sb_, "acc"], w=["stage%d" % sb_])
                    P.dma(I("dma_start", out=RR[t * 128:(t + 1) * 128, :], in_=xs), r=["stage%d" % sb_], w=["RR"])
            P.barrier()
            AR.reset(gmark)

        if "5" in phases:
            wpg = AR.alloc([128, 8, 1024], BF16); wpp = AR.alloc([128, 2, 1024], BF16)
            for kt in range(8):
                P.dma(I("dma_start", out=wpg[:, kt, :], in_=w_pg[kt * 128:(kt + 1) * 128, :]), w=["wpg"], q="gpsimd")
            for kt in range(2):
                P.dma(I("dma_start", out=wpp[:, kt, :], in_=w_pp[kt * 128:(kt + 1) * 128, :]), w=["wpp"], q="gpsimd")
            g2b = AR.alloc([128, 1024]); b2b = AR.alloc([128, 1024])
            bcast_row(g2b, ln2_g, 1024, "lng"); bcast_row(b2b, ln2_b, 1024, "lng")
            AR_t["st"] = AR.alloc([128, 12]); AR_t["mv"] = AR.alloc([128, 2]); AR_t["rs"] = AR.alloc([128, 1])
            rt = [AR.alloc([128, 1024]) for _ in range(2)]; ptl = [AR.alloc([128, 256]) for _ in range(2)]
            rT = AR.alloc([128, 8, 128], BF16); pT = AR.alloc([128, 2, 128], BF16)
            sgt = AR.alloc([128, 1024]); yv = AR.alloc([128, 1024]); yo = [AR.alloc([128, 1024]) for _ in range(2)]
            for t in range(32):
                b_ = t % 2
                P.dma(I("dma_start", out=rt[b_], in_=RR[t * 128:(t + 1) * 128, :]), r=["RR"], w=["rt%d" % b_])
                P.dma(I("dma_start", out=ptl[b_], in_=pc[t * 128:(t + 1) * 128, :]), w=["ptl%d" % b_])
                ptr = bank(4, 2).rearrange("p (a b) -> p a b", a=8)
                for kt in range(8):
                    P.pe(I("transpose", out=ptr[:, kt, :], in_=rt[b_][:, kt * 128:(kt + 1) * 128], identity=ident), r=["rt%d" % b_, "ident"], w=["ptr5"])
                P.act(I("activation", out=rT, in_=ptr, func=AF.Copy), r=["ptr5"], w=["rT"])
                ptp = bank(6).rearrange("p (a b) -> p a b", a=4)
                for kt in range(2):
                    P.pe(I("transpose", out=ptp[:, kt, :], in_=ptl[b_][:, kt * 128:(kt + 1) * 128], identity=ident), r=["ptl%d" % b_, "ident"], w=["ptp"])
                P.dve(I("tensor_copy", out=pT, in_=ptp[:, 0:2, :]), r=["ptp"], w=["pT"])
                pgt = bank(0, 2); ppp = bank(2, 2)
                for dh in range(2):
                    for kt in range(8):
                        P.pe(I("matmul", pgt[:, dh * 512:(dh + 1) * 512], lhsT=rT[:, kt, :], rhs=wpg[:, kt, dh * 512:(dh + 1) * 512], start=(kt == 0), stop=(kt == 7)), r=["rT", "wpg"], w=["pgt"])
                    for kt in range(2):
                        P.pe(I("matmul", ppp[:, dh * 512:(dh + 1) * 512], lhsT=pT[:, kt, :], rhs=wpp[:, kt, dh * 512:(dh + 1) * 512], start=(kt == 0), stop=(kt == 1)), r=["pT", "wpp"], w=["ppp"])
                P.act(I("activation", out=sgt, in_=pgt, func=AF.Sigmoid), r=["pgt"], w=["sgt"])
                P.dve(I("tensor_tensor", out=sgt, in0=sgt, in1=ppp, op=ALU.mult), r=["sgt", "ppp"], w=["sgt"])
                P.pool(I("tensor_tensor", out=yv, in0=sgt, in1=rt[b_], op=ALU.add), r=["sgt", "rt%d" % b_], w=["yv"])
                layer_norm_tile(yv, g2b, b2b, yo[b_], "yv")
                P.dma(I("dma_start", out=out[t * 128:(t + 1) * 128, :], in_=yo[b_]), r=["yvo"], w=["out"])
            P.barrier()

        P.emit(st)
    return nc


_NC_CACHE = {}


def _prep_inputs(inputs):
    sq = lambda k: np.ascontiguousarray(np.asarray(inputs[k])[0])
    x = np.asarray(inputs["x"]); p = np.asarray(inputs["p"])[0]
    shared = {
        "w_in": sq("w_in"), "lambda_q1": np.asarray(inputs["lambda_q1"]), "lambda_k1": np.asarray(inputs["lambda_k1"]),
        "lambda_q2": np.asarray(inputs["lambda_q2"]), "lambda_k2": np.asarray(inputs["lambda_k2"]),
        "subln_g": np.ascontiguousarray(np.asarray(inputs["subln_g"]).reshape(64, 1)),
        "ssm_a_re": sq("ssm_a_re"), "ssm_a_im": sq("ssm_a_im"), "ssm_log_dt": np.asarray(inputs["ssm_log_dt"]),
        "ssm_b_re": sq("ssm_b_re"), "ssm_b_im": sq("ssm_b_im"), "ssm_c_re": sq("ssm_c_re"), "ssm_c_im": sq("ssm_c_im"),
        "ssm_d": sq("ssm_d"), "w_glu": sq("w_glu"), "ssm_norm_g": sq("ssm_norm_g"), "w_out": sq("w_out"),
        "ln1_g": np.asarray(inputs["ln1_g"]), "ln1_b": np.asarray(inputs["ln1_b"]),
        "w_router": sq("w_router"), "b_router": np.asarray(inputs["b_router"]),
        "w_gate_up": sq("w_gate_up")[:NE], "b_gate_up": sq("b_gate_up"), "w_down": sq("w_down")[:NE], "b_down": sq("b_down"),
        "w_ple_gate": sq("w_ple_gate"), "w_ple_proj": sq("w_ple_proj"),
        "ln2_g": np.asarray(inputs["ln2_g"]), "ln2_b": np.asarray(inputs["ln2_b"]),
    }
    shared = {k: np.ascontiguousarray(v, dtype=np.float32) for k, v in shared.items()}
    maps = []
    for c in range(8):
        b, h = c // 2, c % 2
        if h == 0:
            xcore = np.concatenate([np.zeros((LO, DM), np.float32), x[b, :LO]], axis=0)
        else:
            xcore = np.ascontiguousarray(x[b])
        m = dict(shared)
        m["xc"] = np.ascontiguousarray(xcore, dtype=np.float32)
        m["pc"] = np.ascontiguousarray(p[b, h * LO:(h + 1) * LO], dtype=np.float32)
        m["pref"] = np.full((128, 1), NEG if h == 0 else 0.0, np.float32)
        maps.append(m)
    return maps


def kernel(**inputs):
    dbg = KDEBUG
    if dbg not in _NC_CACHE:
        _NC_CACHE[dbg] = build_program(dbg)
    nc = _NC_CACHE[dbg]
    maps = _prep_inputs(inputs)
    res = run_bass_kernel_spmd(nc, maps, core_ids=list(range(8)))
    if dbg:
        return res.results
    outp = np.zeros((4, L, DM), np.float32)
    for c in range(8):
        b, h = c // 2, c % 2
        outp[b, h * LO:(h + 1) * LO] = res.results[c]["out"]
    return outp
```

```python
import math
import os
from contextlib import ExitStack

import numpy as np
import concourse.bass as bass
import concourse.mybir as mybir
from concourse.bass_utils import run_bass_kernel_spmd

F32 = mybir.dt.float32
BF16 = mybir.dt.bfloat16
AF = mybir.ActivationFunctionType
ALU = mybir.AluOpType
AX = mybir.AxisListType

ENGINES = ("tensor", "vector", "scalar", "gpsimd", "sync")
SAME_ENGINE_SYNC = True
KDEBUG = os.environ.get("KDEBUG", "")

L = 8192
LO = 4096
DM = 1024
ALPHA = 2.0 ** 0.25
LAMBDA_INIT = 0.2
ATT_TH = 64.0
NEG = -30000.0
NE = int(os.environ.get('KNE', '32'))
NCH = int(os.environ.get('KNC', '4'))


KEYMAP = {
    "pb0": ["B0"], "pb1": ["B1"], "pbC": ["B2"], "pbu_re": ["B2"], "pbu_im": ["B3"], "py0": ["B4"], "py1": ["B5"], "pms": ["B6"],
    "sc0": ["B0", "B1"], "sc1": ["B2", "B3"], "Ooff": ["B4", "B5"], "Odg": ["B6", "B7"],
    "pm": ["B0", "B1"], "ptr": ["B2", "B3"], "pl": ["B4"],
    "pa": ["B0", "B1"], "pg": ["B6"], "pgl0": ["B0"], "pgl0l": ["B1"], "pgl1": ["B2"], "pgl1l": ["B3"],
    "pdn0": ["B4"], "pdn1": ["B5"], "pdn2": ["B6"], "pdn3": ["B7"],
    "ptr5": ["B4", "B5"], "ptp": ["B6"], "pgt": ["B0", "B1"], "ppp": ["B2", "B3"],
}


def _mapkeys(ks):
    out = []
    for k in ks:
        out.extend(KEYMAP.get(k, [k]))
    return out


class Prog:
    def __init__(self, nc, n_dma_sems=24):
        self.nc = nc
        self.ops = []
        self.last_w = {}
        self.readers = {}
        self.n_dma_sems = n_dma_sems
        self.last_of = {e: None for e in ENGINES}
        self.recent_dma = {e: [] for e in ENGINES}
        self.pe_sync = False

    def op(self, eng, fn, r=(), w=(), dma=False, extra=()):
        idx = len(self.ops)
        deps = set(extra)
        r = _mapkeys(r); w = _mapkeys(w)
        for k in r:
            if k in self.last_w:
                deps.add(self.last_w[k])
        for k in w:
            if k in self.last_w:
                deps.add(self.last_w[k])
            for x in self.readers.get(k, ()):
                deps.add(x)
        for k in w:
            self.last_w[k] = idx
            self.readers[k] = []
        for k in r:
            if k not in w:
                self.readers.setdefault(k, []).append(idx)
        deps.discard(idx)
        self.ops.append(dict(eng=eng, fn=fn, deps=deps, dma=dma, idx=idx, pesync=self.pe_sync))
        self.last_of[eng] = idx
        if dma:
            self.recent_dma[eng].append(idx)
            self.recent_dma[eng] = self.recent_dma[eng][-self.n_dma_sems:]
        return idx

    def pe(self, fn, r=(), w=()): return self.op("tensor", fn, r, w)
    def dve(self, fn, r=(), w=()): return self.op("vector", fn, r, w)
    def act(self, fn, r=(), w=()): return self.op("scalar", fn, r, w)
    def pool(self, fn, r=(), w=()): return self.op("gpsimd", fn, r, w)
    def dma(self, fn, r=(), w=(), q="sync"): return self.op(q, fn, r, w, dma=True)

    def barrier(self):
        deps = set()
        for e in ENGINES:
            if self.last_of[e] is not None:
                deps.add(self.last_of[e])
            deps.update(self.recent_dma[e])
        for e in ENGINES:
            self.op(e, None, extra=tuple(deps))
        self.last_w = {}
        self.readers = {}

    def emit(self, stack):
        nc = self.nc
        ops = self.ops
        needed = set()
        for o in ops:
            for d in o["deps"]:
                if o["eng"] == "tensor" and ops[d]["eng"] == "tensor" and o["fn"] is not None and not o["pesync"]:
                    continue
                needed.add(d)
        for e in ENGINES:
            if self.last_of[e] is not None:
                needed.add(self.last_of[e])
        csem = {e: stack.enter_context(nc.semaphore(f"c_{e}")) for e in ENGINES}
        ccount = {e: 0 for e in ENGINES}
        dsems, dcount = {}, {}
        dnext = {e: 0 for e in ENGINES}
        for e in ("sync", "gpsimd", "scalar"):
            dsems[e] = [stack.enter_context(nc.semaphore(f"d_{e}_{i}")) for i in range(self.n_dma_sems)]
            dcount[e] = [0] * self.n_dma_sems
        for o in ops:
            e = o["eng"]
            if o["fn"] is None:
                o["sig"] = None
            elif o["dma"]:
                j = dnext[e]
                dnext[e] = (j + 1) % self.n_dma_sems
                o["dma_prev"] = (dsems[e][j], dcount[e][j])
                dcount[e][j] += 16
                o["sig"] = (dsems[e][j], dcount[e][j])
            elif o["idx"] in needed:
                ccount[e] += 1
                o["sig"] = (csem[e], ccount[e])
            else:
                o["sig"] = None
        def resolve(d, seen):
            od = ops[d]
            if od["fn"] is not None:
                return [d]
            out = []
            for dd in od["deps"]:
                if dd not in seen:
                    seen.add(dd)
                    out.extend(resolve(dd, seen))
            return out
        waited = {e: {} for e in ENGINES}
        per_eng = {e: [o for o in ops if o["eng"] == e] for e in ENGINES}
        block = stack.enter_context(nc.Block())

        def make(e):
            def body(eng):
                wd = waited[e]

                def wait(sem, val):
                    if val <= 0:
                        return
                    key = id(sem)
                    if wd.get(key, 0) >= val:
                        return
                    eng.wait_ge(sem, val)
                    wd[key] = val
                for o in per_eng[e]:
                    alld = []
                    seen = set()
                    for d in sorted(o["deps"]):
                        alld.extend(resolve(d, seen))
                    for d in sorted(set(alld)):
                        od = ops[d]
                        if od["sig"] is None:
                            continue
                        if (not od["dma"]) and od["eng"] == e and (not SAME_ENGINE_SYNC or (e == "tensor" and not o["pesync"])):
                            continue
                        wait(*od["sig"])
                    if o["fn"] is None:
                        continue
                    if o["dma"]:
                        wait(*o["dma_prev"])
                    ins = o["fn"](eng)
                    if o["sig"] is not None:
                        ins.then_inc(o["sig"][0], 16 if o["dma"] else 1)
                if e == "sync":
                    for q in dsems:
                        for j in range(self.n_dma_sems):
                            wait(dsems[q][j], dcount[q][j])
                    for e2 in ENGINES:
                        if e2 != "sync":
                            wait(csem[e2], ccount[e2])
            return body

        block.tensor(make("tensor"))
        block.vector(make("vector"))
        block.scalar(make("scalar"))
        block.gpsimd(make("gpsimd"))
        block.sync(make("sync"))


def I(name, *a, **k):
    return lambda e: getattr(e, name)(*a, **k)


class Arena:
    def __init__(self, ap_f32, nbytes):
        self.ap = ap_f32
        self.n = nbytes
        self.off = 0

    def mark(self): return self.off
    def reset(self, m): self.off = m

    def alloc(self, shape, dt=F32, parts=128):
        esz = 4 if dt == F32 else 2
        n = int(np.prod(shape[1:])) * esz
        n4 = (n + 3) // 4
        assert self.off + n4 * 4 <= self.n, f"arena overflow {self.off + n4 * 4} > {self.n}"
        a = self.ap[0:shape[0], self.off // 4:self.off // 4 + n4]
        self.off += n4 * 4
        if dt != F32:
            a = a.bitcast(dt)
        if len(shape) == 3:
            a = a.rearrange("p (a b) -> p a b", a=shape[1])
        elif len(shape) == 4:
            a = a.rearrange("p (a b c) -> p a b c", a=shape[1], b=shape[2])
        return a


def _qkey_lo(q0, slope):
    kmin = q0 - ATT_TH / slope
    return max(0, int(math.floor(kmin / 128.0)))


def build_program(dbg=""):
    nc = bass.Bass("TRN2", target_bir_lowering=False)
    D = {}

    def din(name, shape, dt=F32):
        D[name] = nc.dram_tensor(name, list(shape), dt, kind="ExternalInput").ap()
        return D[name]

    def dscr(name, shape, dt):
        kind = "ExternalOutput" if name in dbg.split(",") else "Internal"
        D[name] = nc.dram_tensor(name, list(shape), dt, kind=kind).ap()
        return D[name]

    xc = din("xc", [L, DM]); pc = din("pc", [LO, 256]); pref = din("pref", [128, 1])
    w_in = din("w_in", [1024, 2048])
    lq1 = din("lambda_q1", [1, 32]); lk1 = din("lambda_k1", [1, 32]); lq2 = din("lambda_q2", [1, 32]); lk2 = din("lambda_k2", [1, 32])
    subln_g = din("subln_g", [64, 1])
    a_re = din("ssm_a_re", [32, 64]); a_im = din("ssm_a_im", [32, 64]); log_dt = din("ssm_log_dt", [1, 32])
    b_re = din("ssm_b_re", [32, 64, 16]); b_im = din("ssm_b_im", [32, 64, 16])
    c_re = din("ssm_c_re", [32, 16, 64]); c_im = din("ssm_c_im", [32, 16, 64])
    ssm_d = din("ssm_d", [512]); w_glu = din("w_glu", [512, 1024]); ssm_norm_g = din("ssm_norm_g", [512])
    w_out = din("w_out", [1024, 1024]); ln1_g = din("ln1_g", [1, 1024]); ln1_b = din("ln1_b", [1, 1024])
    w_router = din("w_router", [1024, 32]); b_router = din("b_router", [1, 32])
    phases = os.environ.get("KPHASES", "12345")
    if "4" in phases:
        w_gu = din("w_gate_up", [NE, 1024, 2048]); b_gu = din("b_gate_up", [32, 2048])
        w_dn = din("w_down", [NE, 1024, 1024]); b_dn = din("b_down", [32, 1024])
    w_pg = din("w_ple_gate", [1024, 1024]); w_pp = din("w_ple_proj", [256, 1024])
    ln2_g = din("ln2_g", [1, 1024]); ln2_b = din("ln2_b", [1, 1024])
    out = nc.dram_tensor("out", [LO, DM], F32, kind="ExternalOutput").ap()

    KT = dscr("KT", [4, 128, L], BF16); QT = dscr("QT", [4, 128, LO], BF16)
    VV = dscr("VV", [64, 128, 520], BF16)
    MIXT = dscr("MIXT", [8, 128, LO], BF16)
    X1 = dscr("X1", [LO, DM], F32); X1T = dscr("X1T", [8, 128, LO], BF16)
    RR = dscr("RR", [LO, DM], F32)

    with ExitStack() as st:
        st.enter_context(nc.allow_non_contiguous_dma(reason="layout"))
        ARENA_B = 200 * 1024
        arena_t = st.enter_context(nc.sbuf_tensor("arena", [128, ARENA_B // 4], F32))
        psum_t = st.enter_context(nc.psum_tensor("psum", [128, 4096], F32))
        AR = Arena(arena_t, ARENA_B)
        P = Prog(nc)

        def bank(i, n=1):
            return psum_t[:, 512 * i:512 * (i + n)]

        ident = AR.alloc([128, 128]); ones = AR.alloc([128, 128])
        P.pool(I("memset", ident, 1.0), w=["ident"])
        P.pool(I("affine_select", out=ident, in_=ident, pattern=[[-1, 128]], compare_op=ALU.is_equal,
                                         fill=0.0, base=0, channel_multiplier=1), r=["ident"], w=["ident"])
        P.pool(I("memset", ones, 1.0), w=["ones"])
        gmark = AR.mark()

        if "1" in phases:
            P.pe_sync = True
            win = AR.alloc([128, 8, 2048], BF16)
            for kt in range(8):
                P.dma(I("dma_start", out=win[:, kt, :], in_=w_in[kt * 128:(kt + 1) * 128, :]), w=["win"], q="gpsimd")
            wglu = AR.alloc([128, 4, 1024], BF16)
            for kt in range(4):
                P.dma(I("dma_start", out=wglu[:, kt, :], in_=w_glu[kt * 128:(kt + 1) * 128, :]), w=["wglu"], q="gpsimd")
            sm = lambda: AR.alloc([128, 16])
            are, aim, dtt, rho, th, cth, sth, Are, Aim, t0, t1, t2, Fre, Fim, d2 = [sm() for _ in range(15)]
            P.dma(I("dma_start", out=are, in_=a_re.rearrange("(gp g2) p -> (g2 p) gp", g2=2)), w=["are"])
            P.dma(I("dma_start", out=aim, in_=a_im.rearrange("(gp g2) p -> (g2 p) gp", g2=2)), w=["aim"])
            ldv = log_dt.rearrange("o (gp g2) -> o g2 gp", g2=2)
            for g2 in range(2):
                P.dma(I("dma_start", out=dtt[64 * g2:64 * g2 + 64, :], in_=ldv[:, g2, :].to_broadcast([64, 16])), w=["dtt"])
            P.act(I("activation", out=dtt, in_=dtt, func=AF.Exp), r=["dtt"], w=["dtt"])
            P.dve(I("tensor_tensor", out=t0, in0=are, in1=dtt, op=ALU.mult), r=["are", "dtt"], w=["t0"])
            P.act(I("activation", out=rho, in_=t0, func=AF.Exp), r=["t0"], w=["rho"])
            P.dve(I("tensor_tensor", out=th, in0=aim, in1=dtt, op=ALU.mult), r=["aim", "dtt"], w=["th"])
            P.dve(I("memset", t1, 0.0), w=["t1"])
            for kk in range(6):
                thr = (2 * kk + 1) * math.pi
                P.dve(I("tensor_scalar", out=t2, in0=th, scalar1=-thr, scalar2=1e6, op0=ALU.add, op1=ALU.mult), r=["th"], w=["t2"])
                P.dve(I("tensor_scalar", out=t2, in0=t2, scalar1=0.0, scalar2=1.0, op0=ALU.max, op1=ALU.min), r=["t2"], w=["t2"])
                P.dve(I("tensor_tensor", out=t1, in0=t1, in1=t2, op=ALU.add), r=["t1", "t2"], w=["t1"])
            P.dve(I("scalar_tensor_tensor", out=th, in0=t1, scalar=-2.0 * math.pi, in1=th, op0=ALU.mult, op1=ALU.add), r=["t1", "th"], w=["th"])
            P.act(I("activation", out=sth, in_=th, func=AF.Sin), r=["th"], w=["sth"])
            P.dve(I("tensor_scalar", out=t2, in0=th, scalar1=-1.0, scalar2=None, op0=ALU.mult), r=["th"], w=["t2"])
            P.dve(I("tensor_tensor", out=t2, in0=t2, in1=th, op=ALU.max), r=["t2", "th"], w=["t2"])
            P.dve(I("tensor_scalar", out=t2, in0=t2, scalar1=-1.0, scalar2=math.pi / 2, op0=ALU.mult, op1=ALU.add), r=["t2"], w=["t2"])
            P.act(I("activation", out=cth, in_=t2, func=AF.Sin), r=["t2"], w=["cth"])
            P.dve(I("tensor_tensor", out=Are, in0=rho, in1=cth, op=ALU.mult), r=["rho", "cth"], w=["Are"])
            P.dve(I("tensor_tensor", out=Aim, in0=rho, in1=sth, op=ALU.mult), r=["rho", "sth"], w=["Aim"])
            P.dve(I("tensor_scalar", out=t0, in0=Are, scalar1=-1.0, scalar2=None, op0=ALU.add), r=["Are"], w=["t0"])
            P.dve(I("tensor_tensor", out=d2, in0=are, in1=are, op=ALU.mult), r=["are"], w=["d2"])
            P.dve(I("tensor_tensor", out=t1, in0=aim, in1=aim, op=ALU.mult), r=["aim"], w=["t1"])
            P.dve(I("tensor_tensor", out=d2, in0=d2, in1=t1, op=ALU.add), r=["d2", "t1"], w=["d2"])
            P.dve(I("reciprocal", out=d2, in_=d2), r=["d2"], w=["d2"])
            P.dve(I("tensor_tensor", out=t1, in0=t0, in1=are, op=ALU.mult), r=["t0", "are"], w=["t1"])
            P.dve(I("tensor_tensor", out=t2, in0=Aim, in1=aim, op=ALU.mult), r=["Aim", "aim"], w=["t2"])
            P.dve(I("tensor_tensor", out=t1, in0=t1, in1=t2, op=ALU.add), r=["t1", "t2"], w=["t1"])
            P.dve(I("tensor_tensor", out=Fre, in0=t1, in1=d2, op=ALU.mult), r=["t1", "d2"], w=["Fre"])
            P.dve(I("tensor_tensor", out=t1, in0=Aim, in1=are, op=ALU.mult), r=["Aim", "are"], w=["t1"])
            P.dve(I("tensor_tensor", out=t2, in0=t0, in1=aim, op=ALU.mult), r=["t0", "aim"], w=["t2"])
            P.dve(I("tensor_tensor", out=t1, in0=t1, in1=t2, op=ALU.subtract), r=["t1", "t2"], w=["t1"])
            P.dve(I("tensor_tensor", out=Fim, in0=t1, in1=d2, op=ALU.mult), r=["t1", "d2"], w=["Fim"])
            Bre = AR.alloc([128, 16, 16]); Bim = AR.alloc([128, 16, 16]); Bbr = AR.alloc([128, 16, 16]); Bbi = AR.alloc([128, 16, 16]); Bt = AR.alloc([128, 16, 16])
            P.dma(I("dma_start", out=Bre, in_=b_re.rearrange("(gp g2) p c -> (g2 p) gp c", g2=2)), w=["Bre"])
            P.dma(I("dma_start", out=Bim, in_=b_im.rearrange("(gp g2) p c -> (g2 p) gp c", g2=2)), w=["Bim"])
            bc = lambda a: a.unsqueeze(2).to_broadcast([128, 16, 16])
            P.dve(I("tensor_tensor", out=Bbr, in0=Bre, in1=bc(Fre), op=ALU.mult), r=["Bre", "Fre"], w=["Bbr"])
            P.dve(I("tensor_tensor", out=Bt, in0=Bim, in1=bc(Fim), op=ALU.mult), r=["Bim", "Fim"], w=["Bt"])
            P.dve(I("tensor_tensor", out=Bbr, in0=Bbr, in1=Bt, op=ALU.subtract), r=["Bbr", "Bt"], w=["Bbr"])
            P.dve(I("tensor_tensor", out=Bbi, in0=Bim, in1=bc(Fre), op=ALU.mult), r=["Bim", "Fre"], w=["Bbi"])
            P.dve(I("tensor_tensor", out=Bt, in0=Bre, in1=bc(Fim), op=ALU.mult), r=["Bre", "Fim"], w=["Bt"])
            P.dve(I("tensor_tensor", out=Bbi, in0=Bbi, in1=Bt, op=ALU.add), r=["Bbi", "Bt"], w=["Bbi"])
            Bp = [AR.alloc([128, 16, 128], BF16), AR.alloc([128, 16, 128], BF16)]
            m1 = AR.mark()
            Lx = AR.alloc([128, 16, 128])
            for ri, Bb in enumerate((Bbr, Bbi)):
                P.dve(I("memset", Lx, 0.0), w=["Lx"])
                Lv = Lx.rearrange("q gp (l g c) -> q gp l g c", l=4, g=2)
                for g2 in range(2):
                    P.dve(I("tensor_copy",
                        out=Lv[64 * g2:64 * g2 + 64, :, :, g2, :],
                        in_=Bb[64 * g2:64 * g2 + 64].unsqueeze(2).to_broadcast([64, 16, 4, 16])), r=["Bbr", "Bbi"], w=["Lx"])
                for g4 in range(4):
                    pb = bank(g4 % 2).rearrange("p (a b) -> p a b", a=4)
                    for j in range(4):
                        gp = g4 * 4 + j
                        P.pe(I("transpose", out=pb[:, j, :], in_=Lx[:, gp, :], identity=ident), r=["Lx", "ident"], w=["pb%d" % (g4 % 2)])
                    P.dve(I("tensor_copy", out=Bp[ri][:, g4 * 4:g4 * 4 + 4, :], in_=pb), r=["pb%d" % (g4 % 2)], w=["Bp"])
            AR.reset(m1)
            Cr = AR.alloc([128, 16, 32]); Cni = AR.alloc([128, 16, 32])
            m1 = AR.mark()
            Cx = AR.alloc([128, 16, 128])
            for ri, csrc in enumerate((c_re, c_im)):
                P.dve(I("memset", Cx[0:32], 0.0), w=["Cx"])
                cv = csrc.rearrange("(gp g2) c p -> g2 c gp p", g2=2)
                for g2 in range(2):
                    P.dma(I("dma_start", out=Cx[16 * g2:16 * g2 + 16, :, 64 * g2:64 * g2 + 64], in_=cv[g2]), r=["Cx"], w=["Cx"])
                pb = bank(2).rearrange("p (a b) -> p a b", a=16)
                for gp in range(16):
                    P.pe(I("transpose", out=pb[:, gp, :], in_=Cx[0:32, gp, :], identity=ident[0:32, 0:32]), r=["Cx", "ident"], w=["pbC"])
                if ri == 0:
                    P.dve(I("tensor_copy", out=Cr, in_=pb), r=["pbC"], w=["Cr"])
                else:
                    P.dve(I("tensor_scalar", out=Cni, in0=pb, scalar1=-1.0, scalar2=None, op0=ALU.mult), r=["pbC"], w=["Cni"])
            AR.reset(m1)
            cn = AR.alloc([128, 16, 128]); sn = AR.alloc([128, 16, 128]); tA = AR.alloc([128, 16, 64]); tB = AR.alloc([128, 16, 64])
            P.dve(I("tensor_copy", out=cn[:, :, 0:1], in_=cth.unsqueeze(2)), r=["cth"], w=["cn"])
            P.dve(I("tensor_copy", out=sn[:, :, 0:1], in_=sth.unsqueeze(2)), r=["sth"], w=["sn"])
            m = 1
            while m < 128:
                cm = cn[:, :, m - 1:m].to_broadcast([128, 16, m]); smm = sn[:, :, m - 1:m].to_broadcast([128, 16, m])
                ta = tA[:, :, 0:m]; tb = tB[:, :, 0:m]
                P.dve(I("tensor_tensor", out=ta, in0=cn[:, :, 0:m], in1=cm, op=ALU.mult), r=["cn"], w=["tA"])
                P.dve(I("tensor_tensor", out=tb, in0=sn[:, :, 0:m], in1=smm, op=ALU.mult), r=["sn"], w=["tB"])
                P.dve(I("tensor_tensor", out=ta, in0=ta, in1=tb, op=ALU.subtract), r=["tA", "tB"], w=["tA"])
                P.dve(I("tensor_tensor", out=tb, in0=cn[:, :, 0:m], in1=smm, op=ALU.mult), r=["cn", "sn"], w=["tB"])
                P.dve(I("tensor_copy", out=cn[:, :, m:2 * m], in_=ta), r=["tA"], w=["cn"])
                P.dve(I("tensor_tensor", out=ta, in0=sn[:, :, 0:m], in1=cm, op=ALU.mult), r=["sn", "cn"], w=["tA"])
                P.dve(I("tensor_tensor", out=sn[:, :, m:2 * m], in0=ta, in1=tb, op=ALU.add), r=["tA", "tB"], w=["sn"])
                m *= 2
            dcol = AR.alloc([128, 4]); gncol = AR.alloc([128, 4])
            P.dma(I("dma_start", out=dcol, in_=ssm_d.rearrange("(ut r) -> r ut", r=128)), w=["dcol"])
            P.dma(I("dma_start", out=gncol, in_=ssm_norm_g.rearrange("(ut r) -> r ut", r=128)), w=["gncol"])
            Sre = AR.alloc([128, 16]); Sim = AR.alloc([128, 16])
            P.dve(I("memset", Sre, 0.0), w=["Sre"]); P.dve(I("memset", Sim, 0.0), w=["Sim"])
            xin = [AR.alloc([128, 1024]) for _ in range(2)]
            xT = [AR.alloc([128, 8, 512], BF16) for _ in range(2)]
            kst = AR.alloc([128, 4, 512], BF16); qst = AR.alloc([128, 4, 512], BF16)
            vst = [AR.alloc([128, 8, 65], BF16) for _ in range(2)]
            for b_ in range(2):
                P.dve(I("memset", vst[b_], 1.0), w=["vst%d" % b_])
            uT = AR.alloc([128, 4, 512], BF16)
            vre = AR.alloc([128, 4, 128]); vim = AR.alloc([128, 4, 128]); e1 = AR.alloc([128, 4, 128]); e2 = AR.alloc([128, 4, 128])
            wre = AR.alloc([128, 4, 128]); wim = AR.alloc([128, 4, 128]); sre = AR.alloc([128, 4, 128]); sim = AR.alloc([128, 4, 128])
            yd = AR.alloc([128, 512]); g1 = AR.alloc([128, 512]); g2t = AR.alloc([128, 512])
            glT = AR.alloc([128, 4, 512], BF16)
            y2 = AR.alloc([128, 4, 512]); sq = AR.alloc([128, 512]); sig = AR.alloc([128, 512]); rstd = AR.alloc([128, 512])
            soT = AR.alloc([128, 4, 512], BF16)
            pbu_re = bank(2).rearrange("p (a b) -> p a b", a=4); pbu_im = bank(3).rearrange("p (a b) -> p a b", a=4)
            for s in range(16):
                own = s >= 8
                xt = xT[s % 2]; xk = "xT%d" % (s % 2)
                for ti in range(4):
                    xb = xin[ti % 2]; xbk = "xin%d" % (ti % 2)
                    r0 = s * 512 + ti * 128
                    P.dma(I("dma_start", out=xb, in_=xc[r0:r0 + 128, :]), w=[xbk])
                    for half in range(2):
                        pb = bank(half).rearrange("p (a b) -> p a b", a=4)
                        for j in range(4):
                            kt = half * 4 + j
                            P.pe(I("transpose", out=pb[:, j, :], in_=xb[:, kt * 128:(kt + 1) * 128], identity=ident), r=[xbk, "ident"], w=["pb%d" % half])
                        if half == 0:
                            P.dve(I("tensor_copy", out=xt[:, 0:4, ti * 128:(ti + 1) * 128], in_=pb), r=["pb0"], w=[xk])
                        else:
                            P.act(I("activation", out=xt[:, 4:8, ti * 128:(ti + 1) * 128], in_=pb, func=AF.Copy), r=["pb1"], w=[xk])
                def proj_fm(col0, dst, dk, n_m=4):
                    for mt in range(n_m):
                        pb = bank(mt % 2); pk = "pb%d" % (mt % 2)
                        for kt in range(8):
                            P.pe(I("matmul", pb, lhsT=win[:, kt, col0 + mt * 128:col0 + (mt + 1) * 128], rhs=xt[:, kt, :], start=(kt == 0), stop=(kt == 7)), r=["win", xk], w=[pk])
                        if mt % 2 == 0:
                            P.dve(I("tensor_copy", out=dst[:, mt, :], in_=pb), r=[pk], w=[dk])
                        else:
                            P.act(I("activation", out=dst[:, mt, :], in_=pb, func=AF.Copy), r=[pk], w=[dk])
                proj_fm(512, kst, "kst")
                P.dma(I("dma_start", out=KT[:, :, s * 512:(s + 1) * 512].rearrange("h p t -> p h t"), in_=kst), r=["kst"], w=["KT"])
                if own:
                    proj_fm(0, qst, "qst")
                    P.dma(I("dma_start", out=QT[:, :, (s - 8) * 512:(s - 7) * 512].rearrange("h p t -> p h t"), in_=qst), r=["qst"], w=["QT"])
                for ti in range(4):
                    pb = bank(ti % 2); pk = "pb%d" % (ti % 2)
                    vb = vst[ti % 2]; vk = "vst%d" % (ti % 2)
                    for kt in range(8):
                        P.pe(I("matmul", pb, lhsT=xt[:, kt, ti * 128:(ti + 1) * 128], rhs=win[:, kt, 1024:1536], start=(kt == 0), stop=(kt == 7)), r=["win", xk], w=[pk])
                    P.dve(I("tensor_copy", out=vb[:, :, 0:64], in_=pb.rearrange("p (h d) -> p h d", h=8)), r=[pk], w=[vk])
                    P.dma(I("dma_start", out=VV[s * 4 + ti], in_=vb.rearrange("p h d -> p (h d)")), r=[vk], w=["VV"])
                proj_fm(1536, uT, "uT")
                for ut in range(4):
                    py = bank(4 + ut % 2); pyk = "py%d" % (ut % 2)
                    for un in range(4):
                        tsl = slice(un * 128, (un + 1) * 128)
                        for gl in range(4):
                            gp = ut * 4 + gl
                            P.pe(I("matmul", pbu_re[:, gl, :], lhsT=Bp[0][32 * gl:32 * gl + 32, gp, :], rhs=uT[32 * gl:32 * gl + 32, ut, tsl], start=True, stop=True, tile_position=(32 * gl, 0)), r=["Bp", "uT"], w=["pbu_re"])
                            P.pe(I("matmul", pbu_im[:, gl, :], lhsT=Bp[1][32 * gl:32 * gl + 32, gp, :], rhs=uT[32 * gl:32 * gl + 32, ut, tsl], start=True, stop=True, tile_position=(32 * gl, 0)), r=["Bp", "uT"], w=["pbu_im"])
                        g4 = slice(ut * 4, ut * 4 + 4)
                        cnv = cn[:, g4, :]; snv = sn[:, g4, :]
                        P.dve(I("tensor_tensor", out=vre, in0=pbu_re, in1=cnv, op=ALU.mult), r=["pbu_re", "cn"], w=["vre"])
                        P.dve(I("tensor_tensor", out=e1, in0=pbu_im, in1=snv, op=ALU.mult), r=["pbu_im", "sn"], w=["e1"])
                        P.dve(I("tensor_tensor", out=vim, in0=pbu_im, in1=cnv, op=ALU.mult), r=["pbu_im", "cn"], w=["vim"])
                        P.dve(I("tensor_tensor", out=e2, in0=pbu_re, in1=snv, op=ALU.mult), r=["pbu_re", "sn"], w=["e2"])
                        P.pool(I("tensor_tensor", out=vre, in0=vre, in1=e1, op=ALU.add), r=["vre", "e1"], w=["vre"])
                        P.pool(I("tensor_tensor", out=vim, in0=vim, in1=e2, op=ALU.subtract), r=["vim", "e2"], w=["vim"])
                        for gl in range(4):
                            gp = ut * 4 + gl
                            P.dve(I("tensor_tensor_scan", out=wre[:, gl, :], data0=rho[:, gp:gp + 1].to_broadcast([128, 128]), data1=vre[:, gl, :], initial=Sre[:, gp:gp + 1], op0=ALU.mult, op1=ALU.add), r=["vre", "rho", "Sre"], w=["wre"])
                            P.dve(I("tensor_tensor_scan", out=wim[:, gl, :], data0=rho[:, gp:gp + 1].to_broadcast([128, 128]), data1=vim[:, gl, :], initial=Sim[:, gp:gp + 1], op0=ALU.mult, op1=ALU.add), r=["vim", "rho", "Sim"], w=["wim"])
                        if own:
                            cs, ws = slice(0, 128), slice(0, 4)
                        else:
                            cs, ws = slice(127, 128), slice(0, 4)
                        P.pool(I("tensor_tensor", out=sre[:, :, cs], in0=wre[:, :, cs], in1=cnv[:, :, cs], op=ALU.mult), r=["wre", "cn"], w=["sre"])
                        P.pool(I("tensor_tensor", out=e1[:, :, cs], in0=wim[:, :, cs], in1=snv[:, :, cs], op=ALU.mult), r=["wim", "sn"], w=["e1"])
                        P.pool(I("tensor_tensor", out=sre[:, :, cs], in0=sre[:, :, cs], in1=e1[:, :, cs], op=ALU.subtract), r=["sre", "e1"], w=["sre"])
                        P.pool(I("tensor_tensor", out=sim[:, :, cs], in0=wim[:, :, cs], in1=cnv[:, :, cs], op=ALU.mult), r=["wim", "cn"], w=["sim"])
                        P.pool(I("tensor_tensor", out=e2[:, :, cs], in0=wre[:, :, cs], in1=snv[:, :, cs], op=ALU.mult), r=["wre", "sn"], w=["e2"])
                        P.pool(I("tensor_tensor", out=sim[:, :, cs], in0=sim[:, :, cs], in1=e2[:, :, cs], op=ALU.add), r=["sim", "e2"], w=["sim"])
                        P.dve(I("tensor_copy", out=Sre[:, g4], in_=sre[:, :, 127]), r=["sre"], w=["Sre"])
                        P.dve(I("tensor_copy", out=Sim[:, g4], in_=sim[:, :, 127]), r=["sim"], w=["Sim"])
                        if own:
                            for gl in range(4):
                                gp = ut * 4 + gl
                                P.pe(I("matmul", py[32 * gl:32 * gl + 32, tsl], lhsT=Cr[:, gp, :], rhs=sre[:, gl, :], start=True, stop=False, tile_position=(0, 32 * gl)), r=["Cr", "sre"], w=[pyk])
                                P.pe(I("matmul", py[32 * gl:32 * gl + 32, tsl], lhsT=Cni[:, gp, :], rhs=sim[:, gl, :], start=False, stop=True, tile_position=(0, 32 * gl)), r=["Cni", "sim"], w=[pyk])
                    if own:
                        P.dve(I("scalar_tensor_tensor", out=yd, in0=uT[:, ut, :], scalar=dcol[:, ut:ut + 1], in1=py, op0=ALU.mult, op1=ALU.add), r=["uT", "dcol", pyk], w=["yd"])
                        P.pool(I("tensor_tensor", out=g1, in0=yd, in1=yd, op=ALU.mult), r=["yd"], w=["g1"])
                        P.pool(I("tensor_scalar", out=g1, in0=g1, scalar1=0.044715, scalar2=1.0, op0=ALU.mult, op1=ALU.add), r=["g1"], w=["g1"])
                        P.pool(I("tensor_tensor", out=g1, in0=g1, in1=yd, op=ALU.mult), r=["g1", "yd"], w=["g1"])
                        P.act(I("activation", out=g2t, in_=g1, func=AF.Sigmoid, scale=2.0 * math.sqrt(2.0 / math.pi)), r=["g1"], w=["g2t"])
                        P.pool(I("tensor_tensor", out=glT[:, ut, :], in0=yd, in1=g2t, op=ALU.mult), r=["yd", "g2t"], w=["glT"])
                if own:
                    for mo in range(4):
                        plo = bank(0); phi = bank(1)
                        for kt in range(4):
                            P.pe(I("matmul", plo, lhsT=wglu[:, kt, mo * 128:(mo + 1) * 128], rhs=glT[:, kt, :], start=(kt == 0), stop=(kt == 3)), r=["wglu", "glT"], w=["pb0"])
                        for kt in range(4):
                            P.pe(I("matmul", phi, lhsT=wglu[:, kt, 512 + mo * 128:512 + (mo + 1) * 128], rhs=glT[:, kt, :], start=(kt == 0), stop=(kt == 3)), r=["wglu", "glT"], w=["pb1"])
                        P.act(I("activation", out=sig, in_=phi, func=AF.Sigmoid), r=["pb1"], w=["sig"])
                        P.dve(I("tensor_tensor", out=y2[:, mo, :], in0=plo, in1=sig, op=ALU.mult), r=["pb0", "sig"], w=["y2"])
                    pms = bank(6)
                    for mo in range(4):
                        P.pool(I("tensor_tensor", out=sq, in0=y2[:, mo, :], in1=y2[:, mo, :], op=ALU.mult), r=["y2"], w=["sq"])
                        P.pe(I("matmul", pms, lhsT=ones, rhs=sq, start=(mo == 0), stop=(mo == 3)), r=["ones", "sq"], w=["pms"])
                    P.act(I("activation", out=rstd, in_=pms, func=AF.Ln, scale=1.0 / 512.0, bias=1e-5), r=["pms"], w=["rstd"])
                    P.act(I("activation", out=rstd, in_=rstd, func=AF.Exp, scale=-0.5), r=["rstd"], w=["rstd"])
                    for mo in range(4):
                        P.dve(I("scalar_tensor_tensor", out=soT[:, mo, :], in0=y2[:, mo, :], scalar=gncol[:, mo:mo + 1], in1=rstd, op0=ALU.mult, op1=ALU.mult), r=["y2", "gncol", "rstd"], w=["soT"])
                    P.dma(I("dma_start", out=MIXT[4:8, :, (s - 8) * 512:(s - 7) * 512].rearrange("h p t -> p h t"), in_=soT), r=["soT"], w=["MIXT"])
            P.barrier()
            AR.reset(gmark)


        P.pe_sync = False
        G = AR.alloc([128, 32, 32])
        gmark = AR.mark()
        SC = 1.0 / math.sqrt(32.0)

        def layer_norm_tile(pre, gb, bb, dst, tg):
            stt = AR_t["st"]; mv = AR_t["mv"]; rs = AR_t["rs"]
            P.dve(I("bn_stats", out=stt[:, 0:6], in_=pre[:, 0:512]), r=[tg], w=["lnst"])
            P.dve(I("bn_stats", out=stt[:, 6:12], in_=pre[:, 512:1024]), r=[tg], w=["lnst2"])
            P.dve(I("bn_aggr", out=mv, in_=stt), r=["lnst", "lnst2"], w=["lnmv"])
            P.act(I("activation", out=rs, in_=mv[:, 1:2], func=AF.Ln, bias=1e-5), r=["lnmv"], w=["lnrs"])
            P.act(I("activation", out=rs, in_=rs, func=AF.Exp, scale=-0.5), r=["lnrs"], w=["lnrs"])
            P.dve(I("tensor_scalar", out=dst, in0=pre, scalar1=mv[:, 0:1], scalar2=rs[:, 0:1], op0=ALU.subtract, op1=ALU.mult), r=[tg, "lnmv", "lnrs"], w=[tg + "o"])
            P.pool(I("tensor_tensor", out=dst, in0=dst, in1=gb, op=ALU.mult), r=[tg + "o", "lng"], w=[tg + "o"])
            P.pool(I("tensor_tensor", out=dst, in0=dst, in1=bb, op=ALU.add), r=[tg + "o", "lng"], w=[tg + "o"])
        AR_t = {}

        if "2" in phases:
            slopes = [2.0 ** (-(h + 1)) for h in range(8)]
            tri = AR.alloc([128, 128]); trib = AR.alloc([128, 128], BF16); U = AR.alloc([128, 128]); kl = AR.alloc([128, 1])
            P.pool(I("memset", tri, 1.0), w=["tri"])
            P.pool(I("affine_select", out=tri, in_=tri, pattern=[[1, 128]], compare_op=ALU.is_ge, fill=0.0, base=0, channel_multiplier=-1), r=["tri"], w=["tri"])
            P.dve(I("tensor_copy", out=trib, in_=tri), r=["tri"], w=["trib"])
            P.pool(I("memset", U, 1.0), w=["U"])
            P.pool(I("affine_select", out=U, in_=U, pattern=[[1, 128]], compare_op=ALU.is_gt, fill=0.0, base=0, channel_multiplier=-1), r=["U"], w=["U"])
            P.pe(I("matmul", bank(0)[:, 0:1], lhsT=U, rhs=ones[:, 0:1], start=True, stop=True), r=["U", "ones"], w=["pb0"])
            P.dve(I("tensor_copy", out=kl, in_=bank(0)[:, 0:1]), r=["pb0"], w=["kl"])
            ND = 68
            bt = AR.alloc([128, 8, ND]); btp = AR.alloc([128, 8, ND]); prefc = AR.alloc([128, 1])
            P.dma(I("dma_start", out=prefc, in_=pref), w=["prefc"])
            for h in range(8):
                for Dd in range(ND):
                    fn = P.dve if (Dd % 2 == 0) else P.pool
                    fn(I("tensor_scalar", out=bt[:, h, Dd:Dd + 1], in0=kl, scalar1=slopes[h], scalar2=-slopes[h] * 128.0 * Dd, op0=ALU.mult, op1=ALU.add), r=["kl"], w=["bt%d" % (Dd % 2)])
            P.dve(I("tensor_scalar", out=btp, in0=bt, scalar1=prefc[:, 0:1], scalar2=None, op0=ALU.add), r=["bt0", "bt1", "prefc"], w=["btp"])
            lv = [AR.alloc([64, 32]) for _ in range(4)]; lt = AR.alloc([64, 32]); l1 = AR.alloc([64, 1]); l2 = AR.alloc([64, 1]); neglam = AR.alloc([64, 1]); gcol = AR.alloc([64, 1])
            for i_, src in enumerate((lq1, lk1, lq2, lk2)):
                P.dma(I("dma_start", out=lv[i_], in_=src.to_broadcast([64, 32])), w=["lv%d" % i_])
            P.dve(I("tensor_tensor", out=lt, in0=lv[0], in1=lv[1], op=ALU.mult), r=["lv0", "lv1"], w=["lt"])
            P.dve(I("reduce_sum", out=l1, in_=lt, axis=AX.X), r=["lt"], w=["l1"])
            P.dve(I("tensor_tensor", out=lt, in0=lv[2], in1=lv[3], op=ALU.mult), r=["lv2", "lv3", "l1"], w=["lt"])
            P.dve(I("reduce_sum", out=l2, in_=lt, axis=AX.X), r=["lt"], w=["l2"])
            P.act(I("activation", out=l1, in_=l1, func=AF.Exp), r=["l1"], w=["l1"])
            P.act(I("activation", out=l2, in_=l2, func=AF.Exp), r=["l2"], w=["l2"])
            P.dve(I("tensor_tensor", out=neglam, in0=l2, in1=l1, op=ALU.subtract), r=["l1", "l2"], w=["neglam"])
            P.dve(I("tensor_scalar", out=neglam, in0=neglam, scalar1=-LAMBDA_INIT, scalar2=None, op0=ALU.add), r=["neglam"], w=["neglam"])
            P.dma(I("dma_start", out=gcol, in_=subln_g), w=["gcol"])
            P.dve(I("tensor_scalar", out=gcol, in0=gcol, scalar1=1.0 - LAMBDA_INIT, scalar2=None, op0=ALU.mult), r=["gcol"], w=["gcol"])
            sel = AR.alloc([65, 64])
            P.dve(I("memset", sel, 0.0), w=["sel"])
            P.dve(I("memset", sel[64:65, :], 1.0), r=["sel"], w=["sel"])
            KTh = AR.alloc([128, L], BF16); QTh = AR.alloc([128, LO], BF16); Vh = AR.alloc([128, 64, 130], BF16)
            Pt = [AR.alloc([128, 2, 512], BF16) for _ in range(2)]
            Osb = AR.alloc([65, 2, 512]); Od = AR.alloc([65, 2, 512]); rL = AR.alloc([64, 2, 512])
            a1 = AR.alloc([64, 512]); a2 = AR.alloc([64, 512]); dif = AR.alloc([64, 512]); sq2 = AR.alloc([64, 512]); rs2 = AR.alloc([64, 512])
            oT = AR.alloc([64, 512], BF16)
            scb = [bank(0, 2).rearrange("p (a b) -> p a b", a=2), bank(2, 2).rearrange("p (a b) -> p a b", a=2)]
            Ooff = bank(4, 2).rearrange("p (a b) -> p a b", a=2); Odg = bank(6, 2).rearrange("p (a b) -> p a b", a=2)
            it = 0
            for hp in range(4):
                P.dma(I("dma_start", out=KTh, in_=KT[hp]), r=["KT"], w=["KTh"])
                P.dma(I("dma_start", out=QTh, in_=QT[hp]), r=["QT"], w=["QTh"])
                P.dma(I("dma_start", out=Vh, in_=VV[:, :, hp * 130:(hp + 1) * 130].rearrange("b p c -> p b c")), r=["VV"], w=["Vh"])
                for j in range(8):
                    for hl in range(2):
                        h = 2 * hp + hl; sl = slopes[h]
                        q0 = LO + 512 * j; kd0 = q0 // 128; klo = _qkey_lo(q0, sl)
                        offs = list(range(klo, kd0))
                        for ii, kb in enumerate(offs):
                            sc = scb[it % 2]; sk = "sc%d" % (it % 2); pt = Pt[it % 2]; pk = "Pt%d" % (it % 2); it += 1
                            for m_ in range(2):
                                pr = 32 * (2 * hl + m_)
                                P.pe(I("matmul", sc[:, m_, :], lhsT=KTh[pr:pr + 32, kb * 128:(kb + 1) * 128], rhs=QTh[pr:pr + 32, j * 512:(j + 1) * 512], start=True, stop=True, tile_position=(pr, 0)), r=["KTh", "QTh"], w=[sk])
                            btab = btp if kb < 32 else bt
                            P.act(I("activation", out=pt, in_=sc, func=AF.Exp, bias=btab[:, h, kd0 - kb:kd0 - kb + 1], scale=SC), r=[sk, "btp", "bt0", "bt1"], w=[pk])
                            for m_ in range(2):
                                P.pe(I("matmul", Ooff[0:65, m_, :], lhsT=Vh[:, kb, hl * 65:(hl + 1) * 65], rhs=pt[:, m_, :], start=(ii == 0), stop=(ii == len(offs) - 1)), r=["Vh", pk], w=["Ooff"])
                        for r_ in range(4):
                            kb = kd0 + r_
                            sc = scb[it % 2]; sk = "sc%d" % (it % 2); pt = Pt[it % 2]; pk = "Pt%d" % (it % 2); it += 1
                            c0 = 128 * r_
                            for m_ in range(2):
                                pr = 32 * (2 * hl + m_)
                                P.pe(I("matmul", sc[:, m_, c0:512], lhsT=KTh[pr:pr + 32, kb * 128:(kb + 1) * 128], rhs=QTh[pr:pr + 32, j * 512 + c0:(j + 1) * 512], start=True, stop=True, tile_position=(pr, 0)), r=["KTh", "QTh"], w=[sk])
                            for qs in range(r_, 4):
                                P.act(I("activation", out=pt[:, :, 128 * qs:128 * qs + 128], in_=sc[:, :, 128 * qs:128 * qs + 128], func=AF.Exp, bias=bt[:, h, qs - r_:qs - r_ + 1], scale=SC), r=[sk, "bt0", "bt1"], w=[pk])
                            P.pool(I("tensor_tensor", out=pt[:, :, c0:c0 + 128], in0=pt[:, :, c0:c0 + 128], in1=trib.unsqueeze(1).to_broadcast([128, 2, 128]), op=ALU.mult), r=[pk, "trib"], w=[pk])
                            for m_ in range(2):
                                P.pe(I("matmul", Odg[0:65, m_, c0:512], lhsT=Vh[:, kb, hl * 65:(hl + 1) * 65], rhs=pt[:, m_, c0:512], start=(r_ == 0), stop=(r_ == 3)), r=["Vh", pk], w=["Odg"])
                        P.act(I("activation", out=Od, in_=Odg[0:65], func=AF.Copy), r=["Odg"], w=["Od"])
                        for qs in range(4):
                            f = math.exp(-sl * 128.0 * qs)
                            cs = slice(128 * qs, 128 * qs + 128)
                            P.dve(I("scalar_tensor_tensor", out=Osb[:, :, cs], in0=Ooff[0:65, :, cs], scalar=f, in1=Od[:, :, cs], op0=ALU.mult, op1=ALU.add), r=["Ooff", "Od"], w=["Osb"])
                        for m_ in range(2):
                            P.pe(I("matmul", Odg[0:64, m_, :], lhsT=sel, rhs=Osb[:, m_, :], start=True, stop=True), r=["sel", "Osb", "Od"], w=["Odg"])
                        P.dve(I("reciprocal", out=rL, in_=Odg[0:64]), r=["Odg"], w=["rL"])
                        P.pool(I("tensor_tensor", out=a1, in0=Osb[0:64, 0, :], in1=rL[:, 0, :], op=ALU.mult), r=["Osb", "rL"], w=["a1"])
                        P.pool(I("tensor_tensor", out=a2, in0=Osb[0:64, 1, :], in1=rL[:, 1, :], op=ALU.mult), r=["Osb", "rL"], w=["a2"])
                        P.dve(I("scalar_tensor_tensor", out=dif, in0=a2, scalar=neglam[:, 0:1], in1=a1, op0=ALU.mult, op1=ALU.add), r=["a1", "a2", "neglam"], w=["dif"])
                        P.pool(I("tensor_tensor", out=sq2, in0=dif, in1=dif, op=ALU.mult), r=["dif"], w=["sq2"])
                        P.pe(I("matmul", Odg[0:64, 0, :], lhsT=ones[0:64, 0:64], rhs=sq2, start=True, stop=True), r=["ones", "sq2", "rL"], w=["Odg"])
                        P.act(I("activation", out=rs2, in_=Odg[0:64, 0, :], func=AF.Ln, scale=1.0 / 64.0, bias=1e-5), r=["Odg"], w=["rs2"])
                        P.act(I("activation", out=rs2, in_=rs2, func=AF.Exp, scale=-0.5), r=["rs2"], w=["rs2"])
                        P.dve(I("scalar_tensor_tensor", out=oT, in0=dif, scalar=gcol[:, 0:1], in1=rs2, op0=ALU.mult, op1=ALU.mult), r=["dif", "gcol", "rs2"], w=["oT"])
                        P.dma(I("dma_start", out=MIXT[h // 2, (h % 2) * 64:(h % 2) * 64 + 64, j * 512:(j + 1) * 512], in_=oT), r=["oT"], w=["MIXT"])
            P.barrier()
            AR.reset(gmark)

        def bcast_row(dst, src, n, key):
            P.dma(I("dma_start", out=dst, in_=src.to_broadcast([128, n])), w=[key])

        if "3" in phases:
            if os.environ.get('KZERO'):
                zt = AR.alloc([128, 8, 512], BF16)
                P.dve(I("memset", zt, 0.0), w=["zt"])
                for cc in range(8):
                    P.dma(I("dma_start", out=MIXT[:, :, cc * 512:(cc + 1) * 512].rearrange("k p t -> p k t"), in_=zt), r=["zt"], w=["MIXT"])
            wout = AR.alloc([128, 8, 1024], BF16)
            for kt in range(8):
                P.dma(I("dma_start", out=wout[:, kt, :], in_=w_out[kt * 128:(kt + 1) * 128, :]), w=["wout"], q="gpsimd")
            wr = AR.alloc([128, 8, 32])
            P.dma(I("dma_start", out=wr, in_=w_router.rearrange("(k p) e -> p k e", p=128)), w=["wr"])
            g1b = AR.alloc([128, 1024]); b1b = AR.alloc([128, 1024]); brb = AR.alloc([128, 32])
            bcast_row(g1b, ln1_g, 1024, "lng"); bcast_row(b1b, ln1_b, 1024, "lng"); bcast_row(brb, b_router, 32, "brb")
            AR_t["st"] = AR.alloc([128, 12]); AR_t["mv"] = AR.alloc([128, 2]); AR_t["rs"] = AR.alloc([128, 1])
            mixT = [AR.alloc([128, 8, 128], BF16) for _ in range(2)]
            xt_ = [AR.alloc([128, 1024]) for _ in range(2)]
            pre = AR.alloc([128, 1024]); x1 = [AR.alloc([128, 1024]) for _ in range(2)]
            x1Tf = AR.alloc([128, 8, 128]); x1Tb = [AR.alloc([128, 8, 128], BF16) for _ in range(2)]
            lg = AR.alloc([128, 32]); mx8 = AR.alloc([128, 8]); msk = AR.alloc([128, 32]); ex = AR.alloc([128, 32]); nmx = AR.alloc([128, 1]); ssum = AR.alloc([128, 1])
            CUT = int(os.environ.get('KCUT', '99'))
            for t in range(int(os.environ.get('KNT3', '32'))):
                b_ = t % 2
                P.dma(I("dma_start", out=mixT[b_], in_=MIXT[:, :, t * 128:(t + 1) * 128].rearrange("k p t -> p k t")), r=["MIXT"], w=["mixT%d" % b_])
                P.dma(I("dma_start", out=xt_[b_], in_=xc[LO + t * 128:LO + (t + 1) * 128, :]), w=["xt%d" % b_])
                if CUT < 2: continue
                pm = bank(0, 2)
                for dh in range(2):
                    for kt in range(8):
                        P.pe(I("matmul", pm[:, dh * 512:(dh + 1) * 512], lhsT=mixT[b_][:, kt, :], rhs=wout[:, kt, dh * 512:(dh + 1) * 512], start=(kt == 0), stop=(kt == 7)), r=["mixT%d" % b_, "wout"], w=["pm"])
                P.dve(I("scalar_tensor_tensor", out=pre, in0=xt_[b_], scalar=ALPHA, in1=pm, op0=ALU.mult, op1=ALU.add), r=["xt%d" % b_, "pm"], w=["pre"])
                if CUT < 3: continue
                layer_norm_tile(pre, g1b, b1b, x1[b_], "pre")
                if CUT < 4: continue
                P.dma(I("dma_start", out=X1[t * 128:(t + 1) * 128, :], in_=x1[b_]), r=["preo"], w=["X1"])
                if CUT < 5: continue
                ptr = bank(2, 2).rearrange("p (a b) -> p a b", a=8)
                for kt in range(8):
                    P.pe(I("transpose", out=ptr[:, kt, :], in_=x1[b_][:, kt * 128:(kt + 1) * 128], identity=ident), r=["preo", "ident"], w=["ptr"])
                KS = os.environ.get('KSUB', 'abc')
                if 'a' in KS:
                    P.act(I("activation", out=x1Tf, in_=ptr, func=AF.Copy), r=["ptr"], w=["x1Tf"])
                if 'b' in KS:
                    P.act(I("activation", out=x1Tb[b_], in_=ptr, func=AF.Copy), r=["ptr"], w=["x1Tb%d" % b_])
                if 'c' in KS:
                    P.dma(I("dma_start", out=X1T[:, :, t * 128:(t + 1) * 128].rearrange("k p t -> p k t"), in_=x1Tb[b_]), r=["x1Tb%d" % b_], w=["X1T"])
                if CUT < 6: continue
                pl = bank(4)[:, 0:32]
                for kt in range(8):
                    P.pe(I("matmul", pl, lhsT=x1Tf[:, kt, :], rhs=wr[:, kt, :], start=(kt == 0), stop=(kt == 7)), r=["x1Tf", "wr"], w=["pl"])
                if CUT < 7: continue
                P.dve(I("tensor_tensor", out=lg, in0=pl, in1=brb, op=ALU.add), r=["pl", "brb"], w=["lg"])
                P.dve(I("max", out=mx8, in_=lg), r=["lg"], w=["mx8"])
                P.dve(I("tensor_scalar", out=msk, in0=lg, scalar1=mx8[:, 3:4], scalar2=1e-7, op0=ALU.subtract, op1=ALU.add), r=["lg", "mx8"], w=["msk"])
                P.dve(I("tensor_scalar", out=msk, in0=msk, scalar1=1e10, scalar2=0.0, op0=ALU.mult, op1=ALU.max), r=["msk"], w=["msk"])
                P.dve(I("tensor_scalar", out=msk, in0=msk, scalar1=1.0, scalar2=None, op0=ALU.min), r=["msk"], w=["msk"])
                P.dve(I("tensor_scalar", out=nmx, in0=mx8[:, 0:1], scalar1=-1.0, scalar2=None, op0=ALU.mult), r=["mx8"], w=["nmx"])
                P.act(I("activation", out=ex, in_=lg, func=AF.Exp, bias=nmx[:, 0:1]), r=["lg", "nmx"], w=["ex"])
                P.dve(I("tensor_tensor", out=ex, in0=ex, in1=msk, op=ALU.mult), r=["ex", "msk"], w=["ex"])
                P.dve(I("reduce_sum", out=ssum, in_=ex, axis=AX.X), r=["ex"], w=["ssum"])
                P.dve(I("reciprocal", out=ssum, in_=ssum), r=["ssum"], w=["ssum"])
                P.dve(I("tensor_scalar", out=G[:, t, :], in0=ex, scalar1=ssum[:, 0:1], scalar2=None, op0=ALU.mult), r=["ex", "ssum"], w=["G"])
            P.barrier()
            AR.reset(gmark)

        if "4" in phases:
            Wgu = [AR.alloc([128, 8, 2, 1024], BF16) for _ in range(2)]
            Wd = [AR.alloc([128, 8, 1024], BF16) for _ in range(2)]
            stage = [AR.alloc([128, 2048]) for _ in range(2)]
            X1Tc = AR.alloc([128, 8, 1024], BF16); acc = AR.alloc([128, 8, 1024]); actT = AR.alloc([128, 8, 512], BF16)
            tgs = [AR.alloc([128, 512]) for _ in range(2)]; tsgs = [AR.alloc([128, 512]) for _ in range(2)]; tls = [AR.alloc([128, 512]) for _ in range(2)]
            BGU = AR.alloc([128, 8, 2, 32]); bd = AR.alloc([32, 1024]); GTc = AR.alloc([32, 8, 128])
            bgn = stage[0][0:32, :]
            P.dma(I("dma_start", out=bgn, in_=b_gu), w=["stage0"])
            bgv = bgn.rearrange("e (ft p two) -> e ft two p", p=128, two=2)
            pbg = bank(7).rearrange("p (a b c) -> p a b c", a=8, b=2)
            for ft in range(8):
                for two in range(2):
                    P.pe(I("transpose", out=pbg[:, ft, two, :], in_=bgv[:, ft, two, :], identity=ident[0:32, 0:32]), r=["stage0", "ident"], w=["B7"])
            P.dve(I("tensor_copy", out=BGU, in_=pbg), r=["B7"], w=["BGU"])
            P.dma(I("dma_start", out=bd, in_=b_dn), w=["bd"])
            stn = [0]

            def load_expert(e):
                b_ = e % 2
                for kt in range(8):
                    sb_ = stn[0] % 2; stn[0] += 1
                    P.dma(I("dma_start", out=stage[sb_], in_=w_gu[e, kt * 128:(kt + 1) * 128, :]), w=["stage%d" % sb_])
                    src = stage[sb_].rearrange("p (f two) -> p two f", two=2)
                    if kt % 2 == 0:
                        P.act(I("activation", out=Wgu[b_][:, kt, :, :], in_=src, func=AF.Copy), r=["stage%d" % sb_], w=["Wgu%d" % b_])
                    else:
                        P.pool(I("tensor_copy", out=Wgu[b_][:, kt, :, :], in_=src), r=["stage%d" % sb_], w=["Wgu%d" % b_])
                for k2 in range(4):
                    sb_ = stn[0] % 2; stn[0] += 1
                    P.dma(I("dma_start", out=stage[sb_].rearrange("p (k f) -> p k f", k=2), in_=w_dn[e, k2 * 256:(k2 + 1) * 256, :].rearrange("(k p) f -> p k f", p=128)), w=["stage%d" % sb_])
                    src = stage[sb_].rearrange("p (k f) -> p k f", k=2)
                    if k2 % 2 == 0:
                        P.act(I("activation", out=Wd[b_][:, 2 * k2:2 * k2 + 2, :], in_=src, func=AF.Copy), r=["stage%d" % sb_], w=["Wd%d" % b_])
                    else:
                        P.pool(I("tensor_copy", out=Wd[b_][:, 2 * k2:2 * k2 + 2, :], in_=src), r=["stage%d" % sb_], w=["Wd%d" % b_])

            for c in range(NCH):
                P.dma(I("dma_start", out=X1Tc, in_=X1T[:, :, c * 1024:(c + 1) * 1024].rearrange("k p t -> p k t")), r=["X1T"], w=["X1Tc"])
                pg = bank(6)[0:32, :].rearrange("p (a b) -> p a b", a=4)
                for half in range(2):
                    for i_ in range(4):
                        tl_ = half * 4 + i_
                        P.pe(I("transpose", out=pg[:, i_, :], in_=G[:, c * 8 + tl_, :], identity=ident), r=["G", "ident"], w=["pg"])
                    P.dve(I("tensor_copy", out=GTc[:, half * 4:half * 4 + 4, :], in_=pg), r=["pg"], w=["GTc"])
                for tl_ in range(8):
                    pa = bank(0, 2)
                    for dh in range(2):
                        P.pe(I("matmul", pa[:, dh * 512:(dh + 1) * 512], lhsT=GTc[:, tl_, :], rhs=bd[:, dh * 512:(dh + 1) * 512], start=True, stop=True), r=["GTc", "bd"], w=["pa"])
                    P.dve(I("tensor_copy", out=acc[:, tl_, :], in_=pa), r=["pa"], w=["acc"])
                load_expert(0)
                for e in range(NE):
                    if e + 1 < NE:
                        load_expert(e + 1)
                    b_ = e % 2
                    for tc in range(2):
                        for ft in range(8):
                            pgl = bank((ft % 2) * 2); pln = bank((ft % 2) * 2 + 1); gk = "pgl%d" % (ft % 2)
                            tg = tgs[ft % 2]; tsg = tsgs[ft % 2]; tl = tls[ft % 2]; kg = "tg%d" % (ft % 2); ksg = "tsg%d" % (ft % 2); kl_ = "tl%d" % (ft % 2)
                            for kt in range(8):
                                P.pe(I("matmul", pgl, lhsT=Wgu[b_][:, kt, 0, ft * 128:(ft + 1) * 128], rhs=X1Tc[:, kt, tc * 512:(tc + 1) * 512], start=(kt == 0), stop=(kt == 7)), r=["Wgu%d" % b_, "X1Tc"], w=[gk])
                            for kt in range(8):
                                P.pe(I("matmul", pln, lhsT=Wgu[b_][:, kt, 1, ft * 128:(ft + 1) * 128], rhs=X1Tc[:, kt, tc * 512:(tc + 1) * 512], start=(kt == 0), stop=(kt == 7)), r=["Wgu%d" % b_, "X1Tc"], w=[gk + "l"])
                            P.dve(I("tensor_scalar", out=tg, in0=pgl, scalar1=BGU[:, ft, 0, e:e + 1], scalar2=7.0, op0=ALU.add, op1=ALU.min), r=[gk, "BGU"], w=[kg])
                            P.act(I("activation", out=tsg, in_=tg, func=AF.Sigmoid, scale=1.702), r=[kg], w=[ksg])
                            P.dve(I("tensor_scalar", out=tl, in0=pln, scalar1=BGU[:, ft, 1, e:e + 1], scalar2=7.0, op0=ALU.add, op1=ALU.min), r=[gk + "l", "BGU"], w=[kl_])
                            P.pool(I("tensor_scalar", out=tl, in0=tl, scalar1=-7.0, scalar2=1.0, op0=ALU.max, op1=ALU.add), r=[kl_], w=[kl_])
                            P.pool(I("tensor_tensor", out=tg, in0=tg, in1=tsg, op=ALU.mult), r=[kg, ksg], w=[kg])
                            P.pool(I("tensor_tensor", out=actT[:, ft, :], in0=tg, in1=tl, op=ALU.mult), r=[kg, kl_], w=["actT"])
                        for ti in range(4):
                            tl_ = tc * 4 + ti
                            for dh in range(2):
                                pdn = bank(4 + (ti * 2 + dh) % 4); dk = "pdn%d" % ((ti * 2 + dh) % 4)
                                for ft in range(8):
                                    P.pe(I("matmul", pdn, lhsT=actT[:, ft, ti * 128:(ti + 1) * 128], rhs=Wd[b_][:, ft, dh * 512:(dh + 1) * 512], start=(ft == 0), stop=(ft == 7)), r=["actT", "Wd%d" % b_], w=[dk])
                                P.dve(I("scalar_tensor_tensor", out=acc[:, tl_, dh * 512:(dh + 1) * 512], in0=pdn, scalar=G[:, c * 8 + tl_, e:e + 1], in1=acc[:, tl_, dh * 512:(dh + 1) * 512], op0=ALU.mult, op1=ALU.add), r=[dk, "G", "acc"], w=["acc"])
                for tl_ in range(8):
                    t = c * 8 + tl_
                    sb_ = stn[0] % 2; stn[0] += 1
                    xs = stage[sb_][:, 0:1024]
                    P.dma(I("dma_start", out=xs, in_=X1[t * 128:(t + 1) * 128, :]), r=["X1"], w=["stage%d" % sb_])
                    P.dve(I("scalar_tensor_tensor", out=xs, in0=xs, scalar=ALPHA, in1=acc[:, tl_, :], op0=ALU.mult, op1=ALU.add), r=["stage%d" % sb_, "acc"], w=["stage%d" % sb_])
                    P.dma(I("dma_start", out=RR[t * 128:(t + 1) * 128, :], in_=xs), r=["stage%d" % sb_], w=["RR"])
            P.barrier()
            AR.reset(gmark)

        if "5" in phases:
            wpg = AR.alloc([128, 8, 1024], BF16); wpp = AR.alloc([128, 2, 1024], BF16)
            for kt in range(8):
                P.dma(I("dma_start", out=wpg[:, kt, :], in_=w_pg[kt * 128:(kt + 1) * 128, :]), w=["wpg"], q="gpsimd")
            for kt in range(2):
                P.dma(I("dma_start", out=wpp[:, kt, :], in_=w_pp[kt * 128:(kt + 1) * 128, :]), w=["wpp"], q="gpsimd")
            g2b = AR.alloc([128, 1024]); b2b = AR.alloc([128, 1024])
            bcast_row(g2b, ln2_g, 1024, "lng"); bcast_row(b2b, ln2_b, 1024, "lng")
            AR_t["st"] = AR.alloc([128, 12]); AR_t["mv"] = AR.alloc([128, 2]); AR_t["rs"] = AR.alloc([128, 1])
            rt = [AR.alloc([128, 1024]) for _ in range(2)]; ptl = [AR.alloc([128, 256]) for _ in range(2)]
            rT = AR.alloc([128, 8, 128], BF16); pT = AR.alloc([128, 2, 128], BF16)
            sgt = AR.alloc([128, 1024]); yv = AR.alloc([128, 1024]); yo = [AR.alloc([128, 1024]) for _ in range(2)]
            for t in range(32):
                b_ = t % 2
                P.dma(I("dma_start", out=rt[b_], in_=RR[t * 128:(t + 1) * 128, :]), r=["RR"], w=["rt%d" % b_])
                P.dma(I("dma_start", out=ptl[b_], in_=pc[t * 128:(t + 1) * 128, :]), w=["ptl%d" % b_])
                ptr = bank(4, 2).rearrange("p (a b) -> p a b", a=8)
                for kt in range(8):
                    P.pe(I("transpose", out=ptr[:, kt, :], in_=rt[b_][:, kt * 128:(kt + 1) * 128], identity=ident), r=["rt%d" % b_, "ident"], w=["ptr5"])
                P.act(I("activation", out=rT, in_=ptr, func=AF.Copy), r=["ptr5"], w=["rT"])
                ptp = bank(6).rearrange("p (a b) -> p a b", a=4)
                for kt in range(2):
                    P.pe(I("transpose", out=ptp[:, kt, :], in_=ptl[b_][:, kt * 128:(kt + 1) * 128], identity=ident), r=["ptl%d" % b_, "ident"], w=["ptp"])
                P.dve(I("tensor_copy", out=pT, in_=ptp[:, 0:2, :]), r=["ptp"], w=["pT"])
                pgt = bank(0, 2); ppp = bank(2, 2)
                for dh in range(2):
                    for kt in range(8):
                        P.pe(I("matmul", pgt[:, dh * 512:(dh + 1) * 512], lhsT=rT[:, kt, :], rhs=wpg[:, kt, dh * 512:(dh + 1) * 512], start=(kt == 0), stop=(kt == 7)), r=["rT", "wpg"], w=["pgt"])
                    for kt in range(2):
                        P.pe(I("matmul", ppp[:, dh * 512:(dh + 1) * 512], lhsT=pT[:, kt, :], rhs=wpp[:, kt, dh * 512:(dh + 1) * 512], start=(kt == 0), stop=(kt == 1)), r=["pT", "wpp"], w=["ppp"])
                P.act(I("activation", out=sgt, in_=pgt, func=AF.Sigmoid), r=["pgt"], w=["sgt"])
                P.dve(I("tensor_tensor", out=sgt, in0=sgt, in1=ppp, op=ALU.mult), r=["sgt", "ppp"], w=["sgt"])
                P.pool(I("tensor_tensor", out=yv, in0=sgt, in1=rt[b_], op=ALU.add), r=["sgt", "rt%d" % b_], w=["yv"])
                layer_norm_tile(yv, g2b, b2b, yo[b_], "yv")
                P.dma(I("dma_start", out=out[t * 128:(t + 1) * 128, :], in_=yo[b_]), r=["yvo"], w=["out"])
            P.barrier()

        P.emit(st)
    return nc


_NC_CACHE = {}


def _prep_inputs(inputs):
    sq = lambda k: np.ascontiguousarray(np.asarray(inputs[k])[0])
    x = np.asarray(inputs["x"]); p = np.asarray(inputs["p"])[0]
    shared = {
        "w_in": sq("w_in"), "lambda_q1": np.asarray(inputs["lambda_q1"]), "lambda_k1": np.asarray(inputs["lambda_k1"]),
        "lambda_q2": np.asarray(inputs["lambda_q2"]), "lambda_k2": np.asarray(inputs["lambda_k2"]),
        "subln_g": np.ascontiguousarray(np.asarray(inputs["subln_g"]).reshape(64, 1)),
        "ssm_a_re": sq("ssm_a_re"), "ssm_a_im": sq("ssm_a_im"), "ssm_log_dt": np.asarray(inputs["ssm_log_dt"]),
        "ssm_b_re": sq("ssm_b_re"), "ssm_b_im": sq("ssm_b_im"), "ssm_c_re": sq("ssm_c_re"), "ssm_c_im": sq("ssm_c_im"),
        "ssm_d": sq("ssm_d"), "w_glu": sq("w_glu"), "ssm_norm_g": sq("ssm_norm_g"), "w_out": sq("w_out"),
        "ln1_g": np.asarray(inputs["ln1_g"]), "ln1_b": np.asarray(inputs["ln1_b"]),
        "w_router": sq("w_router"), "b_router": np.asarray(inputs["b_router"]),
        "w_gate_up": sq("w_gate_up")[:NE], "b_gate_up": sq("b_gate_up"), "w_down": sq("w_down")[:NE], "b_down": sq("b_down"),
        "w_ple_gate": sq("w_ple_gate"), "w_ple_proj": sq("w_ple_proj"),
        "ln2_g": np.asarray(inputs["ln2_g"]), "ln2_b": np.asarray(inputs["ln2_b"]),
    }
    shared = {k: np.ascontiguousarray(v, dtype=np.float32) for k, v in shared.items()}
    maps = []
    for c in range(8):
        b, h = c // 2, c % 2
        if h == 0:
            xcore = np.concatenate([np.zeros((LO, DM), np.float32), x[b, :LO]], axis=0)
        else:
            xcore = np.ascontiguousarray(x[b])
        m = dict(shared)
        m["xc"] = np.ascontiguousarray(xcore, dtype=np.float32)
        m["pc"] = np.ascontiguousarray(p[b, h * LO:(h + 1) * LO], dtype=np.float32)
        m["pref"] = np.full((128, 1), NEG if h == 0 else 0.0, np.float32)
        maps.append(m)
    return maps


def kernel(**inputs):
    dbg = KDEBUG
    if dbg not in _NC_CACHE:
        _NC_CACHE[dbg] = build_program(dbg)
    nc = _NC_CACHE[dbg]
    maps = _prep_inputs(inputs)
    res = run_bass_kernel_spmd(nc, maps, core_ids=list(range(8)))
    if dbg:
        return res.results
    outp = np.zeros((4, L, DM), np.float32)
    for c in range(8):
        b, h = c // 2, c % 2
        outp[b, h * LO:(h + 1) * LO] = res.results[c]["out"]
    return outp
```

```python
import math
import os
from contextlib import ExitStack

import numpy as np
import concourse.bass as bass
import concourse.mybir as mybir
from concourse.bass_utils import run_bass_kernel_spmd

F32 = mybir.dt.float32
BF16 = mybir.dt.bfloat16
AF = mybir.ActivationFunctionType
ALU = mybir.AluOpType
AX = mybir.AxisListType

ENGINES = ("tensor", "vector", "scalar", "gpsimd", "sync")
SAME_ENGINE_SYNC = True
KDEBUG = os.environ.get("KDEBUG", "")

L = 8192
LO = 4096
DM = 1024
ALPHA = 2.0 ** 0.25
LAMBDA_INIT = 0.2
ATT_TH = 64.0
NEG = -30000.0
NE = int(os.environ.get('KNE', '32'))
NCH = int(os.environ.get('KNC', '4'))


KEYMAP = {
    "pb0": ["B0"], "pb1": ["B1"], "pbC": ["B2"], "pbu_re": ["B2"], "pbu_im": ["B3"], "py0": ["B4"], "py1": ["B5"], "pms": ["B6"],
    "sc0": ["B0", "B1"], "sc1": ["B2", "B3"], "Ooff": ["B4", "B5"], "Odg": ["B6", "B7"],
    "pm": ["B0", "B1"], "ptr": ["B2", "B3"], "pl": ["B4"],
    "pa": ["B0", "B1"], "pg": ["B6"], "pgl0": ["B0"], "pgl0l": ["B1"], "pgl1": ["B2"], "pgl1l": ["B3"],
    "pdn0": ["B4"], "pdn1": ["B5"], "pdn2": ["B6"], "pdn3": ["B7"],
    "ptr5": ["B4", "B5"], "ptp": ["B6"], "pgt": ["B0", "B1"], "ppp": ["B2", "B3"],
}


def _mapkeys(ks):
    out = []
    for k in ks:
        out.extend(KEYMAP.get(k, [k]))
    return out


class Prog:
    def __init__(self, nc, n_dma_sems=24):
        self.nc = nc
        self.ops = []
        self.last_w = {}
        self.readers = {}
        self.n_dma_sems = n_dma_sems
        self.last_of = {e: None for e in ENGINES}
        self.recent_dma = {e: [] for e in ENGINES}
        self.pe_sync = False

    def op(self, eng, fn, r=(), w=(), dma=False, extra=()):
        idx = len(self.ops)
        deps = set(extra)
        r = _mapkeys(r); w = _mapkeys(w)
        for k in r:
            if k in self.last_w:
                deps.add(self.last_w[k])
        for k in w:
            if k in self.last_w:
                deps.add(self.last_w[k])
            for x in self.readers.get(k, ()):
                deps.add(x)
        for k in w:
            self.last_w[k] = idx
            self.readers[k] = []
        for k in r:
            if k not in w:
                self.readers.setdefault(k, []).append(idx)
        deps.discard(idx)
        self.ops.append(dict(eng=eng, fn=fn, deps=deps, dma=dma, idx=idx, pesync=self.pe_sync))
        self.last_of[eng] = idx
        if dma:
            self.recent_dma[eng].append(idx)
            self.recent_dma[eng] = self.recent_dma[eng][-self.n_dma_sems:]
        return idx

    def pe(self, fn, r=(), w=(), tiled=False):
        extra = ()
        sv = self.pe_sync
        if tiled:
            self.pe_sync = True
            self.pe_was_tiled = True
        elif getattr(self, "pe_was_tiled", False):
            extra = (self.last_of["tensor"],)
            self.pe_sync = True
            self.pe_was_tiled = False
        i = self.op("tensor", fn, r, w, extra=extra)
        self.pe_sync = sv
        return i
    def dve(self, fn, r=(), w=()): return self.op("vector", fn, r, w)
    def act(self, fn, r=(), w=()): return self.op("scalar", fn, r, w)
    def pool(self, fn, r=(), w=()): return self.op("gpsimd", fn, r, w)
    def dma(self, fn, r=(), w=(), q="sync"): return self.op(q, fn, r, w, dma=True)

    def barrier(self):
        deps = set()
        for e in ENGINES:
            if self.last_of[e] is not None:
                deps.add(self.last_of[e])
            deps.update(self.recent_dma[e])
        for e in ENGINES:
            self.op(e, None, extra=tuple(deps))
        self.last_w = {}
        self.readers = {}

    def emit(self, stack):
        nc = self.nc
        ops = self.ops
        needed = set()
        for o in ops:
            for d in o["deps"]:
                if o["eng"] == "tensor" and ops[d]["eng"] == "tensor" and o["fn"] is not None and not o["pesync"]:
                    continue
                needed.add(d)
        for e in ENGINES:
            if self.last_of[e] is not None:
                needed.add(self.last_of[e])
        csem = {e: stack.enter_context(nc.semaphore(f"c_{e}")) for e in ENGINES}
        ccount = {e: 0 for e in ENGINES}
        dsems, dcount = {}, {}
        dnext = {e: 0 for e in ENGINES}
        for e in ("sync", "gpsimd", "scalar"):
            dsems[e] = [stack.enter_context(nc.semaphore(f"d_{e}_{i}")) for i in range(self.n_dma_sems)]
            dcount[e] = [0] * self.n_dma_sems
        for o in ops:
            e = o["eng"]
            if o["fn"] is None:
                o["sig"] = None
            elif o["dma"]:
                j = dnext[e]
                dnext[e] = (j + 1) % self.n_dma_sems
                o["dma_prev"] = (dsems[e][j], dcount[e][j])
                dcount[e][j] += 16
                o["sig"] = (dsems[e][j], dcount[e][j])
            elif o["idx"] in needed:
                ccount[e] += 1
                o["sig"] = (csem[e], ccount[e])
            else:
                o["sig"] = None
        def resolve(d, seen):
            od = ops[d]
            if od["fn"] is not None:
                return [d]
            out = []
            for dd in od["deps"]:
                if dd not in seen:
                    seen.add(dd)
                    out.extend(resolve(dd, seen))
            return out
        waited = {e: {} for e in ENGINES}
        per_eng = {e: [o for o in ops if o["eng"] == e] for e in ENGINES}
        block = stack.enter_context(nc.Block())

        def make(e):
            def body(eng):
                wd = waited[e]

                def wait(sem, val):
                    if val <= 0:
                        return
                    key = id(sem)
                    if wd.get(key, 0) >= val:
                        return
                    eng.wait_ge(sem, val)
                    wd[key] = val
                for o in per_eng[e]:
                    alld = []
                    seen = set()
                    for d in sorted(o["deps"]):
                        alld.extend(resolve(d, seen))
                    for d in sorted(set(alld)):
                        od = ops[d]
                        if od["sig"] is None:
                            continue
                        if (not od["dma"]) and od["eng"] == e and (not SAME_ENGINE_SYNC or (e == "tensor" and not o["pesync"])):
                            continue
                        wait(*od["sig"])
                    if o["fn"] is None:
                        continue
                    if o["dma"]:
                        wait(*o["dma_prev"])
                    ins = o["fn"](eng)
                    if o["sig"] is not None:
                        ins.then_inc(o["sig"][0], 16 if o["dma"] else 1)
                if e == "sync":
                    for q in dsems:
                        for j in range(self.n_dma_sems):
                            wait(dsems[q][j], dcount[q][j])
                    for e2 in ENGINES:
                        if e2 != "sync":
                            wait(csem[e2], ccount[e2])
            return body

        block.tensor(make("tensor"))
        block.vector(make("vector"))
        block.scalar(make("scalar"))
        block.gpsimd(make("gpsimd"))
        block.sync(make("sync"))


def I(name, *a, **k):
    return lambda e: getattr(e, name)(*a, **k)


class Arena:
    def __init__(self, ap_f32, nbytes):
        self.ap = ap_f32
        self.n = nbytes
        self.off = 0

    def mark(self): return self.off
    def reset(self, m): self.off = m

    def alloc(self, shape, dt=F32, parts=128):
        esz = 4 if dt == F32 else 2
        n = int(np.prod(shape[1:])) * esz
        n4 = (n + 3) // 4
        assert self.off + n4 * 4 <= self.n, f"arena overflow {self.off + n4 * 4} > {self.n}"
        a = self.ap[0:shape[0], self.off // 4:self.off // 4 + n4]
        self.off += n4 * 4
        if dt != F32:
            a = a.bitcast(dt)
        if len(shape) == 3:
            a = a.rearrange("p (a b) -> p a b", a=shape[1])
        elif len(shape) == 4:
            a = a.rearrange("p (a b c) -> p a b c", a=shape[1], b=shape[2])
        return a


def _qkey_lo(q0, slope):
    kmin = q0 - ATT_TH / slope
    return max(0, int(math.floor(kmin / 128.0)))


def build_program(dbg=""):
    nc = bass.Bass("TRN2", target_bir_lowering=False)
    D = {}

    def din(name, shape, dt=F32):
        D[name] = nc.dram_tensor(name, list(shape), dt, kind="ExternalInput").ap()
        return D[name]

    def dscr(name, shape, dt):
        kind = "ExternalOutput" if name in dbg.split(",") else "Internal"
        D[name] = nc.dram_tensor(name, list(shape), dt, kind=kind).ap()
        return D[name]

    xc = din("xc", [L, DM]); pc = din("pc", [LO, 256]); pref = din("pref", [128, 1])
    w_in = din("w_in", [1024, 2048])
    lq1 = din("lambda_q1", [1, 32]); lk1 = din("lambda_k1", [1, 32]); lq2 = din("lambda_q2", [1, 32]); lk2 = din("lambda_k2", [1, 32])
    subln_g = din("subln_g", [64, 1])
    a_re = din("ssm_a_re", [32, 64]); a_im = din("ssm_a_im", [32, 64]); log_dt = din("ssm_log_dt", [1, 32])
    b_re = din("ssm_b_re", [32, 64, 16]); b_im = din("ssm_b_im", [32, 64, 16])
    c_re = din("ssm_c_re", [32, 16, 64]); c_im = din("ssm_c_im", [32, 16, 64])
    ssm_d = din("ssm_d", [512]); w_glu = din("w_glu", [512, 1024]); ssm_norm_g = din("ssm_norm_g", [512])
    w_out = din("w_out", [1024, 1024]); ln1_g = din("ln1_g", [1, 1024]); ln1_b = din("ln1_b", [1, 1024])
    w_router = din("w_router", [1024, 32]); b_router = din("b_router", [1, 32])
    phases = os.environ.get("KPHASES", "12345")
    if "4" in phases:
        w_gu = din("w_gate_up", [NE, 1024, 2048]); b_gu = din("b_gate_up", [32, 2048])
        w_dn = din("w_down", [NE, 1024, 1024]); b_dn = din("b_down", [32, 1024])
    w_pg = din("w_ple_gate", [1024, 1024]); w_pp = din("w_ple_proj", [256, 1024])
    ln2_g = din("ln2_g", [1, 1024]); ln2_b = din("ln2_b", [1, 1024])
    out = nc.dram_tensor("out", [LO, DM], F32, kind="ExternalOutput").ap()

    KT = dscr("KT", [4, 128, L], BF16); QT = dscr("QT", [4, 128, LO], BF16)
    VV = dscr("VV", [64, 128, 520], BF16)
    MIXT = dscr("MIXT", [8, 128, LO], BF16)
    X1 = dscr("X1", [LO, DM], F32); X1T = dscr("X1T", [8, 128, LO], BF16)
    RR = dscr("RR", [LO, DM], F32)

    with ExitStack() as st:
        st.enter_context(nc.allow_non_contiguous_dma(reason="layout"))
        ARENA_B = 200 * 1024
        arena_t = st.enter_context(nc.sbuf_tensor("arena", [128, ARENA_B // 4], F32))
        psum_t = st.enter_context(nc.psum_tensor("psum", [128, 4096], F32))
        AR = Arena(arena_t, ARENA_B)
        P = Prog(nc)

        def bank(i, n=1):
            return psum_t[:, 512 * i:512 * (i + n)]

        ident = AR.alloc([128, 128]); ones = AR.alloc([128, 128])
        P.pool(I("memset", ident, 1.0), w=["ident"])
        P.pool(I("affine_select", out=ident, in_=ident, pattern=[[-1, 128]], compare_op=ALU.is_equal,
                                         fill=0.0, base=0, channel_multiplier=1), r=["ident"], w=["ident"])
        P.pool(I("memset", ones, 1.0), w=["ones"])
        gmark = AR.mark()

        if "1" in phases:
            P.pe_sync = False
            win = AR.alloc([128, 8, 2048], BF16)
            for kt in range(8):
                P.dma(I("dma_start", out=win[:, kt, :], in_=w_in[kt * 128:(kt + 1) * 128, :]), w=["win"], q="gpsimd")
            wglu = AR.alloc([128, 4, 1024], BF16)
            for kt in range(4):
                P.dma(I("dma_start", out=wglu[:, kt, :], in_=w_glu[kt * 128:(kt + 1) * 128, :]), w=["wglu"], q="gpsimd")
            sm = lambda: AR.alloc([128, 16])
            are, aim, dtt, rho, th, cth, sth, Are, Aim, t0, t1, t2, Fre, Fim, d2 = [sm() for _ in range(15)]
            P.dma(I("dma_start", out=are, in_=a_re.rearrange("(gp g2) p -> (g2 p) gp", g2=2)), w=["are"])
            P.dma(I("dma_start", out=aim, in_=a_im.rearrange("(gp g2) p -> (g2 p) gp", g2=2)), w=["aim"])
            ldv = log_dt.rearrange("o (gp g2) -> o g2 gp", g2=2)
            for g2 in range(2):
                P.dma(I("dma_start", out=dtt[64 * g2:64 * g2 + 64, :], in_=ldv[:, g2, :].to_broadcast([64, 16])), w=["dtt"])
            P.act(I("activation", out=dtt, in_=dtt, func=AF.Exp), r=["dtt"], w=["dtt"])
            P.dve(I("tensor_tensor", out=t0, in0=are, in1=dtt, op=ALU.mult), r=["are", "dtt"], w=["t0"])
            P.act(I("activation", out=rho, in_=t0, func=AF.Exp), r=["t0"], w=["rho"])
            P.dve(I("tensor_tensor", out=th, in0=aim, in1=dtt, op=ALU.mult), r=["aim", "dtt"], w=["th"])
            P.dve(I("memset", t1, 0.0), w=["t1"])
            for kk in range(6):
                thr = (2 * kk + 1) * math.pi
                P.dve(I("tensor_scalar", out=t2, in0=th, scalar1=-thr, scalar2=1e6, op0=ALU.add, op1=ALU.mult), r=["th"], w=["t2"])
                P.dve(I("tensor_scalar", out=t2, in0=t2, scalar1=0.0, scalar2=1.0, op0=ALU.max, op1=ALU.min), r=["t2"], w=["t2"])
                P.dve(I("tensor_tensor", out=t1, in0=t1, in1=t2, op=ALU.add), r=["t1", "t2"], w=["t1"])
            P.dve(I("scalar_tensor_tensor", out=th, in0=t1, scalar=-2.0 * math.pi, in1=th, op0=ALU.mult, op1=ALU.add), r=["t1", "th"], w=["th"])
            P.act(I("activation", out=sth, in_=th, func=AF.Sin), r=["th"], w=["sth"])
            P.dve(I("tensor_scalar", out=t2, in0=th, scalar1=-1.0, scalar2=None, op0=ALU.mult), r=["th"], w=["t2"])
            P.dve(I("tensor_tensor", out=t2, in0=t2, in1=th, op=ALU.max), r=["t2", "th"], w=["t2"])
            P.dve(I("tensor_scalar", out=t2, in0=t2, scalar1=-1.0, scalar2=math.pi / 2, op0=ALU.mult, op1=ALU.add), r=["t2"], w=["t2"])
            P.act(I("activation", out=cth, in_=t2, func=AF.Sin), r=["t2"], w=["cth"])
            P.dve(I("tensor_tensor", out=Are, in0=rho, in1=cth, op=ALU.mult), r=["rho", "cth"], w=["Are"])
            P.dve(I("tensor_tensor", out=Aim, in0=rho, in1=sth, op=ALU.mult), r=["rho", "sth"], w=["Aim"])
            P.dve(I("tensor_scalar", out=t0, in0=Are, scalar1=-1.0, scalar2=None, op0=ALU.add), r=["Are"], w=["t0"])
            P.dve(I("tensor_tensor", out=d2, in0=are, in1=are, op=ALU.mult), r=["are"], w=["d2"])
            P.dve(I("tensor_tensor", out=t1, in0=aim, in1=aim, op=ALU.mult), r=["aim"], w=["t1"])
            P.dve(I("tensor_tensor", out=d2, in0=d2, in1=t1, op=ALU.add), r=["d2", "t1"], w=["d2"])
            P.dve(I("reciprocal", out=d2, in_=d2), r=["d2"], w=["d2"])
            P.dve(I("tensor_tensor", out=t1, in0=t0, in1=are, op=ALU.mult), r=["t0", "are"], w=["t1"])
            P.dve(I("tensor_tensor", out=t2, in0=Aim, in1=aim, op=ALU.mult), r=["Aim", "aim"], w=["t2"])
            P.dve(I("tensor_tensor", out=t1, in0=t1, in1=t2, op=ALU.add), r=["t1", "t2"], w=["t1"])
            P.dve(I("tensor_tensor", out=Fre, in0=t1, in1=d2, op=ALU.mult), r=["t1", "d2"], w=["Fre"])
            P.dve(I("tensor_tensor", out=t1, in0=Aim, in1=are, op=ALU.mult), r=["Aim", "are"], w=["t1"])
            P.dve(I("tensor_tensor", out=t2, in0=t0, in1=aim, op=ALU.mult), r=["t0", "aim"], w=["t2"])
            P.dve(I("tensor_tensor", out=t1, in0=t1, in1=t2, op=ALU.subtract), r=["t1", "t2"], w=["t1"])
            P.dve(I("tensor_tensor", out=Fim, in0=t1, in1=d2, op=ALU.mult), r=["t1", "d2"], w=["Fim"])
            Bre = AR.alloc([128, 16, 16]); Bim = AR.alloc([128, 16, 16]); Bbr = AR.alloc([128, 16, 16]); Bbi = AR.alloc([128, 16, 16]); Bt = AR.alloc([128, 16, 16])
            P.dma(I("dma_start", out=Bre, in_=b_re.rearrange("(gp g2) p c -> (g2 p) gp c", g2=2)), w=["Bre"])
            P.dma(I("dma_start", out=Bim, in_=b_im.rearrange("(gp g2) p c -> (g2 p) gp c", g2=2)), w=["Bim"])
            bc = lambda a: a.unsqueeze(2).to_broadcast([128, 16, 16])
            P.dve(I("tensor_tensor", out=Bbr, in0=Bre, in1=bc(Fre), op=ALU.mult), r=["Bre", "Fre"], w=["Bbr"])
            P.dve(I("tensor_tensor", out=Bt, in0=Bim, in1=bc(Fim), op=ALU.mult), r=["Bim", "Fim"], w=["Bt"])
            P.dve(I("tensor_tensor", out=Bbr, in0=Bbr, in1=Bt, op=ALU.subtract), r=["Bbr", "Bt"], w=["Bbr"])
            P.dve(I("tensor_tensor", out=Bbi, in0=Bim, in1=bc(Fre), op=ALU.mult), r=["Bim", "Fre"], w=["Bbi"])
            P.dve(I("tensor_tensor", out=Bt, in0=Bre, in1=bc(Fim), op=ALU.mult), r=["Bre", "Fim"], w=["Bt"])
            P.dve(I("tensor_tensor", out=Bbi, in0=Bbi, in1=Bt, op=ALU.add), r=["Bbi", "Bt"], w=["Bbi"])
            Bp = [AR.alloc([128, 16, 128], BF16), AR.alloc([128, 16, 128], BF16)]
            m1 = AR.mark()
            Lx = AR.alloc([128, 16, 128])
            for ri, Bb in enumerate((Bbr, Bbi)):
                P.dve(I("memset", Lx, 0.0), w=["Lx"])
                Lv = Lx.rearrange("q gp (l g c) -> q gp l g c", l=4, g=2)
                for g2 in range(2):
                    P.dve(I("tensor_copy",
                        out=Lv[64 * g2:64 * g2 + 64, :, :, g2, :],
                        in_=Bb[64 * g2:64 * g2 + 64].unsqueeze(2).to_broadcast([64, 16, 4, 16])), r=["Bbr", "Bbi"], w=["Lx"])
                for g4 in range(4):
                    pb = bank(g4 % 2).rearrange("p (a b) -> p a b", a=4)
                    for j in range(4):
                        gp = g4 * 4 + j
                        P.pe(I("transpose", out=pb[:, j, :], in_=Lx[:, gp, :], identity=ident), r=["Lx", "ident"], w=["pb%d" % (g4 % 2)])
                    P.dve(I("tensor_copy", out=Bp[ri][:, g4 * 4:g4 * 4 + 4, :], in_=pb), r=["pb%d" % (g4 % 2)], w=["Bp"])
            AR.reset(m1)
            Cr = AR.alloc([128, 16, 32]); Cni = AR.alloc([128, 16, 32])
            m1 = AR.mark()
            Cx = AR.alloc([128, 16, 128])
            for ri, csrc in enumerate((c_re, c_im)):
                P.dve(I("memset", Cx[0:32], 0.0), w=["Cx"])
                cv = csrc.rearrange("(gp g2) c p -> g2 c gp p", g2=2)
                for g2 in range(2):
                    P.dma(I("dma_start", out=Cx[16 * g2:16 * g2 + 16, :, 64 * g2:64 * g2 + 64], in_=cv[g2]), r=["Cx"], w=["Cx"])
                pb = bank(2).rearrange("p (a b) -> p a b", a=16)
                for gp in range(16):
                    P.pe(I("transpose", out=pb[:, gp, :], in_=Cx[0:32, gp, :], identity=ident[0:32, 0:32]), r=["Cx", "ident"], w=["pbC"])
                if ri == 0:
                    P.dve(I("tensor_copy", out=Cr, in_=pb), r=["pbC"], w=["Cr"])
                else:
                    P.dve(I("tensor_scalar", out=Cni, in0=pb, scalar1=-1.0, scalar2=None, op0=ALU.mult), r=["pbC"], w=["Cni"])
            AR.reset(m1)
            cn = AR.alloc([128, 16, 128]); sn = AR.alloc([128, 16, 128]); tA = AR.alloc([128, 16, 64]); tB = AR.alloc([128, 16, 64])
            P.dve(I("tensor_copy", out=cn[:, :, 0:1], in_=cth.unsqueeze(2)), r=["cth"], w=["cn"])
            P.dve(I("tensor_copy", out=sn[:, :, 0:1], in_=sth.unsqueeze(2)), r=["sth"], w=["sn"])
            m = 1
            while m < 128:
                cm = cn[:, :, m - 1:m].to_broadcast([128, 16, m]); smm = sn[:, :, m - 1:m].to_broadcast([128, 16, m])
                ta = tA[:, :, 0:m]; tb = tB[:, :, 0:m]
                P.dve(I("tensor_tensor", out=ta, in0=cn[:, :, 0:m], in1=cm, op=ALU.mult), r=["cn"], w=["tA"])
                P.dve(I("tensor_tensor", out=tb, in0=sn[:, :, 0:m], in1=smm, op=ALU.mult), r=["sn"], w=["tB"])
                P.dve(I("tensor_tensor", out=ta, in0=ta, in1=tb, op=ALU.subtract), r=["tA", "tB"], w=["tA"])
                P.dve(I("tensor_tensor", out=tb, in0=cn[:, :, 0:m], in1=smm, op=ALU.mult), r=["cn", "sn"], w=["tB"])
                P.dve(I("tensor_copy", out=cn[:, :, m:2 * m], in_=ta), r=["tA"], w=["cn"])
                P.dve(I("tensor_tensor", out=ta, in0=sn[:, :, 0:m], in1=cm, op=ALU.mult), r=["sn", "cn"], w=["tA"])
                P.dve(I("tensor_tensor", out=sn[:, :, m:2 * m], in0=ta, in1=tb, op=ALU.add), r=["tA", "tB"], w=["sn"])
                m *= 2
            dcol = AR.alloc([128, 4]); gncol = AR.alloc([128, 4])
            P.dma(I("dma_start", out=dcol, in_=ssm_d.rearrange("(ut r) -> r ut", r=128)), w=["dcol"])
            P.dma(I("dma_start", out=gncol, in_=ssm_norm_g.rearrange("(ut r) -> r ut", r=128)), w=["gncol"])
            Sre = AR.alloc([128, 16]); Sim = AR.alloc([128, 16])
            P.dve(I("memset", Sre, 0.0), w=["Sre"]); P.dve(I("memset", Sim, 0.0), w=["Sim"])
            xin = [AR.alloc([128, 1024]) for _ in range(2)]
            xT = [AR.alloc([128, 8, 512], BF16) for _ in range(2)]
            kst = AR.alloc([128, 4, 512], BF16); qst = AR.alloc([128, 4, 512], BF16)
            vst = [AR.alloc([128, 8, 65], BF16) for _ in range(2)]
            for b_ in range(2):
                P.dve(I("memset", vst[b_], 1.0), w=["vst%d" % b_])
            uT = AR.alloc([128, 4, 512], BF16)
            vre = AR.alloc([128, 4, 128]); vim = AR.alloc([128, 4, 128]); e1 = AR.alloc([128, 4, 128]); e2 = AR.alloc([128, 4, 128])
            wre = AR.alloc([128, 4, 128]); wim = AR.alloc([128, 4, 128]); sre = AR.alloc([128, 4, 128]); sim = AR.alloc([128, 4, 128])
            yd = AR.alloc([128, 512]); g1 = AR.alloc([128, 512]); g2t = AR.alloc([128, 512])
            glT = AR.alloc([128, 4, 512], BF16)
            y2 = AR.alloc([128, 4, 512]); sq = AR.alloc([128, 512]); sig = AR.alloc([128, 512]); rstd = AR.alloc([128, 512])
            soT = AR.alloc([128, 4, 512], BF16)
            pbu_re = bank(2).rearrange("p (a b) -> p a b", a=4); pbu_im = bank(3).rearrange("p (a b) -> p a b", a=4)
            for s in range(16):
                own = s >= 8
                xt = xT[s % 2]; xk = "xT%d" % (s % 2)
                for ti in range(4):
                    xb = xin[ti % 2]; xbk = "xin%d" % (ti % 2)
                    r0 = s * 512 + ti * 128
                    P.dma(I("dma_start", out=xb, in_=xc[r0:r0 + 128, :]), w=[xbk])
                    for half in range(2):
                        pb = bank(half).rearrange("p (a b) -> p a b", a=4)
                        for j in range(4):
                            kt = half * 4 + j
                            P.pe(I("transpose", out=pb[:, j, :], in_=xb[:, kt * 128:(kt + 1) * 128], identity=ident), r=[xbk, "ident"], w=["pb%d" % half])
                        if half == 0:
                            P.dve(I("tensor_copy", out=xt[:, 0:4, ti * 128:(ti + 1) * 128], in_=pb), r=["pb0"], w=[xk])
                        else:
                            P.act(I("activation", out=xt[:, 4:8, ti * 128:(ti + 1) * 128], in_=pb, func=AF.Copy), r=["pb1"], w=[xk])
                def proj_fm(col0, dst, dk, n_m=4):
                    for mt in range(n_m):
                        pb = bank(mt % 2); pk = "pb%d" % (mt % 2)
                        for kt in range(8):
                            P.pe(I("matmul", pb, lhsT=win[:, kt, col0 + mt * 128:col0 + (mt + 1) * 128], rhs=xt[:, kt, :], start=(kt == 0), stop=(kt == 7)), r=["win", xk], w=[pk])
                        if mt % 2 == 0:
                            P.dve(I("tensor_copy", out=dst[:, mt, :], in_=pb), r=[pk], w=[dk])
                        else:
                            P.act(I("activation", out=dst[:, mt, :], in_=pb, func=AF.Copy), r=[pk], w=[dk])
                proj_fm(512, kst, "kst")
                P.dma(I("dma_start", out=KT[:, :, s * 512:(s + 1) * 512].rearrange("h p t -> p h t"), in_=kst), r=["kst"], w=["KT"])
                if own:
                    proj_fm(0, qst, "qst")
                    P.dma(I("dma_start", out=QT[:, :, (s - 8) * 512:(s - 7) * 512].rearrange("h p t -> p h t"), in_=qst), r=["qst"], w=["QT"])
                for ti in range(4):
                    pb = bank(ti % 2); pk = "pb%d" % (ti % 2)
                    vb = vst[ti % 2]; vk = "vst%d" % (ti % 2)
                    for kt in range(8):
                        P.pe(I("matmul", pb, lhsT=xt[:, kt, ti * 128:(ti + 1) * 128], rhs=win[:, kt, 1024:1536], start=(kt == 0), stop=(kt == 7)), r=["win", xk], w=[pk])
                    P.dve(I("tensor_copy", out=vb[:, :, 0:64], in_=pb.rearrange("p (h d) -> p h d", h=8)), r=[pk], w=[vk])
                    P.dma(I("dma_start", out=VV[s * 4 + ti], in_=vb.rearrange("p h d -> p (h d)")), r=[vk], w=["VV"])
                proj_fm(1536, uT, "uT")
                for ut in range(4):
                    py = bank(4 + ut % 2); pyk = "py%d" % (ut % 2)
                    for un in range(4):
                        tsl = slice(un * 128, (un + 1) * 128)
                        for gl in range(4):
                            gp = ut * 4 + gl
                            P.pe(I("matmul", pbu_re[:, gl, :], lhsT=Bp[0][32 * gl:32 * gl + 32, gp, :], rhs=uT[32 * gl:32 * gl + 32, ut, tsl], start=True, stop=True, tile_position=(32 * gl, 0)), r=["Bp", "uT"], w=["pbu_re"], tiled=True)
                            P.pe(I("matmul", pbu_im[:, gl, :], lhsT=Bp[1][32 * gl:32 * gl + 32, gp, :], rhs=uT[32 * gl:32 * gl + 32, ut, tsl], start=True, stop=True, tile_position=(32 * gl, 0)), r=["Bp", "uT"], w=["pbu_im"], tiled=True)
                        g4 = slice(ut * 4, ut * 4 + 4)
                        cnv = cn[:, g4, :]; snv = sn[:, g4, :]
                        P.dve(I("tensor_tensor", out=vre, in0=pbu_re, in1=cnv, op=ALU.mult), r=["pbu_re", "cn"], w=["vre"])
                        P.dve(I("tensor_tensor", out=e1, in0=pbu_im, in1=snv, op=ALU.mult), r=["pbu_im", "sn"], w=["e1"])
                        P.dve(I("tensor_tensor", out=vim, in0=pbu_im, in1=cnv, op=ALU.mult), r=["pbu_im", "cn"], w=["vim"])
                        P.dve(I("tensor_tensor", out=e2, in0=pbu_re, in1=snv, op=ALU.mult), r=["pbu_re", "sn"], w=["e2"])
                        P.pool(I("tensor_tensor", out=vre, in0=vre, in1=e1, op=ALU.add), r=["vre", "e1"], w=["vre"])
                        P.pool(I("tensor_tensor", out=vim, in0=vim, in1=e2, op=ALU.subtract), r=["vim", "e2"], w=["vim"])
                        for gl in range(4):
                            gp = ut * 4 + gl
                            P.dve(I("tensor_tensor_scan", out=wre[:, gl, :], data0=rho[:, gp:gp + 1].to_broadcast([128, 128]), data1=vre[:, gl, :], initial=Sre[:, gp:gp + 1], op0=ALU.mult, op1=ALU.add), r=["vre", "rho", "Sre"], w=["wre"])
                            P.dve(I("tensor_tensor_scan", out=wim[:, gl, :], data0=rho[:, gp:gp + 1].to_broadcast([128, 128]), data1=vim[:, gl, :], initial=Sim[:, gp:gp + 1], op0=ALU.mult, op1=ALU.add), r=["vim", "rho", "Sim"], w=["wim"])
                        if own:
                            cs, ws = slice(0, 128), slice(0, 4)
                        else:
                            cs, ws = slice(127, 128), slice(0, 4)
                        P.pool(I("tensor_tensor", out=sre[:, :, cs], in0=wre[:, :, cs], in1=cnv[:, :, cs], op=ALU.mult), r=["wre", "cn"], w=["sre"])
                        P.pool(I("tensor_tensor", out=e1[:, :, cs], in0=wim[:, :, cs], in1=snv[:, :, cs], op=ALU.mult), r=["wim", "sn"], w=["e1"])
                        P.pool(I("tensor_tensor", out=sre[:, :, cs], in0=sre[:, :, cs], in1=e1[:, :, cs], op=ALU.subtract), r=["sre", "e1"], w=["sre"])
                        P.pool(I("tensor_tensor", out=sim[:, :, cs], in0=wim[:, :, cs], in1=cnv[:, :, cs], op=ALU.mult), r=["wim", "cn"], w=["sim"])
                        P.pool(I("tensor_tensor", out=e2[:, :, cs], in0=wre[:, :, cs], in1=snv[:, :, cs], op=ALU.mult), r=["wre", "sn"], w=["e2"])
                        P.pool(I("tensor_tensor", out=sim[:, :, cs], in0=sim[:, :, cs], in1=e2[:, :, cs], op=ALU.add), r=["sim", "e2"], w=["sim"])
                        P.dve(I("tensor_copy", out=Sre[:, g4], in_=sre[:, :, 127]), r=["sre"], w=["Sre"])
                        P.dve(I("tensor_copy", out=Sim[:, g4], in_=sim[:, :, 127]), r=["sim"], w=["Sim"])
                        if own:
                            for gl in range(4):
                                gp = ut * 4 + gl
                                P.pe(I("matmul", py[32 * gl:32 * gl + 32, tsl], lhsT=Cr[:, gp, :], rhs=sre[:, gl, :], start=True, stop=False, tile_position=(0, 32 * gl)), r=["Cr", "sre"], w=[pyk], tiled=True)
                                P.pe(I("matmul", py[32 * gl:32 * gl + 32, tsl], lhsT=Cni[:, gp, :], rhs=sim[:, gl, :], start=False, stop=True, tile_position=(0, 32 * gl)), r=["Cni", "sim"], w=[pyk], tiled=True)
                    if own:
                        P.dve(I("scalar_tensor_tensor", out=yd, in0=uT[:, ut, :], scalar=dcol[:, ut:ut + 1], in1=py, op0=ALU.mult, op1=ALU.add), r=["uT", "dcol", pyk], w=["yd"])
                        P.pool(I("tensor_tensor", out=g1, in0=yd, in1=yd, op=ALU.mult), r=["yd"], w=["g1"])
                        P.pool(I("tensor_scalar", out=g1, in0=g1, scalar1=0.044715, scalar2=1.0, op0=ALU.mult, op1=ALU.add), r=["g1"], w=["g1"])
                        P.pool(I("tensor_tensor", out=g1, in0=g1, in1=yd, op=ALU.mult), r=["g1", "yd"], w=["g1"])
                        P.act(I("activation", out=g2t, in_=g1, func=AF.Sigmoid, scale=2.0 * math.sqrt(2.0 / math.pi)), r=["g1"], w=["g2t"])
                        P.pool(I("tensor_tensor", out=glT[:, ut, :], in0=yd, in1=g2t, op=ALU.mult), r=["yd", "g2t"], w=["glT"])
                if own:
                    for mo in range(4):
                        plo = bank(0); phi = bank(1)
                        for kt in range(4):
                            P.pe(I("matmul", plo, lhsT=wglu[:, kt, mo * 128:(mo + 1) * 128], rhs=glT[:, kt, :], start=(kt == 0), stop=(kt == 3)), r=["wglu", "glT"], w=["pb0"])
                        for kt in range(4):
                            P.pe(I("matmul", phi, lhsT=wglu[:, kt, 512 + mo * 128:512 + (mo + 1) * 128], rhs=glT[:, kt, :], start=(kt == 0), stop=(kt == 3)), r=["wglu", "glT"], w=["pb1"])
                        P.act(I("activation", out=sig, in_=phi, func=AF.Sigmoid), r=["pb1"], w=["sig"])
                        P.dve(I("tensor_tensor", out=y2[:, mo, :], in0=plo, in1=sig, op=ALU.mult), r=["pb0", "sig"], w=["y2"])
                    pms = bank(6)
                    for mo in range(4):
                        P.pool(I("tensor_tensor", out=sq, in0=y2[:, mo, :], in1=y2[:, mo, :], op=ALU.mult), r=["y2"], w=["sq"])
                        P.pe(I("matmul", pms, lhsT=ones, rhs=sq, start=(mo == 0), stop=(mo == 3)), r=["ones", "sq"], w=["pms"])
                    P.act(I("activation", out=rstd, in_=pms, func=AF.Ln, scale=1.0 / 512.0, bias=1e-5), r=["pms"], w=["rstd"])
                    P.act(I("activation", out=rstd, in_=rstd, func=AF.Exp, scale=-0.5), r=["rstd"], w=["rstd"])
                    for mo in range(4):
                        P.dve(I("scalar_tensor_tensor", out=soT[:, mo, :], in0=y2[:, mo, :], scalar=gncol[:, mo:mo + 1], in1=rstd, op0=ALU.mult, op1=ALU.mult), r=["y2", "gncol", "rstd"], w=["soT"])
                    P.dma(I("dma_start", out=MIXT[4:8, :, (s - 8) * 512:(s - 7) * 512].rearrange("h p t -> p h t"), in_=soT), r=["soT"], w=["MIXT"])
            P.barrier()
            AR.reset(gmark)


        P.pe_sync = False
        G = AR.alloc([128, 32, 32])
        gmark = AR.mark()
        SC = 1.0 / math.sqrt(32.0)

        def layer_norm_tile(pre, gb, bb, dst, tg):
            stt = AR_t["st"]; mv = AR_t["mv"]; rs = AR_t["rs"]
            P.dve(I("bn_stats", out=stt[:, 0:6], in_=pre[:, 0:512]), r=[tg], w=["lnst"])
            P.dve(I("bn_stats", out=stt[:, 6:12], in_=pre[:, 512:1024]), r=[tg], w=["lnst2"])
            P.dve(I("bn_aggr", out=mv, in_=stt), r=["lnst", "lnst2"], w=["lnmv"])
            P.act(I("activation", out=rs, in_=mv[:, 1:2], func=AF.Ln, bias=1e-5), r=["lnmv"], w=["lnrs"])
            P.act(I("activation", out=rs, in_=rs, func=AF.Exp, scale=-0.5), r=["lnrs"], w=["lnrs"])
            P.dve(I("tensor_scalar", out=dst, in0=pre, scalar1=mv[:, 0:1], scalar2=rs[:, 0:1], op0=ALU.subtract, op1=ALU.mult), r=[tg, "lnmv", "lnrs"], w=[tg + "o"])
            P.pool(I("tensor_tensor", out=dst, in0=dst, in1=gb, op=ALU.mult), r=[tg + "o", "lng"], w=[tg + "o"])
            P.pool(I("tensor_tensor", out=dst, in0=dst, in1=bb, op=ALU.add), r=[tg + "o", "lng"], w=[tg + "o"])
        AR_t = {}

        if "2" in phases:
            slopes = [2.0 ** (-(h + 1)) for h in range(8)]
            tri = AR.alloc([128, 128]); trib = AR.alloc([128, 128], BF16); U = AR.alloc([128, 128]); kl = AR.alloc([128, 1])
            P.pool(I("memset", tri, 1.0), w=["tri"])
            P.pool(I("affine_select", out=tri, in_=tri, pattern=[[1, 128]], compare_op=ALU.is_ge, fill=0.0, base=0, channel_multiplier=-1), r=["tri"], w=["tri"])
            P.dve(I("tensor_copy", out=trib, in_=tri), r=["tri"], w=["trib"])
            P.pool(I("memset", U, 1.0), w=["U"])
            P.pool(I("affine_select", out=U, in_=U, pattern=[[1, 128]], compare_op=ALU.is_gt, fill=0.0, base=0, channel_multiplier=-1), r=["U"], w=["U"])
            P.pe(I("matmul", bank(0)[:, 0:1], lhsT=U, rhs=ones[:, 0:1], start=True, stop=True), r=["U", "ones"], w=["pb0"])
            P.dve(I("tensor_copy", out=kl, in_=bank(0)[:, 0:1]), r=["pb0"], w=["kl"])
            ND = 68
            bt = AR.alloc([128, 8, ND]); btp = AR.alloc([128, 8, ND]); prefc = AR.alloc([128, 1])
            P.dma(I("dma_start", out=prefc, in_=pref), w=["prefc"])
            for h in range(8):
                for Dd in range(ND):
                    fn = P.dve if (Dd % 2 == 0) else P.pool
                    fn(I("tensor_scalar", out=bt[:, h, Dd:Dd + 1], in0=kl, scalar1=slopes[h], scalar2=-slopes[h] * 128.0 * Dd, op0=ALU.mult, op1=ALU.add), r=["kl"], w=["bt%d" % (Dd % 2)])
            P.dve(I("tensor_scalar", out=btp, in0=bt, scalar1=prefc[:, 0:1], scalar2=None, op0=ALU.add), r=["bt0", "bt1", "prefc"], w=["btp"])
            lv = [AR.alloc([64, 32]) for _ in range(4)]; lt = AR.alloc([64, 32]); l1 = AR.alloc([64, 1]); l2 = AR.alloc([64, 1]); neglam = AR.alloc([64, 1]); gcol = AR.alloc([64, 1])
            for i_, src in enumerate((lq1, lk1, lq2, lk2)):
                P.dma(I("dma_start", out=lv[i_], in_=src.to_broadcast([64, 32])), w=["lv%d" % i_])
            P.dve(I("tensor_tensor", out=lt, in0=lv[0], in1=lv[1], op=ALU.mult), r=["lv0", "lv1"], w=["lt"])
            P.dve(I("reduce_sum", out=l1, in_=lt, axis=AX.X), r=["lt"], w=["l1"])
            P.dve(I("tensor_tensor", out=lt, in0=lv[2], in1=lv[3], op=ALU.mult), r=["lv2", "lv3", "l1"], w=["lt"])
            P.dve(I("reduce_sum", out=l2, in_=lt, axis=AX.X), r=["lt"], w=["l2"])
            P.act(I("activation", out=l1, in_=l1, func=AF.Exp), r=["l1"], w=["l1"])
            P.act(I("activation", out=l2, in_=l2, func=AF.Exp), r=["l2"], w=["l2"])
            P.dve(I("tensor_tensor", out=neglam, in0=l2, in1=l1, op=ALU.subtract), r=["l1", "l2"], w=["neglam"])
            P.dve(I("tensor_scalar", out=neglam, in0=neglam, scalar1=-LAMBDA_INIT, scalar2=None, op0=ALU.add), r=["neglam"], w=["neglam"])
            P.dma(I("dma_start", out=gcol, in_=subln_g), w=["gcol"])
            P.dve(I("tensor_scalar", out=gcol, in0=gcol, scalar1=1.0 - LAMBDA_INIT, scalar2=None, op0=ALU.mult), r=["gcol"], w=["gcol"])
            sel = AR.alloc([65, 64])
            P.dve(I("memset", sel, 0.0), w=["sel"])
            P.dve(I("memset", sel[64:65, :], 1.0), r=["sel"], w=["sel"])
            KTh = AR.alloc([128, L], BF16); QTh = AR.alloc([128, LO], BF16); Vh = AR.alloc([128, 64, 130], BF16)
            Pt = [AR.alloc([128, 2, 512], BF16) for _ in range(2)]
            Osb = AR.alloc([65, 2, 512]); Od = AR.alloc([65, 2, 512]); rL = AR.alloc([64, 2, 512])
            a1 = AR.alloc([64, 512]); a2 = AR.alloc([64, 512]); dif = AR.alloc([64, 512]); sq2 = AR.alloc([64, 512]); rs2 = AR.alloc([64, 512])
            oT = AR.alloc([64, 512], BF16)
            scb = [bank(0, 2).rearrange("p (a b) -> p a b", a=2), bank(2, 2).rearrange("p (a b) -> p a b", a=2)]
            Ooff = bank(4, 2).rearrange("p (a b) -> p a b", a=2); Odg = bank(6, 2).rearrange("p (a b) -> p a b", a=2)
            it = 0
            for hp in range(4):
                P.dma(I("dma_start", out=KTh, in_=KT[hp]), r=["KT"], w=["KTh"])
                P.dma(I("dma_start", out=QTh, in_=QT[hp]), r=["QT"], w=["QTh"])
                P.dma(I("dma_start", out=Vh, in_=VV[:, :, hp * 130:(hp + 1) * 130].rearrange("b p c -> p b c")), r=["VV"], w=["Vh"])
                for j in range(8):
                    for hl in range(2):
                        h = 2 * hp + hl; sl = slopes[h]
                        q0 = LO + 512 * j; kd0 = q0 // 128; klo = _qkey_lo(q0, sl)
                        offs = list(range(klo, kd0))
                        for ii, kb in enumerate(offs):
                            sc = scb[it % 2]; sk = "sc%d" % (it % 2); pt = Pt[it % 2]; pk = "Pt%d" % (it % 2); it += 1
                            for m_ in range(2):
                                pr = 32 * (2 * hl + m_)
                                P.pe(I("matmul", sc[:, m_, :], lhsT=KTh[pr:pr + 32, kb * 128:(kb + 1) * 128], rhs=QTh[pr:pr + 32, j * 512:(j + 1) * 512], start=True, stop=True, tile_position=(pr, 0)), r=["KTh", "QTh"], w=[sk])
                            btab = btp if kb < 32 else bt
                            P.act(I("activation", out=pt, in_=sc, func=AF.Exp, bias=btab[:, h, kd0 - kb:kd0 - kb + 1], scale=SC), r=[sk, "btp", "bt0", "bt1"], w=[pk])
                            for m_ in range(2):
                                P.pe(I("matmul", Ooff[0:65, m_, :], lhsT=Vh[:, kb, hl * 65:(hl + 1) * 65], rhs=pt[:, m_, :], start=(ii == 0), stop=(ii == len(offs) - 1)), r=["Vh", pk], w=["Ooff"])
                        for r_ in range(4):
                            kb = kd0 + r_
                            sc = scb[it % 2]; sk = "sc%d" % (it % 2); pt = Pt[it % 2]; pk = "Pt%d" % (it % 2); it += 1
                            c0 = 128 * r_
                            for m_ in range(2):
                                pr = 32 * (2 * hl + m_)
                                P.pe(I("matmul", sc[:, m_, c0:512], lhsT=KTh[pr:pr + 32, kb * 128:(kb + 1) * 128], rhs=QTh[pr:pr + 32, j * 512 + c0:(j + 1) * 512], start=True, stop=True, tile_position=(pr, 0)), r=["KTh", "QTh"], w=[sk])
                            for qs in range(r_, 4):
                                P.act(I("activation", out=pt[:, :, 128 * qs:128 * qs + 128], in_=sc[:, :, 128 * qs:128 * qs + 128], func=AF.Exp, bias=bt[:, h, qs - r_:qs - r_ + 1], scale=SC), r=[sk, "bt0", "bt1"], w=[pk])
                            P.pool(I("tensor_tensor", out=pt[:, :, c0:c0 + 128], in0=pt[:, :, c0:c0 + 128], in1=trib.unsqueeze(1).to_broadcast([128, 2, 128]), op=ALU.mult), r=[pk, "trib"], w=[pk])
                            for m_ in range(2):
                                P.pe(I("matmul", Odg[0:65, m_, c0:512], lhsT=Vh[:, kb, hl * 65:(hl + 1) * 65], rhs=pt[:, m_, c0:512], start=(r_ == 0), stop=(r_ == 3)), r=["Vh", pk], w=["Odg"])
                        P.act(I("activation", out=Od, in_=Odg[0:65], func=AF.Copy), r=["Odg"], w=["Od"])
                        for qs in range(4):
                            f = math.exp(-sl * 128.0 * qs)
                            cs = slice(128 * qs, 128 * qs + 128)
                            P.dve(I("scalar_tensor_tensor", out=Osb[:, :, cs], in0=Ooff[0:65, :, cs], scalar=f, in1=Od[:, :, cs], op0=ALU.mult, op1=ALU.add), r=["Ooff", "Od"], w=["Osb"])
                        for m_ in range(2):
                            P.pe(I("matmul", Odg[0:64, m_, :], lhsT=sel, rhs=Osb[:, m_, :], start=True, stop=True), r=["sel", "Osb", "Od"], w=["Odg"])
                        P.dve(I("reciprocal", out=rL, in_=Odg[0:64]), r=["Odg"], w=["rL"])
                        P.pool(I("tensor_tensor", out=a1, in0=Osb[0:64, 0, :], in1=rL[:, 0, :], op=ALU.mult), r=["Osb", "rL"], w=["a1"])
                        P.pool(I("tensor_tensor", out=a2, in0=Osb[0:64, 1, :], in1=rL[:, 1, :], op=ALU.mult), r=["Osb", "rL"], w=["a2"])
                        P.dve(I("scalar_tensor_tensor", out=dif, in0=a2, scalar=neglam[:, 0:1], in1=a1, op0=ALU.mult, op1=ALU.add), r=["a1", "a2", "neglam"], w=["dif"])
                        P.pool(I("tensor_tensor", out=sq2, in0=dif, in1=dif, op=ALU.mult), r=["dif"], w=["sq2"])
                        P.pe(I("matmul", Odg[0:64, 0, :], lhsT=ones[0:64, 0:64], rhs=sq2, start=True, stop=True), r=["ones", "sq2", "rL"], w=["Odg"])
                        P.act(I("activation", out=rs2, in_=Odg[0:64, 0, :], func=AF.Ln, scale=1.0 / 64.0, bias=1e-5), r=["Odg"], w=["rs2"])
                        P.act(I("activation", out=rs2, in_=rs2, func=AF.Exp, scale=-0.5), r=["rs2"], w=["rs2"])
                        P.dve(I("scalar_tensor_tensor", out=oT, in0=dif, scalar=gcol[:, 0:1], in1=rs2, op0=ALU.mult, op1=ALU.mult), r=["dif", "gcol", "rs2"], w=["oT"])
                        P.dma(I("dma_start", out=MIXT[h // 2, (h % 2) * 64:(h % 2) * 64 + 64, j * 512:(j + 1) * 512], in_=oT), r=["oT"], w=["MIXT"])
            P.barrier()
            AR.reset(gmark)

        def bcast_row(dst, src, n, key):
            P.dma(I("dma_start", out=dst, in_=src.to_broadcast([128, n])), w=[key])

        if "3" in phases:
            if os.environ.get('KZERO'):
                zt = AR.alloc([128, 8, 512], BF16)
                P.dve(I("memset", zt, 0.0), w=["zt"])
                for cc in range(8):
                    P.dma(I("dma_start", out=MIXT[:, :, cc * 512:(cc + 1) * 512].rearrange("k p t -> p k t"), in_=zt), r=["zt"], w=["MIXT"])
            wout = AR.alloc([128, 8, 1024], BF16)
            for kt in range(8):
                P.dma(I("dma_start", out=wout[:, kt, :], in_=w_out[kt * 128:(kt + 1) * 128, :]), w=["wout"], q="gpsimd")
            wr = AR.alloc([128, 8, 32])
            P.dma(I("dma_start", out=wr, in_=w_router.rearrange("(k p) e -> p k e", p=128)), w=["wr"])
            g1b = AR.alloc([128, 1024]); b1b = AR.alloc([128, 1024]); brb = AR.alloc([128, 32])
            bcast_row(g1b, ln1_g, 1024, "lng"); bcast_row(b1b, ln1_b, 1024, "lng"); bcast_row(brb, b_router, 32, "brb")
            AR_t["st"] = AR.alloc([128, 12]); AR_t["mv"] = AR.alloc([128, 2]); AR_t["rs"] = AR.alloc([128, 1])
            mixT = [AR.alloc([128, 8, 128], BF16) for _ in range(2)]
            xt_ = [AR.alloc([128, 1024]) for _ in range(2)]
            pre = AR.alloc([128, 1024]); x1 = [AR.alloc([128, 1024]) for _ in range(2)]
            x1Tf = AR.alloc([128, 8, 128]); x1Tb = [AR.alloc([128, 8, 128], BF16) for _ in range(2)]
            lg = AR.alloc([128, 32]); mx8 = AR.alloc([128, 8]); msk = AR.alloc([128, 32]); ex = AR.alloc([128, 32]); nmx = AR.alloc([128, 1]); ssum = AR.alloc([128, 1])
            CUT = int(os.environ.get('KCUT', '99'))
            for t in range(int(os.environ.get('KNT3', '32'))):
                b_ = t % 2
                P.dma(I("dma_start", out=mixT[b_], in_=MIXT[:, :, t * 128:(t + 1) * 128].rearrange("k p t -> p k t")), r=["MIXT"], w=["mixT%d" % b_])
                P.dma(I("dma_start", out=xt_[b_], in_=xc[LO + t * 128:LO + (t + 1) * 128, :]), w=["xt%d" % b_])
                if CUT < 2: continue
                pm = bank(0, 2)
                for dh in range(2):
                    for kt in range(8):
                        P.pe(I("matmul", pm[:, dh * 512:(dh + 1) * 512], lhsT=mixT[b_][:, kt, :], rhs=wout[:, kt, dh * 512:(dh + 1) * 512], start=(kt == 0), stop=(kt == 7)), r=["mixT%d" % b_, "wout"], w=["pm"])
                P.dve(I("scalar_tensor_tensor", out=pre, in0=xt_[b_], scalar=ALPHA, in1=pm, op0=ALU.mult, op1=ALU.add), r=["xt%d" % b_, "pm"], w=["pre"])
                if CUT < 3: continue
                layer_norm_tile(pre, g1b, b1b, x1[b_], "pre")
                if CUT < 4: continue
                P.dma(I("dma_start", out=X1[t * 128:(t + 1) * 128, :], in_=x1[b_]), r=["preo"], w=["X1"])
                if CUT < 5: continue
                ptr = bank(2, 2).rearrange("p (a b) -> p a b", a=8)
                for kt in range(8):
                    P.pe(I("transpose", out=ptr[:, kt, :], in_=x1[b_][:, kt * 128:(kt + 1) * 128], identity=ident), r=["preo", "ident"], w=["ptr"])
                KS = os.environ.get('KSUB', 'abc')
                if 'a' in KS:
                    P.act(I("activation", out=x1Tf, in_=ptr, func=AF.Copy), r=["ptr"], w=["x1Tf"])
                if 'b' in KS:
                    P.act(I("activation", out=x1Tb[b_], in_=ptr, func=AF.Copy), r=["ptr"], w=["x1Tb%d" % b_])
                if 'c' in KS:
                    P.dma(I("dma_start", out=X1T[:, :, t * 128:(t + 1) * 128].rearrange("k p t -> p k t"), in_=x1Tb[b_]), r=["x1Tb%d" % b_], w=["X1T"])
                if CUT < 6: continue
                pl = bank(4)[:, 0:32]
                for kt in range(8):
                    P.pe(I("matmul", pl, lhsT=x1Tf[:, kt, :], rhs=wr[:, kt, :], start=(kt == 0), stop=(kt == 7)), r=["x1Tf", "wr"], w=["pl"])
                if CUT < 7: continue
                P.dve(I("tensor_tensor", out=lg, in0=pl, in1=brb, op=ALU.add), r=["pl", "brb"], w=["lg"])
                P.dve(I("max", out=mx8, in_=lg), r=["lg"], w=["mx8"])
                P.dve(I("tensor_scalar", out=msk, in0=lg, scalar1=mx8[:, 3:4], scalar2=1e-7, op0=ALU.subtract, op1=ALU.add), r=["lg", "mx8"], w=["msk"])
                P.dve(I("tensor_scalar", out=msk, in0=msk, scalar1=1e10, scalar2=0.0, op0=ALU.mult, op1=ALU.max), r=["msk"], w=["msk"])
                P.dve(I("tensor_scalar", out=msk, in0=msk, scalar1=1.0, scalar2=None, op0=ALU.min), r=["msk"], w=["msk"])
                P.dve(I("tensor_scalar", out=nmx, in0=mx8[:, 0:1], scalar1=-1.0, scalar2=None, op0=ALU.mult), r=["mx8"], w=["nmx"])
                P.act(I("activation", out=ex, in_=lg, func=AF.Exp, bias=nmx[:, 0:1]), r=["lg", "nmx"], w=["ex"])
                P.dve(I("tensor_tensor", out=ex, in0=ex, in1=msk, op=ALU.mult), r=["ex", "msk"], w=["ex"])
                P.dve(I("reduce_sum", out=ssum, in_=ex, axis=AX.X), r=["ex"], w=["ssum"])
                P.dve(I("reciprocal", out=ssum, in_=ssum), r=["ssum"], w=["ssum"])
                P.dve(I("tensor_scalar", out=G[:, t, :], in0=ex, scalar1=ssum[:, 0:1], scalar2=None, op0=ALU.mult), r=["ex", "ssum"], w=["G"])
            P.barrier()
            AR.reset(gmark)

        if "4" in phases:
            Wgu = [AR.alloc([128, 8, 2, 1024], BF16) for _ in range(2)]
            Wd = [AR.alloc([128, 8, 1024], BF16) for _ in range(2)]
            stage = [AR.alloc([128, 2048]) for _ in range(2)]
            X1Tc = AR.alloc([128, 8, 1024], BF16); acc = AR.alloc([128, 8, 1024]); actT = AR.alloc([128, 8, 512], BF16)
            tgs = [AR.alloc([128, 512]) for _ in range(2)]; tsgs = [AR.alloc([128, 512]) for _ in range(2)]; tls = [AR.alloc([128, 512]) for _ in range(2)]
            BGU = AR.alloc([128, 8, 2, 32]); bd = AR.alloc([32, 1024]); GTc = AR.alloc([32, 8, 128])
            bgn = stage[0][0:32, :]
            P.dma(I("dma_start", out=bgn, in_=b_gu), w=["stage0"])
            bgv = bgn.rearrange("e (ft p two) -> e ft two p", p=128, two=2)
            pbg = bank(7).rearrange("p (a b c) -> p a b c", a=8, b=2)
            for ft in range(8):
                for two in range(2):
                    P.pe(I("transpose", out=pbg[:, ft, two, :], in_=bgv[:, ft, two, :], identity=ident[0:32, 0:32]), r=["stage0", "ident"], w=["B7"])
            P.dve(I("tensor_copy", out=BGU, in_=pbg), r=["B7"], w=["BGU"])
            P.dma(I("dma_start", out=bd, in_=b_dn), w=["bd"])
            stn = [0]

            def load_expert(e):
                b_ = e % 2
                for kt in range(8):
                    sb_ = stn[0] % 2; stn[0] += 1
                    P.dma(I("dma_start", out=stage[sb_], in_=w_gu[e, kt * 128:(kt + 1) * 128, :]), w=["stage%d" % sb_])
                    src = stage[sb_].rearrange("p (f two) -> p two f", two=2)
                    if kt % 2 == 0:
                        P.act(I("activation", out=Wgu[b_][:, kt, :, :], in_=src, func=AF.Copy), r=["stage%d" % sb_], w=["Wgu%d" % b_])
                    else:
                        P.pool(I("tensor_copy", out=Wgu[b_][:, kt, :, :], in_=src), r=["stage%d" % sb_], w=["Wgu%d" % b_])
                for k2 in range(4):
                    sb_ = stn[0] % 2; stn[0] += 1
                    P.dma(I("dma_start", out=stage[sb_].rearrange("p (k f) -> p k f", k=2), in_=w_dn[e, k2 * 256:(k2 + 1) * 256, :].rearrange("(k p) f -> p k f", p=128)), w=["stage%d" % sb_])
                    src = stage[sb_].rearrange("p (k f) -> p k f", k=2)
                    if k2 % 2 == 0:
                        P.act(I("activation", out=Wd[b_][:, 2 * k2:2 * k2 + 2, :], in_=src, func=AF.Copy), r=["stage%d" % sb_], w=["Wd%d" % b_])
                    else:
                        P.pool(I("tensor_copy", out=Wd[b_][:, 2 * k2:2 * k2 + 2, :], in_=src), r=["stage%d" % sb_], w=["Wd%d" % b_])

            for c in range(NCH):
                P.dma(I("dma_start", out=X1Tc, in_=X1T[:, :, c * 1024:(c + 1) * 1024].rearrange("k p t -> p k t")), r=["X1T"], w=["X1Tc"])
                pg = bank(6)[0:32, :].rearrange("p (a b) -> p a b", a=4)
                for half in range(2):
                    for i_ in range(4):
                        tl_ = half * 4 + i_
                        P.pe(I("transpose", out=pg[:, i_, :], in_=G[:, c * 8 + tl_, :], identity=ident), r=["G", "ident"], w=["pg"])
                    P.dve(I("tensor_copy", out=GTc[:, half * 4:half * 4 + 4, :], in_=pg), r=["pg"], w=["GTc"])
                for tl_ in range(8):
                    pa = bank(0, 2)
                    for dh in range(2):
                        P.pe(I("matmul", pa[:, dh * 512:(dh + 1) * 512], lhsT=GTc[:, tl_, :], rhs=bd[:, dh * 512:(dh + 1) * 512], start=True, stop=True), r=["GTc", "bd"], w=["pa"])
                    P.dve(I("tensor_copy", out=acc[:, tl_, :], in_=pa), r=["pa"], w=["acc"])
                load_expert(0)
                for e in range(NE):
                    if e + 1 < NE:
                        load_expert(e + 1)
                    b_ = e % 2
                    for tc in range(2):
                        for ft in range(8):
                            pgl = bank((ft % 2) * 2); pln = bank((ft % 2) * 2 + 1); gk = "pgl%d" % (ft % 2)
                            tg = tgs[ft % 2]; tsg = tsgs[ft % 2]; tl = tls[ft % 2]; kg = "tg%d" % (ft % 2); ksg = "tsg%d" % (ft % 2); kl_ = "tl%d" % (ft % 2)
                            for kt in range(8):
                                P.pe(I("matmul", pgl, lhsT=Wgu[b_][:, kt, 0, ft * 128:(ft + 1) * 128], rhs=X1Tc[:, kt, tc * 512:(tc + 1) * 512], start=(kt == 0), stop=(kt == 7)), r=["Wgu%d" % b_, "X1Tc"], w=[gk])
                            for kt in range(8):
                                P.pe(I("matmul", pln, lhsT=Wgu[b_][:, kt, 1, ft * 128:(ft + 1) * 128], rhs=X1Tc[:, kt, tc * 512:(tc + 1) * 512], start=(kt == 0), stop=(kt == 7)), r=["Wgu%d" % b_, "X1Tc"], w=[gk + "l"])
                            P.dve(I("tensor_scalar", out=tg, in0=pgl, scalar1=BGU[:, ft, 0, e:e + 1], scalar2=7.0, op0=ALU.add, op1=ALU.min), r=[gk, "BGU"], w=[kg])
                            P.act(I("activation", out=tsg, in_=tg, func=AF.Sigmoid, scale=1.702), r=[kg], w=[ksg])
                            P.dve(I("tensor_scalar", out=tl, in0=pln, scalar1=BGU[:, ft, 1, e:e + 1], scalar2=7.0, op0=ALU.add, op1=ALU.min), r=[gk + "l", "BGU"], w=[kl_])
                            P.pool(I("tensor_scalar", out=tl, in0=tl, scalar1=-7.0, scalar2=1.0, op0=ALU.max, op1=ALU.add), r=[kl_], w=[kl_])
                            P.pool(I("tensor_tensor", out=tg, in0=tg, in1=tsg, op=ALU.mult), r=[kg, ksg], w=[kg])
                            P.pool(I("tensor_tensor", out=actT[:, ft, :], in0=tg, in1=tl, op=ALU.mult), r=[kg, kl_], w=["actT"])
                        for ti in range(4):
                            tl_ = tc * 4 + ti
                            for dh in range(2):
                                pdn = bank(4 + (ti * 2 + dh) % 4); dk = "pdn%d" % ((ti * 2 + dh) % 4)
                                for ft in range(8):
                                    P.pe(I("matmul", pdn, lhsT=actT[:, ft, ti * 128:(ti + 1) * 128], rhs=Wd[b_][:, ft, dh * 512:(dh + 1) * 512], start=(ft == 0), stop=(ft == 7)), r=["actT", "Wd%d" % b_], w=[dk])
                                P.dve(I("scalar_tensor_tensor", out=acc[:, tl_, dh * 512:(dh + 1) * 512], in0=pdn, scalar=G[:, c * 8 + tl_, e:e + 1], in1=acc[:, tl_, dh * 512:(dh + 1) * 512], op0=ALU.mult, op1=ALU.add), r=[dk, "G", "acc"], w=["acc"])
                for tl_ in range(8):
                    t = c * 8 + tl_
                    sb_ = stn[0] % 2; stn[0] += 1
                    xs = stage[sb_][:, 0:1024]
                    P.dma(I("dma_start", out=xs, in_=X1[t * 128:(t + 1) * 128, :]), r=["X1"], w=["stage%d" % sb_])
                    P.dve(I("scalar_tensor_tensor", out=xs, in0=xs, scalar=ALPHA, in1=acc[:, tl_, :], op0=ALU.mult, op1=ALU.add), r=["stage%d" % sb_, "acc"], w=["stage%d" % sb_])
                    P.dma(I("dma_start", out=RR[t * 128:(t + 1) * 128, :], in_=xs), r=["stage%d" % sb_], w=["RR"])
            P.barrier()
            AR.reset(gmark)

        if "5" in phases:
            wpg = AR.alloc([128, 8, 1024], BF16); wpp = AR.alloc([128, 2, 1024], BF16)
            for kt in range(8):
                P.dma(I("dma_start", out=wpg[:, kt, :], in_=w_pg[kt * 128:(kt + 1) * 128, :]), w=["wpg"], q="gpsimd")
            for kt in range(2):
                P.dma(I("dma_start", out=wpp[:, kt, :], in_=w_pp[kt * 128:(kt + 1) * 128, :]), w=["wpp"], q="gpsimd")
            g2b = AR.alloc([128, 1024]); b2b = AR.alloc([128, 1024])
            bcast_row(g2b, ln2_g, 1024, "lng"); bcast_row(b2b, ln2_b, 1024, "lng")
            AR_t["st"] = AR.alloc([128, 12]); AR_t["mv"] = AR.alloc([128, 2]); AR_t["rs"] = AR.alloc([128, 1])
            rt = [AR.alloc([128, 1024]) for _ in range(2)]; ptl = [AR.alloc([128, 256]) for _ in range(2)]
            rT = AR.alloc([128, 8, 128], BF16); pT = AR.alloc([128, 2, 128], BF16)
            sgt = AR.alloc([128, 1024]); yv = AR.alloc([128, 1024]); yo = [AR.alloc([128, 1024]) for _ in range(2)]
            for t in range(32):
                b_ = t % 2
                P.dma(I("dma_start", out=rt[b_], in_=RR[t * 128:(t + 1) * 128, :]), r=["RR"], w=["rt%d" % b_])
                P.dma(I("dma_start", out=ptl[b_], in_=pc[t * 128:(t + 1) * 128, :]), w=["ptl%d" % b_])
                ptr = bank(4, 2).rearrange("p (a b) -> p a b", a=8)
                for kt in range(8):
                    P.pe(I("transpose", out=ptr[:, kt, :], in_=rt[b_][:, kt * 128:(kt + 1) * 128], identity=ident), r=["rt%d" % b_, "ident"], w=["ptr5"])
                P.act(I("activation", out=rT, in_=ptr, func=AF.Copy), r=["ptr5"], w=["rT"])
                ptp = bank(6).rearrange("p (a b) -> p a b", a=4)
                for kt in range(2):
                    P.pe(I("transpose", out=ptp[:, kt, :], in_=ptl[b_][:, kt * 128:(kt + 1) * 128], identity=ident), r=["ptl%d" % b_, "ident"], w=["ptp"])
                P.dve(I("tensor_copy", out=pT, in_=ptp[:, 0:2, :]), r=["ptp"], w=["pT"])
                pgt = bank(0, 2); ppp = bank(2, 2)
                for dh in range(2):
                    for kt in range(8):
                        P.pe(I("matmul", pgt[:, dh * 512:(dh + 1) * 512], lhsT=rT[:, kt, :], rhs=wpg[:, kt, dh * 512:(dh + 1) * 512], start=(kt == 0), stop=(kt == 7)), r=["rT", "wpg"], w=["pgt"])
                    for kt in range(2):
                        P.pe(I("matmul", ppp[:, dh * 512:(dh + 1) * 512], lhsT=pT[:, kt, :], rhs=wpp[:, kt, dh * 512:(dh + 1) * 512], start=(kt == 0), stop=(kt == 1)), r=["pT", "wpp"], w=["ppp"])
                P.act(I("activation", out=sgt, in_=pgt, func=AF.Sigmoid), r=["pgt"], w=["sgt"])
                P.dve(I("tensor_tensor", out=sgt, in0=sgt, in1=ppp, op=ALU.mult), r=["sgt", "ppp"], w=["sgt"])
                P.pool(I("tensor_tensor", out=yv, in0=sgt, in1=rt[b_], op=ALU.add), r=["sgt", "rt%d" % b_], w=["yv"])
                layer_norm_tile(yv, g2b, b2b, yo[b_], "yv")
                P.dma(I("dma_start", out=out[t * 128:(t + 1) * 128, :], in_=yo[b_]), r=["yvo"], w=["out"])
            P.barrier()

        P.emit(st)
    return nc


_NC_CACHE = {}


def _prep_inputs(inputs):
    sq = lambda k: np.ascontiguousarray(np.asarray(inputs[k])[0])
    x = np.asarray(inputs["x"]); p = np.asarray(inputs["p"])[0]
    shared = {
        "w_in": sq("w_in"), "lambda_q1": np.asarray(inputs["lambda_q1"]), "lambda_k1": np.asarray(inputs["lambda_k1"]),
        "lambda_q2": np.asarray(inputs["lambda_q2"]), "lambda_k2": np.asarray(inputs["lambda_k2"]),
        "subln_g": np.ascontiguousarray(np.asarray(inputs["subln_g"]).reshape(64, 1)),
        "ssm_a_re": sq("ssm_a_re"), "ssm_a_im": sq("ssm_a_im"), "ssm_log_dt": np.asarray(inputs["ssm_log_dt"]),
        "ssm_b_re": sq("ssm_b_re"), "ssm_b_im": sq("ssm_b_im"), "ssm_c_re": sq("ssm_c_re"), "ssm_c_im": sq("ssm_c_im"),
        "ssm_d": sq("ssm_d"), "w_glu": sq("w_glu"), "ssm_norm_g": sq("ssm_norm_g"), "w_out": sq("w_out"),
        "ln1_g": np.asarray(inputs["ln1_g"]), "ln1_b": np.asarray(inputs["ln1_b"]),
        "w_router": sq("w_router"), "b_router": np.asarray(inputs["b_router"]),
        "w_gate_up": sq("w_gate_up")[:NE], "b_gate_up": sq("b_gate_up"), "w_down": sq("w_down")[:NE], "b_down": sq("b_down"),
        "w_ple_gate": sq("w_ple_gate"), "w_ple_proj": sq("w_ple_proj"),
        "ln2_g": np.asarray(inputs["ln2_g"]), "ln2_b": np.asarray(inputs["ln2_b"]),
    }
    shared = {k: np.ascontiguousarray(v, dtype=np.float32) for k, v in shared.items()}
    maps = []
    for c in range(8):
        b, h = c // 2, c % 2
        if h == 0:
            xcore = np.concatenate([np.zeros((LO, DM), np.float32), x[b, :LO]], axis=0)
        else:
            xcore = np.ascontiguousarray(x[b])
        m = dict(shared)
        m["xc"] = np.ascontiguousarray(xcore, dtype=np.float32)
        m["pc"] = np.ascontiguousarray(p[b, h * LO:(h + 1) * LO], dtype=np.float32)
        m["pref"] = np.full((128, 1), NEG if h == 0 else 0.0, np.float32)
        maps.append(m)
    return maps


def kernel(**inputs):
    dbg = KDEBUG
    if dbg not in _NC_CACHE:
        _NC_CACHE[dbg] = build_program(dbg)
    nc = _NC_CACHE[dbg]
    maps = _prep_inputs(inputs)
    res = run_bass_kernel_spmd(nc, maps, core_ids=list(range(8)))
    if dbg:
        return res.results
    outp = np.zeros((4, L, DM), np.float32)
    for c in range(8):
        b, h = c // 2, c % 2
        outp[b, h * LO:(h + 1) * LO] = res.results[c]["out"]
    return outp
```

```python
import math
import os
from contextlib import ExitStack

import numpy as np
import concourse.bass as bass
import concourse.mybir as mybir
from concourse.bass_utils import run_bass_kernel_spmd

F32 = mybir.dt.float32
BF16 = mybir.dt.bfloat16
AF = mybir.ActivationFunctionType
ALU = mybir.AluOpType
AX = mybir.AxisListType

ENGINES = ("tensor", "vector", "scalar", "gpsimd", "sync")
SAME_ENGINE_SYNC = True
KDEBUG = os.environ.get("KDEBUG", "")

L = 8192
LO = 4096
DM = 1024
ALPHA = 2.0 ** 0.25
LAMBDA_INIT = 0.2
ATT_TH = 64.0
NEG = -30000.0
NE = int(os.environ.get('KNE', '32'))
NCH = int(os.environ.get('KNC', '4'))


KEYMAP = {
    "pb0": ["B0"], "pb1": ["B1"], "pbC": ["B2"], "pbu_re": ["B2"], "pbu_im": ["B3"], "py0": ["B4"], "py1": ["B5"], "pms": ["B6"],
    "sc0": ["B0", "B1"], "sc1": ["B2", "B3"], "Ooff": ["B4", "B5"], "Odg": ["B6", "B7"],
    "pm": ["B0", "B1"], "ptr": ["B2", "B3"], "pl": ["B4"],
    "pa": ["B0", "B1"], "pg": ["B6"], "pgl0": ["B0"], "pgl0l": ["B1"], "pgl1": ["B2"], "pgl1l": ["B3"],
    "pdn0": ["B4"], "pdn1": ["B5"], "pdn2": ["B6"], "pdn3": ["B7"],
    "ptr5": ["B4", "B5"], "ptp": ["B6"], "pgt": ["B0", "B1"], "ppp": ["B2", "B3"],
}


def _mapkeys(ks):
    out = []
    for k in ks:
        out.extend(KEYMAP.get(k, [k]))
    return out


class Prog:
    def __init__(self, nc, n_dma_sems=24):
        self.nc = nc
        self.ops = []
        self.last_w = {}
        self.readers = {}
        self.n_dma_sems = n_dma_sems
        self.last_of = {e: None for e in ENGINES}
        self.recent_dma = {e: [] for e in ENGINES}
        self.pe_sync = False

    def op(self, eng, fn, r=(), w=(), dma=False, extra=()):
        idx = len(self.ops)
        deps = set(extra)
        r = _mapkeys(r); w = _mapkeys(w)
        for k in r:
            if k in self.last_w:
                deps.add(self.last_w[k])
        for k in w:
            if k in self.last_w:
                deps.add(self.last_w[k])
            for x in self.readers.get(k, ()):
                deps.add(x)
        for k in w:
            self.last_w[k] = idx
            self.readers[k] = []
        for k in r:
            if k not in w:
                self.readers.setdefault(k, []).append(idx)
        deps.discard(idx)
        self.ops.append(dict(eng=eng, fn=fn, deps=deps, dma=dma, idx=idx, pesync=self.pe_sync))
        self.last_of[eng] = idx
        if dma:
            self.recent_dma[eng].append(idx)
            self.recent_dma[eng] = self.recent_dma[eng][-self.n_dma_sems:]
        return idx

    def pe(self, fn, r=(), w=(), tiled=False):
        extra = ()
        sv = self.pe_sync
        if tiled:
            self.pe_sync = True
            self.pe_was_tiled = True
        elif getattr(self, "pe_was_tiled", False):
            extra = (self.last_of["tensor"],)
            self.pe_sync = True
            self.pe_was_tiled = False
        i = self.op("tensor", fn, r, w, extra=extra)
        self.pe_sync = sv
        return i
    def dve(self, fn, r=(), w=()): return self.op("vector", fn, r, w)
    def act(self, fn, r=(), w=()): return self.op("scalar", fn, r, w)
    def pool(self, fn, r=(), w=()): return self.op("gpsimd", fn, r, w)
    def dma(self, fn, r=(), w=(), q="sync"): return self.op(q, fn, r, w, dma=True)

    def barrier(self):
        deps = set()
        for e in ENGINES:
            if self.last_of[e] is not None:
                deps.add(self.last_of[e])
            deps.update(self.recent_dma[e])
        for e in ENGINES:
            self.op(e, None, extra=tuple(deps))
        self.last_w = {}
        self.readers = {}

    def emit(self, stack):
        nc = self.nc
        ops = self.ops
        needed = set()
        for o in ops:
            for d in o["deps"]:
                if o["eng"] == "tensor" and ops[d]["eng"] == "tensor" and o["fn"] is not None and not o["pesync"]:
                    continue
                needed.add(d)
        for e in ENGINES:
            if self.last_of[e] is not None:
                needed.add(self.last_of[e])
        csem = {e: stack.enter_context(nc.semaphore(f"c_{e}")) for e in ENGINES}
        ccount = {e: 0 for e in ENGINES}
        dsems, dcount = {}, {}
        dnext = {e: 0 for e in ENGINES}
        for e in ("sync", "gpsimd", "scalar"):
            dsems[e] = [stack.enter_context(nc.semaphore(f"d_{e}_{i}")) for i in range(self.n_dma_sems)]
            dcount[e] = [0] * self.n_dma_sems
        for o in ops:
            e = o["eng"]
            if o["fn"] is None:
                o["sig"] = None
            elif o["dma"]:
                j = dnext[e]
                dnext[e] = (j + 1) % self.n_dma_sems
                o["dma_prev"] = (dsems[e][j], dcount[e][j])
                dcount[e][j] += 16
                o["sig"] = (dsems[e][j], dcount[e][j])
            elif o["idx"] in needed:
                ccount[e] += 1
                o["sig"] = (csem[e], ccount[e])
            else:
                o["sig"] = None
        def resolve(d, seen):
            od = ops[d]
            if od["fn"] is not None:
                return [d]
            out = []
            for dd in od["deps"]:
                if dd not in seen:
                    seen.add(dd)
                    out.extend(resolve(dd, seen))
            return out
        waited = {e: {} for e in ENGINES}
        per_eng = {e: [o for o in ops if o["eng"] == e] for e in ENGINES}
        block = stack.enter_context(nc.Block())

        def make(e):
            def body(eng):
                wd = waited[e]

                def wait(sem, val):
                    if val <= 0:
                        return
                    key = id(sem)
                    if wd.get(key, 0) >= val:
                        return
                    eng.wait_ge(sem, val)
                    wd[key] = val
                for o in per_eng[e]:
                    alld = []
                    seen = set()
                    for d in sorted(o["deps"]):
                        alld.extend(resolve(d, seen))
                    for d in sorted(set(alld)):
                        od = ops[d]
                        if od["sig"] is None:
                            continue
                        if (not od["dma"]) and od["eng"] == e and (not SAME_ENGINE_SYNC or (e == "tensor" and not o["pesync"])):
                            continue
                        wait(*od["sig"])
                    if o["fn"] is None:
                        continue
                    if o["dma"]:
                        wait(*o["dma_prev"])
                    ins = o["fn"](eng)
                    if o["sig"] is not None:
                        ins.then_inc(o["sig"][0], 16 if o["dma"] else 1)
                if e == "sync":
                    for q in dsems:
                        for j in range(self.n_dma_sems):
                            wait(dsems[q][j], dcount[q][j])
                    for e2 in ENGINES:
                        if e2 != "sync":
                            wait(csem[e2], ccount[e2])
            return body

        block.tensor(make("tensor"))
        block.vector(make("vector"))
        block.scalar(make("scalar"))
        block.gpsimd(make("gpsimd"))
        block.sync(make("sync"))


def I(name, *a, **k):
    return lambda e: getattr(e, name)(*a, **k)


class Arena:
    def __init__(self, ap_f32, nbytes):
        self.ap = ap_f32
        self.n = nbytes
        self.off = 0

    def mark(self): return self.off
    def reset(self, m): self.off = m

    def alloc(self, shape, dt=F32, parts=128):
        esz = 4 if dt == F32 else 2
        n = int(np.prod(shape[1:])) * esz
        n4 = (n + 3) // 4
        assert self.off + n4 * 4 <= self.n, f"arena overflow {self.off + n4 * 4} > {self.n}"
        a = self.ap[0:shape[0], self.off // 4:self.off // 4 + n4]
        self.off += n4 * 4
        if dt != F32:
            a = a.bitcast(dt)
        if len(shape) == 3:
            a = a.rearrange("p (a b) -> p a b", a=shape[1])
        elif len(shape) == 4:
            a = a.rearrange("p (a b c) -> p a b c", a=shape[1], b=shape[2])
        return a


def _qkey_lo(q0, slope):
    kmin = q0 - ATT_TH / slope
    return max(0, int(math.floor(kmin / 128.0)))


def build_program(dbg=""):
    nc = bass.Bass("TRN2", target_bir_lowering=False)
    D = {}

    def din(name, shape, dt=F32):
        D[name] = nc.dram_tensor(name, list(shape), dt, kind="ExternalInput").ap()
        return D[name]

    def dscr(name, shape, dt):
        kind = "ExternalOutput" if name in dbg.split(",") else "Internal"
        D[name] = nc.dram_tensor(name, list(shape), dt, kind=kind).ap()
        return D[name]

    xc = din("xc", [L, DM]); pc = din("pc", [LO, 256]); pref = din("pref", [128, 1])
    w_in = din("w_in", [1024, 2048])
    lq1 = din("lambda_q1", [1, 32]); lk1 = din("lambda_k1", [1, 32]); lq2 = din("lambda_q2", [1, 32]); lk2 = din("lambda_k2", [1, 32])
    subln_g = din("subln_g", [64, 1])
    a_re = din("ssm_a_re", [32, 64]); a_im = din("ssm_a_im", [32, 64]); log_dt = din("ssm_log_dt", [1, 32])
    b_re = din("ssm_b_re", [32, 64, 16]); b_im = din("ssm_b_im", [32, 64, 16])
    c_re = din("ssm_c_re", [32, 16, 64]); c_im = din("ssm_c_im", [32, 16, 64])
    ssm_d = din("ssm_d", [512]); w_glu = din("w_glu", [512, 1024]); ssm_norm_g = din("ssm_norm_g", [512])
    w_out = din("w_out", [1024, 1024]); ln1_g = din("ln1_g", [1, 1024]); ln1_b = din("ln1_b", [1, 1024])
    w_router = din("w_router", [1024, 32]); b_router = din("b_router", [1, 32])
    phases = os.environ.get("KPHASES", "12345")
    if "4" in phases:
        w_gu = din("w_gate_up", [NE, 1024, 2048]); b_gu = din("b_gate_up", [32, 2048])
        w_dn = din("w_down", [NE, 1024, 1024]); b_dn = din("b_down", [32, 1024])
    w_pg = din("w_ple_gate", [1024, 1024]); w_pp = din("w_ple_proj", [256, 1024])
    ln2_g = din("ln2_g", [1, 1024]); ln2_b = din("ln2_b", [1, 1024])
    out = nc.dram_tensor("out", [LO, DM], F32, kind="ExternalOutput").ap()

    KT = dscr("KT", [4, 128, L], BF16); QT = dscr("QT", [4, 128, LO], BF16)
    VV = dscr("VV", [64, 128, 520], BF16)
    MIXT = dscr("MIXT", [8, 128, LO], BF16)
    X1 = dscr("X1", [LO, DM], F32); X1T = dscr("X1T", [8, 128, LO], BF16)
    RR = dscr("RR", [LO, DM], F32)

    with ExitStack() as st:
        st.enter_context(nc.allow_non_contiguous_dma(reason="layout"))
        ARENA_B = 200 * 1024
        arena_t = st.enter_context(nc.sbuf_tensor("arena", [128, ARENA_B // 4], F32))
        psum_t = st.enter_context(nc.psum_tensor("psum", [128, 4096], F32))
        AR = Arena(arena_t, ARENA_B)
        P = Prog(nc)

        def bank(i, n=1):
            return psum_t[:, 512 * i:512 * (i + n)]

        ident = AR.alloc([128, 128]); ones = AR.alloc([128, 128])
        P.pool(I("memset", ident, 1.0), w=["ident"])
        P.pool(I("affine_select", out=ident, in_=ident, pattern=[[-1, 128]], compare_op=ALU.is_equal,
                                         fill=0.0, base=0, channel_multiplier=1), r=["ident"], w=["ident"])
        P.pool(I("memset", ones, 1.0), w=["ones"])
        gmark = AR.mark()

        if "1" in phases:
            P.pe_sync = False
            win = AR.alloc([128, 8, 2048], BF16)
            for kt in range(8):
                P.dma(I("dma_start", out=win[:, kt, :], in_=w_in[kt * 128:(kt + 1) * 128, :]), w=["win"], q="gpsimd")
            wglu = AR.alloc([128, 4, 1024], BF16)
            for kt in range(4):
                P.dma(I("dma_start", out=wglu[:, kt, :], in_=w_glu[kt * 128:(kt + 1) * 128, :]), w=["wglu"], q="gpsimd")
            sm = lambda: AR.alloc([128, 16])
            are, aim, dtt, rho, th, cth, sth, Are, Aim, t0, t1, t2, Fre, Fim, d2 = [sm() for _ in range(15)]
            P.dma(I("dma_start", out=are, in_=a_re.rearrange("(gp g2) p -> (g2 p) gp", g2=2)), w=["are"])
            P.dma(I("dma_start", out=aim, in_=a_im.rearrange("(gp g2) p -> (g2 p) gp", g2=2)), w=["aim"])
            ldv = log_dt.rearrange("o (gp g2) -> o g2 gp", g2=2)
            for g2 in range(2):
                P.dma(I("dma_start", out=dtt[64 * g2:64 * g2 + 64, :], in_=ldv[:, g2, :].to_broadcast([64, 16])), w=["dtt"])
            P.act(I("activation", out=dtt, in_=dtt, func=AF.Exp), r=["dtt"], w=["dtt"])
            P.dve(I("tensor_tensor", out=t0, in0=are, in1=dtt, op=ALU.mult), r=["are", "dtt"], w=["t0"])
            P.act(I("activation", out=rho, in_=t0, func=AF.Exp), r=["t0"], w=["rho"])
            P.dve(I("tensor_tensor", out=th, in0=aim, in1=dtt, op=ALU.mult), r=["aim", "dtt"], w=["th"])
            P.dve(I("memset", t1, 0.0), w=["t1"])
            for kk in range(6):
                thr = (2 * kk + 1) * math.pi
                P.dve(I("tensor_scalar", out=t2, in0=th, scalar1=-thr, scalar2=1e6, op0=ALU.add, op1=ALU.mult), r=["th"], w=["t2"])
                P.dve(I("tensor_scalar", out=t2, in0=t2, scalar1=0.0, scalar2=1.0, op0=ALU.max, op1=ALU.min), r=["t2"], w=["t2"])
                P.dve(I("tensor_tensor", out=t1, in0=t1, in1=t2, op=ALU.add), r=["t1", "t2"], w=["t1"])
            P.dve(I("scalar_tensor_tensor", out=th, in0=t1, scalar=-2.0 * math.pi, in1=th, op0=ALU.mult, op1=ALU.add), r=["t1", "th"], w=["th"])
            P.act(I("activation", out=sth, in_=th, func=AF.Sin), r=["th"], w=["sth"])
            P.dve(I("tensor_scalar", out=t2, in0=th, scalar1=-1.0, scalar2=None, op0=ALU.mult), r=["th"], w=["t2"])
            P.dve(I("tensor_tensor", out=t2, in0=t2, in1=th, op=ALU.max), r=["t2", "th"], w=["t2"])
            P.dve(I("tensor_scalar", out=t2, in0=t2, scalar1=-1.0, scalar2=math.pi / 2, op0=ALU.mult, op1=ALU.add), r=["t2"], w=["t2"])
            P.act(I("activation", out=cth, in_=t2, func=AF.Sin), r=["t2"], w=["cth"])
            P.dve(I("tensor_tensor", out=Are, in0=rho, in1=cth, op=ALU.mult), r=["rho", "cth"], w=["Are"])
            P.dve(I("tensor_tensor", out=Aim, in0=rho, in1=sth, op=ALU.mult), r=["rho", "sth"], w=["Aim"])
            P.dve(I("tensor_scalar", out=t0, in0=Are, scalar1=-1.0, scalar2=None, op0=ALU.add), r=["Are"], w=["t0"])
            P.dve(I("tensor_tensor", out=d2, in0=are, in1=are, op=ALU.mult), r=["are"], w=["d2"])
            P.dve(I("tensor_tensor", out=t1, in0=aim, in1=aim, op=ALU.mult), r=["aim"], w=["t1"])
            P.dve(I("tensor_tensor", out=d2, in0=d2, in1=t1, op=ALU.add), r=["d2", "t1"], w=["d2"])
            P.dve(I("reciprocal", out=d2, in_=d2), r=["d2"], w=["d2"])
            P.dve(I("tensor_tensor", out=t1, in0=t0, in1=are, op=ALU.mult), r=["t0", "are"], w=["t1"])
            P.dve(I("tensor_tensor", out=t2, in0=Aim, in1=aim, op=ALU.mult), r=["Aim", "aim"], w=["t2"])
            P.dve(I("tensor_tensor", out=t1, in0=t1, in1=t2, op=ALU.add), r=["t1", "t2"], w=["t1"])
            P.dve(I("tensor_tensor", out=Fre, in0=t1, in1=d2, op=ALU.mult), r=["t1", "d2"], w=["Fre"])
            P.dve(I("tensor_tensor", out=t1, in0=Aim, in1=are, op=ALU.mult), r=["Aim", "are"], w=["t1"])
            P.dve(I("tensor_tensor", out=t2, in0=t0, in1=aim, op=ALU.mult), r=["t0", "aim"], w=["t2"])
            P.dve(I("tensor_tensor", out=t1, in0=t1, in1=t2, op=ALU.subtract), r=["t1", "t2"], w=["t1"])
            P.dve(I("tensor_tensor", out=Fim, in0=t1, in1=d2, op=ALU.mult), r=["t1", "d2"], w=["Fim"])
            Bre = AR.alloc([128, 16, 16]); Bim = AR.alloc([128, 16, 16]); Bbr = AR.alloc([128, 16, 16]); Bbi = AR.alloc([128, 16, 16]); Bt = AR.alloc([128, 16, 16])
            P.dma(I("dma_start", out=Bre, in_=b_re.rearrange("(gp g2) p c -> (g2 p) gp c", g2=2)), w=["Bre"])
            P.dma(I("dma_start", out=Bim, in_=b_im.rearrange("(gp g2) p c -> (g2 p) gp c", g2=2)), w=["Bim"])
            bc = lambda a: a.unsqueeze(2).to_broadcast([128, 16, 16])
            P.dve(I("tensor_tensor", out=Bbr, in0=Bre, in1=bc(Fre), op=ALU.mult), r=["Bre", "Fre"], w=["Bbr"])
            P.dve(I("tensor_tensor", out=Bt, in0=Bim, in1=bc(Fim), op=ALU.mult), r=["Bim", "Fim"], w=["Bt"])
            P.dve(I("tensor_tensor", out=Bbr, in0=Bbr, in1=Bt, op=ALU.subtract), r=["Bbr", "Bt"], w=["Bbr"])
            P.dve(I("tensor_tensor", out=Bbi, in0=Bim, in1=bc(Fre), op=ALU.mult), r=["Bim", "Fre"], w=["Bbi"])
            P.dve(I("tensor_tensor", out=Bt, in0=Bre, in1=bc(Fim), op=ALU.mult), r=["Bre", "Fim"], w=["Bt"])
            P.dve(I("tensor_tensor", out=Bbi, in0=Bbi, in1=Bt, op=ALU.add), r=["Bbi", "Bt"], w=["Bbi"])
            Bp = [AR.alloc([128, 16, 128], BF16), AR.alloc([128, 16, 128], BF16)]
            m1 = AR.mark()
            Lx = AR.alloc([128, 16, 128])
            for ri, Bb in enumerate((Bbr, Bbi)):
                P.dve(I("memset", Lx, 0.0), w=["Lx"])
                Lv = Lx.rearrange("q gp (l g c) -> q gp l g c", l=4, g=2)
                for g2 in range(2):
                    P.dve(I("tensor_copy",
                        out=Lv[64 * g2:64 * g2 + 64, :, :, g2, :],
                        in_=Bb[64 * g2:64 * g2 + 64].unsqueeze(2).to_broadcast([64, 16, 4, 16])), r=["Bbr", "Bbi"], w=["Lx"])
                for g4 in range(4):
                    pb = bank(g4 % 2).rearrange("p (a b) -> p a b", a=4)
                    for j in range(4):
                        gp = g4 * 4 + j
                        P.pe(I("transpose", out=pb[:, j, :], in_=Lx[:, gp, :], identity=ident), r=["Lx", "ident"], w=["pb%d" % (g4 % 2)])
                    P.dve(I("tensor_copy", out=Bp[ri][:, g4 * 4:g4 * 4 + 4, :], in_=pb), r=["pb%d" % (g4 % 2)], w=["Bp"])
            AR.reset(m1)
            Cr = AR.alloc([128, 16, 32]); Cni = AR.alloc([128, 16, 32])
            m1 = AR.mark()
            Cx = AR.alloc([128, 16, 128])
            for ri, csrc in enumerate((c_re, c_im)):
                P.dve(I("memset", Cx[0:32], 0.0), w=["Cx"])
                cv = csrc.rearrange("(gp g2) c p -> g2 c gp p", g2=2)
                for g2 in range(2):
                    P.dma(I("dma_start", out=Cx[16 * g2:16 * g2 + 16, :, 64 * g2:64 * g2 + 64], in_=cv[g2]), r=["Cx"], w=["Cx"])
                pb = bank(2).rearrange("p (a b) -> p a b", a=16)
                for gp in range(16):
                    P.pe(I("transpose", out=pb[:, gp, :], in_=Cx[0:32, gp, :], identity=ident[0:32, 0:32]), r=["Cx", "ident"], w=["pbC"])
                if ri == 0:
                    P.dve(I("tensor_copy", out=Cr, in_=pb), r=["pbC"], w=["Cr"])
                else:
                    P.dve(I("tensor_scalar", out=Cni, in0=pb, scalar1=-1.0, scalar2=None, op0=ALU.mult), r=["pbC"], w=["Cni"])
            AR.reset(m1)
            cn = AR.alloc([128, 16, 128]); sn = AR.alloc([128, 16, 128]); tA = AR.alloc([128, 16, 64]); tB = AR.alloc([128, 16, 64])
            P.dve(I("tensor_copy", out=cn[:, :, 0:1], in_=cth.unsqueeze(2)), r=["cth"], w=["cn"])
            P.dve(I("tensor_copy", out=sn[:, :, 0:1], in_=sth.unsqueeze(2)), r=["sth"], w=["sn"])
            m = 1
            while m < 128:
                cm = cn[:, :, m - 1:m].to_broadcast([128, 16, m]); smm = sn[:, :, m - 1:m].to_broadcast([128, 16, m])
                ta = tA[:, :, 0:m]; tb = tB[:, :, 0:m]
                P.dve(I("tensor_tensor", out=ta, in0=cn[:, :, 0:m], in1=cm, op=ALU.mult), r=["cn"], w=["tA"])
                P.dve(I("tensor_tensor", out=tb, in0=sn[:, :, 0:m], in1=smm, op=ALU.mult), r=["sn"], w=["tB"])
                P.dve(I("tensor_tensor", out=ta, in0=ta, in1=tb, op=ALU.subtract), r=["tA", "tB"], w=["tA"])
                P.dve(I("tensor_tensor", out=tb, in0=cn[:, :, 0:m], in1=smm, op=ALU.mult), r=["cn", "sn"], w=["tB"])
                P.dve(I("tensor_copy", out=cn[:, :, m:2 * m], in_=ta), r=["tA"], w=["cn"])
                P.dve(I("tensor_tensor", out=ta, in0=sn[:, :, 0:m], in1=cm, op=ALU.mult), r=["sn", "cn"], w=["tA"])
                P.dve(I("tensor_tensor", out=sn[:, :, m:2 * m], in0=ta, in1=tb, op=ALU.add), r=["tA", "tB"], w=["sn"])
                m *= 2
            dcol = AR.alloc([128, 4]); gncol = AR.alloc([128, 4])
            P.dma(I("dma_start", out=dcol, in_=ssm_d.rearrange("(ut r) -> r ut", r=128)), w=["dcol"])
            P.dma(I("dma_start", out=gncol, in_=ssm_norm_g.rearrange("(ut r) -> r ut", r=128)), w=["gncol"])
            Sre = AR.alloc([128, 16]); Sim = AR.alloc([128, 16])
            P.dve(I("memset", Sre, 0.0), w=["Sre"]); P.dve(I("memset", Sim, 0.0), w=["Sim"])
            xin = [AR.alloc([128, 1024]) for _ in range(2)]
            xT = [AR.alloc([128, 8, 512], BF16) for _ in range(2)]
            kst = AR.alloc([128, 4, 512], BF16); qst = AR.alloc([128, 4, 512], BF16)
            vst = [AR.alloc([128, 8, 65], BF16) for _ in range(2)]
            for b_ in range(2):
                P.dve(I("memset", vst[b_], 1.0), w=["vst%d" % b_])
            uT = AR.alloc([128, 4, 512], BF16)
            vre = AR.alloc([128, 4, 128]); vim = AR.alloc([128, 4, 128]); e1 = AR.alloc([128, 4, 128]); e2 = AR.alloc([128, 4, 128])
            wre = AR.alloc([128, 4, 128]); wim = AR.alloc([128, 4, 128]); sre = AR.alloc([128, 4, 128]); sim = AR.alloc([128, 4, 128])
            yd = AR.alloc([128, 512]); g1 = AR.alloc([128, 512]); g2t = AR.alloc([128, 512])
            glT = AR.alloc([128, 4, 512], BF16)
            y2 = AR.alloc([128, 4, 512]); sq = AR.alloc([128, 512]); sig = AR.alloc([128, 512]); rstd = AR.alloc([128, 512])
            soT = AR.alloc([128, 4, 512], BF16)
            pbu_re = bank(2).rearrange("p (a b) -> p a b", a=4); pbu_im = bank(3).rearrange("p (a b) -> p a b", a=4)
            for s in range(16):
                own = s >= 8
                xt = xT[s % 2]; xk = "xT%d" % (s % 2)
                for ti in range(4):
                    xb = xin[ti % 2]; xbk = "xin%d" % (ti % 2)
                    r0 = s * 512 + ti * 128
                    P.dma(I("dma_start", out=xb, in_=xc[r0:r0 + 128, :]), w=[xbk])
                    for half in range(2):
                        pb = bank(half).rearrange("p (a b) -> p a b", a=4)
                        for j in range(4):
                            kt = half * 4 + j
                            P.pe(I("transpose", out=pb[:, j, :], in_=xb[:, kt * 128:(kt + 1) * 128], identity=ident), r=[xbk, "ident"], w=["pb%d" % half])
                        if half == 0:
                            P.dve(I("tensor_copy", out=xt[:, 0:4, ti * 128:(ti + 1) * 128], in_=pb), r=["pb0"], w=[xk])
                        else:
                            P.act(I("activation", out=xt[:, 4:8, ti * 128:(ti + 1) * 128], in_=pb, func=AF.Copy), r=["pb1"], w=[xk])
                def proj_fm(col0, dst, dk, n_m=4):
                    for mt in range(n_m):
                        pb = bank(mt % 2); pk = "pb%d" % (mt % 2)
                        for kt in range(8):
                            P.pe(I("matmul", pb, lhsT=win[:, kt, col0 + mt * 128:col0 + (mt + 1) * 128], rhs=xt[:, kt, :], start=(kt == 0), stop=(kt == 7)), r=["win", xk], w=[pk])
                        if mt % 2 == 0:
                            P.dve(I("tensor_copy", out=dst[:, mt, :], in_=pb), r=[pk], w=[dk])
                        else:
                            P.act(I("activation", out=dst[:, mt, :], in_=pb, func=AF.Copy), r=[pk], w=[dk])
                proj_fm(512, kst, "kst")
                P.dma(I("dma_start", out=KT[:, :, s * 512:(s + 1) * 512].rearrange("h p t -> p h t"), in_=kst), r=["kst"], w=["KT"])
                if own:
                    proj_fm(0, qst, "qst")
                    P.dma(I("dma_start", out=QT[:, :, (s - 8) * 512:(s - 7) * 512].rearrange("h p t -> p h t"), in_=qst), r=["qst"], w=["QT"])
                for ti in range(4):
                    pb = bank(ti % 2); pk = "pb%d" % (ti % 2)
                    vb = vst[ti % 2]; vk = "vst%d" % (ti % 2)
                    for kt in range(8):
                        P.pe(I("matmul", pb, lhsT=xt[:, kt, ti * 128:(ti + 1) * 128], rhs=win[:, kt, 1024:1536], start=(kt == 0), stop=(kt == 7)), r=["win", xk], w=[pk])
                    P.dve(I("tensor_copy", out=vb[:, :, 0:64], in_=pb.rearrange("p (h d) -> p h d", h=8)), r=[pk], w=[vk])
                    P.dma(I("dma_start", out=VV[s * 4 + ti], in_=vb.rearrange("p h d -> p (h d)")), r=[vk], w=["VV"])
                proj_fm(1536, uT, "uT")
                for ut in range(4):
                    py = bank(4 + ut % 2); pyk = "py%d" % (ut % 2)
                    for un in range(4):
                        tsl = slice(un * 128, (un + 1) * 128)
                        for gl in range(4):
                            gp = ut * 4 + gl
                            P.pe(I("matmul", pbu_re[:, gl, :], lhsT=Bp[0][32 * gl:32 * gl + 32, gp, :], rhs=uT[32 * gl:32 * gl + 32, ut, tsl], start=True, stop=True, tile_position=(32 * gl, 0)), r=["Bp", "uT"], w=["pbu_re"], tiled=True)
                            P.pe(I("matmul", pbu_im[:, gl, :], lhsT=Bp[1][32 * gl:32 * gl + 32, gp, :], rhs=uT[32 * gl:32 * gl + 32, ut, tsl], start=True, stop=True, tile_position=(32 * gl, 0)), r=["Bp", "uT"], w=["pbu_im"], tiled=True)
                        g4 = slice(ut * 4, ut * 4 + 4)
                        cnv = cn[:, g4, :]; snv = sn[:, g4, :]
                        P.dve(I("tensor_tensor", out=vre, in0=pbu_re, in1=cnv, op=ALU.mult), r=["pbu_re", "cn"], w=["vre"])
                        P.dve(I("tensor_tensor", out=e1, in0=pbu_im, in1=snv, op=ALU.mult), r=["pbu_im", "sn"], w=["e1"])
                        P.dve(I("tensor_tensor", out=vim, in0=pbu_im, in1=cnv, op=ALU.mult), r=["pbu_im", "cn"], w=["vim"])
                        P.dve(I("tensor_tensor", out=e2, in0=pbu_re, in1=snv, op=ALU.mult), r=["pbu_re", "sn"], w=["e2"])
                        P.pool(I("tensor_tensor", out=vre, in0=vre, in1=e1, op=ALU.add), r=["vre", "e1"], w=["vre"])
                        P.pool(I("tensor_tensor", out=vim, in0=vim, in1=e2, op=ALU.subtract), r=["vim", "e2"], w=["vim"])
                        for gl in range(4):
                            gp = ut * 4 + gl
                            P.dve(I("tensor_tensor_scan", out=wre[:, gl, :], data0=rho[:, gp:gp + 1].to_broadcast([128, 128]), data1=vre[:, gl, :], initial=Sre[:, gp:gp + 1], op0=ALU.mult, op1=ALU.add), r=["vre", "rho", "Sre"], w=["wre"])
                            P.dve(I("tensor_tensor_scan", out=wim[:, gl, :], data0=rho[:, gp:gp + 1].to_broadcast([128, 128]), data1=vim[:, gl, :], initial=Sim[:, gp:gp + 1], op0=ALU.mult, op1=ALU.add), r=["vim", "rho", "Sim"], w=["wim"])
                        if own:
                            cs, ws = slice(0, 128), slice(0, 4)
                        else:
                            cs, ws = slice(127, 128), slice(0, 4)
                        P.pool(I("tensor_tensor", out=sre[:, :, cs], in0=wre[:, :, cs], in1=cnv[:, :, cs], op=ALU.mult), r=["wre", "cn"], w=["sre"])
                        P.pool(I("tensor_tensor", out=e1[:, :, cs], in0=wim[:, :, cs], in1=snv[:, :, cs], op=ALU.mult), r=["wim", "sn"], w=["e1"])
                        P.pool(I("tensor_tensor", out=sre[:, :, cs], in0=sre[:, :, cs], in1=e1[:, :, cs], op=ALU.subtract), r=["sre", "e1"], w=["sre"])
                        P.pool(I("tensor_tensor", out=sim[:, :, cs], in0=wim[:, :, cs], in1=cnv[:, :, cs], op=ALU.mult), r=["wim", "cn"], w=["sim"])
                        P.pool(I("tensor_tensor", out=e2[:, :, cs], in0=wre[:, :, cs], in1=snv[:, :, cs], op=ALU.mult), r=["wre", "sn"], w=["e2"])
                        P.pool(I("tensor_tensor", out=sim[:, :, cs], in0=sim[:, :, cs], in1=e2[:, :, cs], op=ALU.add), r=["sim", "e2"], w=["sim"])
                        P.dve(I("tensor_copy", out=Sre[:, g4], in_=sre[:, :, 127]), r=["sre"], w=["Sre"])
                        P.dve(I("tensor_copy", out=Sim[:, g4], in_=sim[:, :, 127]), r=["sim"], w=["Sim"])
                        if own:
                            for gl in range(4):
                                gp = ut * 4 + gl
                                P.pe(I("matmul", py[32 * gl:32 * gl + 32, tsl], lhsT=Cr[:, gp, :], rhs=sre[:, gl, :], start=True, stop=False, tile_position=(0, 32 * gl)), r=["Cr", "sre"], w=[pyk], tiled=True)
                                P.pe(I("matmul", py[32 * gl:32 * gl + 32, tsl], lhsT=Cni[:, gp, :], rhs=sim[:, gl, :], start=False, stop=True, tile_position=(0, 32 * gl)), r=["Cni", "sim"], w=[pyk], tiled=True)
                    if own:
                        P.dve(I("scalar_tensor_tensor", out=yd, in0=uT[:, ut, :], scalar=dcol[:, ut:ut + 1], in1=py, op0=ALU.mult, op1=ALU.add), r=["uT", "dcol", pyk], w=["yd"])
                        P.pool(I("tensor_tensor", out=g1, in0=yd, in1=yd, op=ALU.mult), r=["yd"], w=["g1"])
                        P.pool(I("tensor_scalar", out=g1, in0=g1, scalar1=0.044715, scalar2=1.0, op0=ALU.mult, op1=ALU.add), r=["g1"], w=["g1"])
                        P.pool(I("tensor_tensor", out=g1, in0=g1, in1=yd, op=ALU.mult), r=["g1", "yd"], w=["g1"])
                        P.act(I("activation", out=g2t, in_=g1, func=AF.Sigmoid, scale=2.0 * math.sqrt(2.0 / math.pi)), r=["g1"], w=["g2t"])
                        P.pool(I("tensor_tensor", out=glT[:, ut, :], in0=yd, in1=g2t, op=ALU.mult), r=["yd", "g2t"], w=["glT"])
                if own:
                    for mo in range(4):
                        plo = bank(0); phi = bank(1)
                        for kt in range(4):
                            P.pe(I("matmul", plo, lhsT=wglu[:, kt, mo * 128:(mo + 1) * 128], rhs=glT[:, kt, :], start=(kt == 0), stop=(kt == 3)), r=["wglu", "glT"], w=["pb0"])
                        for kt in range(4):
                            P.pe(I("matmul", phi, lhsT=wglu[:, kt, 512 + mo * 128:512 + (mo + 1) * 128], rhs=glT[:, kt, :], start=(kt == 0), stop=(kt == 3)), r=["wglu", "glT"], w=["pb1"])
                        P.act(I("activation", out=sig, in_=phi, func=AF.Sigmoid), r=["pb1"], w=["sig"])
                        P.dve(I("tensor_tensor", out=y2[:, mo, :], in0=plo, in1=sig, op=ALU.mult), r=["pb0", "sig"], w=["y2"])
                    pms = bank(6)
                    for mo in range(4):
                        P.pool(I("tensor_tensor", out=sq, in0=y2[:, mo, :], in1=y2[:, mo, :], op=ALU.mult), r=["y2"], w=["sq"])
                        P.pe(I("matmul", pms, lhsT=ones, rhs=sq, start=(mo == 0), stop=(mo == 3)), r=["ones", "sq"], w=["pms"])
                    P.act(I("activation", out=rstd, in_=pms, func=AF.Ln, scale=1.0 / 512.0, bias=1e-5), r=["pms"], w=["rstd"])
                    P.act(I("activation", out=rstd, in_=rstd, func=AF.Exp, scale=-0.5), r=["rstd"], w=["rstd"])
                    for mo in range(4):
                        P.dve(I("scalar_tensor_tensor", out=soT[:, mo, :], in0=y2[:, mo, :], scalar=gncol[:, mo:mo + 1], in1=rstd, op0=ALU.mult, op1=ALU.mult), r=["y2", "gncol", "rstd"], w=["soT"])
                    P.dma(I("dma_start", out=MIXT[4:8, :, (s - 8) * 512:(s - 7) * 512].rearrange("h p t -> p h t"), in_=soT), r=["soT"], w=["MIXT"])
            P.barrier()
            AR.reset(gmark)


        P.pe_sync = False
        G = AR.alloc([128, 32, 32])
        gmark = AR.mark()
        SC = 1.0 / math.sqrt(32.0)

        def layer_norm_tile(pre, gb, bb, dst, tg):
            stt = AR_t["st"]; mv = AR_t["mv"]; rs = AR_t["rs"]
            P.dve(I("bn_stats", out=stt[:, 0:6], in_=pre[:, 0:512]), r=[tg], w=["lnst"])
            P.dve(I("bn_stats", out=stt[:, 6:12], in_=pre[:, 512:1024]), r=[tg], w=["lnst2"])
            P.dve(I("bn_aggr", out=mv, in_=stt), r=["lnst", "lnst2"], w=["lnmv"])
            P.act(I("activation", out=rs, in_=mv[:, 1:2], func=AF.Ln, bias=1e-5), r=["lnmv"], w=["lnrs"])
            P.act(I("activation", out=rs, in_=rs, func=AF.Exp, scale=-0.5), r=["lnrs"], w=["lnrs"])
            P.dve(I("tensor_scalar", out=dst, in0=pre, scalar1=mv[:, 0:1], scalar2=rs[:, 0:1], op0=ALU.subtract, op1=ALU.mult), r=[tg, "lnmv", "lnrs"], w=[tg + "o"])
            P.pool(I("tensor_tensor", out=dst, in0=dst, in1=gb, op=ALU.mult), r=[tg + "o", "lng"], w=[tg + "o"])
            P.pool(I("tensor_tensor", out=dst, in0=dst, in1=bb, op=ALU.add), r=[tg + "o", "lng"], w=[tg + "o"])
        AR_t = {}

        if "2" in phases:
            slopes = [2.0 ** (-(h + 1)) for h in range(8)]
            tri = AR.alloc([128, 128]); trib = AR.alloc([128, 128], BF16); U = AR.alloc([128, 128]); kl = AR.alloc([128, 1])
            P.pool(I("memset", tri, 1.0), w=["tri"])
            P.pool(I("affine_select", out=tri, in_=tri, pattern=[[1, 128]], compare_op=ALU.is_ge, fill=0.0, base=0, channel_multiplier=-1), r=["tri"], w=["tri"])
            P.dve(I("tensor_copy", out=trib, in_=tri), r=["tri"], w=["trib"])
            P.pool(I("memset", U, 1.0), w=["U"])
            P.pool(I("affine_select", out=U, in_=U, pattern=[[1, 128]], compare_op=ALU.is_gt, fill=0.0, base=0, channel_multiplier=-1), r=["U"], w=["U"])
            P.pe(I("matmul", bank(0)[:, 0:1], lhsT=U, rhs=ones[:, 0:1], start=True, stop=True), r=["U", "ones"], w=["pb0"])
            P.dve(I("tensor_copy", out=kl, in_=bank(0)[:, 0:1]), r=["pb0"], w=["kl"])
            ND = 68
            bt = AR.alloc([128, 8, ND]); btp = AR.alloc([128, 8, ND]); prefc = AR.alloc([128, 1])
            P.dma(I("dma_start", out=prefc, in_=pref), w=["prefc"])
            for h in range(8):
                for Dd in range(ND):
                    fn = P.dve if (Dd % 2 == 0) else P.pool
                    fn(I("tensor_scalar", out=bt[:, h, Dd:Dd + 1], in0=kl, scalar1=slopes[h], scalar2=-slopes[h] * 128.0 * Dd, op0=ALU.mult, op1=ALU.add), r=["kl"], w=["bt%d" % (Dd % 2)])
            P.dve(I("tensor_scalar", out=btp, in0=bt, scalar1=prefc[:, 0:1], scalar2=None, op0=ALU.add), r=["bt0", "bt1", "prefc"], w=["btp"])
            lv = [AR.alloc([64, 32]) for _ in range(4)]; lt = AR.alloc([64, 32]); l1 = AR.alloc([64, 1]); l2 = AR.alloc([64, 1]); neglam = AR.alloc([64, 1]); gcol = AR.alloc([64, 1])
            for i_, src in enumerate((lq1, lk1, lq2, lk2)):
                P.dma(I("dma_start", out=lv[i_], in_=src.to_broadcast([64, 32])), w=["lv%d" % i_])
            P.dve(I("tensor_tensor", out=lt, in0=lv[0], in1=lv[1], op=ALU.mult), r=["lv0", "lv1"], w=["lt"])
            P.dve(I("reduce_sum", out=l1, in_=lt, axis=AX.X), r=["lt"], w=["l1"])
            P.dve(I("tensor_tensor", out=lt, in0=lv[2], in1=lv[3], op=ALU.mult), r=["lv2", "lv3", "l1"], w=["lt"])
            P.dve(I("reduce_sum", out=l2, in_=lt, axis=AX.X), r=["lt"], w=["l2"])
            P.act(I("activation", out=l1, in_=l1, func=AF.Exp), r=["l1"], w=["l1"])
            P.act(I("activation", out=l2, in_=l2, func=AF.Exp), r=["l2"], w=["l2"])
            P.dve(I("tensor_tensor", out=neglam, in0=l2, in1=l1, op=ALU.subtract), r=["l1", "l2"], w=["neglam"])
            P.dve(I("tensor_scalar", out=neglam, in0=neglam, scalar1=-LAMBDA_INIT, scalar2=None, op0=ALU.add), r=["neglam"], w=["neglam"])
            P.dma(I("dma_start", out=gcol, in_=subln_g), w=["gcol"])
            P.dve(I("tensor_scalar", out=gcol, in0=gcol, scalar1=1.0 - LAMBDA_INIT, scalar2=None, op0=ALU.mult), r=["gcol"], w=["gcol"])
            sel = AR.alloc([65, 64])
            P.dve(I("memset", sel, 0.0), w=["sel"])
            P.dve(I("memset", sel[64:65, :], 1.0), r=["sel"], w=["sel"])
            KTh = AR.alloc([128, L], BF16); QTh = AR.alloc([128, LO], BF16); Vh = AR.alloc([128, 64, 130], BF16)
            Pt = [AR.alloc([128, 2, 512], BF16) for _ in range(2)]
            Osb = AR.alloc([65, 2, 512]); Od = AR.alloc([65, 2, 512]); rL = AR.alloc([64, 2, 512])
            a1 = AR.alloc([64, 512]); a2 = AR.alloc([64, 512]); dif = AR.alloc([64, 512]); sq2 = AR.alloc([64, 512]); rs2 = AR.alloc([64, 512])
            oT = AR.alloc([64, 512], BF16)
            scb = [bank(0, 2).rearrange("p (a b) -> p a b", a=2), bank(2, 2).rearrange("p (a b) -> p a b", a=2)]
            Ooff = bank(4, 2).rearrange("p (a b) -> p a b", a=2); Odg = bank(6, 2).rearrange("p (a b) -> p a b", a=2)
            it = 0
            for hp in range(4):
                P.dma(I("dma_start", out=KTh, in_=KT[hp]), r=["KT"], w=["KTh"])
                P.dma(I("dma_start", out=QTh, in_=QT[hp]), r=["QT"], w=["QTh"])
                P.dma(I("dma_start", out=Vh, in_=VV[:, :, hp * 130:(hp + 1) * 130].rearrange("b p c -> p b c")), r=["VV"], w=["Vh"])
                for j in range(8):
                    for hl in range(2):
                        h = 2 * hp + hl; sl = slopes[h]
                        q0 = LO + 512 * j; kd0 = q0 // 128; klo = _qkey_lo(q0, sl)
                        offs = list(range(klo, kd0))
                        for ii, kb in enumerate(offs):
                            sc = scb[it % 2]; sk = "sc%d" % (it % 2); pt = Pt[it % 2]; pk = "Pt%d" % (it % 2); it += 1
                            for m_ in range(2):
                                pr = 32 * (2 * hl + m_)
                                P.pe(I("matmul", sc[:, m_, :], lhsT=KTh[pr:pr + 32, kb * 128:(kb + 1) * 128], rhs=QTh[pr:pr + 32, j * 512:(j + 1) * 512], start=True, stop=True, tile_position=(pr, 0)), r=["KTh", "QTh"], w=[sk])
                            btab = btp if kb < 32 else bt
                            P.act(I("activation", out=pt, in_=sc, func=AF.Exp, bias=btab[:, h, kd0 - kb:kd0 - kb + 1], scale=SC), r=[sk, "btp", "bt0", "bt1"], w=[pk])
                            for m_ in range(2):
                                P.pe(I("matmul", Ooff[0:65, m_, :], lhsT=Vh[:, kb, hl * 65:(hl + 1) * 65], rhs=pt[:, m_, :], start=(ii == 0), stop=(ii == len(offs) - 1)), r=["Vh", pk], w=["Ooff"])
                        for r_ in range(4):
                            kb = kd0 + r_
                            sc = scb[it % 2]; sk = "sc%d" % (it % 2); pt = Pt[it % 2]; pk = "Pt%d" % (it % 2); it += 1
                            c0 = 128 * r_
                            for m_ in range(2):
                                pr = 32 * (2 * hl + m_)
                                P.pe(I("matmul", sc[:, m_, c0:512], lhsT=KTh[pr:pr + 32, kb * 128:(kb + 1) * 128], rhs=QTh[pr:pr + 32, j * 512 + c0:(j + 1) * 512], start=True, stop=True, tile_position=(pr, 0)), r=["KTh", "QTh"], w=[sk])
                            for qs in range(r_, 4):
                                P.act(I("activation", out=pt[:, :, 128 * qs:128 * qs + 128], in_=sc[:, :, 128 * qs:128 * qs + 128], func=AF.Exp, bias=bt[:, h, qs - r_:qs - r_ + 1], scale=SC), r=[sk, "bt0", "bt1"], w=[pk])
                            P.pool(I("tensor_tensor", out=pt[:, :, c0:c0 + 128], in0=pt[:, :, c0:c0 + 128], in1=trib.unsqueeze(1).to_broadcast([128, 2, 128]), op=ALU.mult), r=[pk, "trib"], w=[pk])
                            for m_ in range(2):
                                P.pe(I("matmul", Odg[0:65, m_, c0:512], lhsT=Vh[:, kb, hl * 65:(hl + 1) * 65], rhs=pt[:, m_, c0:512], start=(r_ == 0), stop=(r_ == 3)), r=["Vh", pk], w=["Odg"])
                        P.act(I("activation", out=Od, in_=Odg[0:65], func=AF.Copy), r=["Odg"], w=["Od"])
                        for qs in range(4):
                            f = math.exp(-sl * 128.0 * qs)
                            cs = slice(128 * qs, 128 * qs + 128)
                            P.dve(I("scalar_tensor_tensor", out=Osb[:, :, cs], in0=Ooff[0:65, :, cs], scalar=f, in1=Od[:, :, cs], op0=ALU.mult, op1=ALU.add), r=["Ooff", "Od"], w=["Osb"])
                        for m_ in range(2):
                            P.pe(I("matmul", Odg[0:64, m_, :], lhsT=sel, rhs=Osb[:, m_, :], start=True, stop=True), r=["sel", "Osb", "Od"], w=["Odg"])
                        P.dve(I("reciprocal", out=rL, in_=Odg[0:64]), r=["Odg"], w=["rL"])
                        P.pool(I("tensor_tensor", out=a1, in0=Osb[0:64, 0, :], in1=rL[:, 0, :], op=ALU.mult), r=["Osb", "rL"], w=["a1"])
                        P.pool(I("tensor_tensor", out=a2, in0=Osb[0:64, 1, :], in1=rL[:, 1, :], op=ALU.mult), r=["Osb", "rL"], w=["a2"])
                        P.dve(I("scalar_tensor_tensor", out=dif, in0=a2, scalar=neglam[:, 0:1], in1=a1, op0=ALU.mult, op1=ALU.add), r=["a1", "a2", "neglam"], w=["dif"])
                        P.pool(I("tensor_tensor", out=sq2, in0=dif, in1=dif, op=ALU.mult), r=["dif"], w=["sq2"])
                        P.pe(I("matmul", Odg[0:64, 0, :], lhsT=ones[0:64, 0:64], rhs=sq2, start=True, stop=True), r=["ones", "sq2", "rL"], w=["Odg"])
                        P.act(I("activation", out=rs2, in_=Odg[0:64, 0, :], func=AF.Ln, scale=1.0 / 64.0, bias=1e-5), r=["Odg"], w=["rs2"])
                        P.act(I("activation", out=rs2, in_=rs2, func=AF.Exp, scale=-0.5), r=["rs2"], w=["rs2"])
                        P.dve(I("scalar_tensor_tensor", out=oT, in0=dif, scalar=gcol[:, 0:1], in1=rs2, op0=ALU.mult, op1=ALU.mult), r=["dif", "gcol", "rs2"], w=["oT"])
                        P.dma(I("dma_start", out=MIXT[h // 2, (h % 2) * 64:(h % 2) * 64 + 64, j * 512:(j + 1) * 512], in_=oT), r=["oT"], w=["MIXT"])
            P.barrier()
            AR.reset(gmark)

        def bcast_row(dst, src, n, key):
            P.dma(I("dma_start", out=dst, in_=src.to_broadcast([128, n])), w=[key])

        if "3" in phases:
            if os.environ.get('KZERO'):
                zt = AR.alloc([128, 8, 512], BF16)
                P.dve(I("memset", zt, 0.0), w=["zt"])
                for cc in range(8):
                    P.dma(I("dma_start", out=MIXT[:, :, cc * 512:(cc + 1) * 512].rearrange("k p t -> p k t"), in_=zt), r=["zt"], w=["MIXT"])
            wout = AR.alloc([128, 8, 1024], BF16)
            for kt in range(8):
                P.dma(I("dma_start", out=wout[:, kt, :], in_=w_out[kt * 128:(kt + 1) * 128, :]), w=["wout"], q="gpsimd")
            wr = AR.alloc([128, 8, 32])
            P.dma(I("dma_start", out=wr, in_=w_router.rearrange("(k p) e -> p k e", p=128)), w=["wr"])
            g1b = AR.alloc([128, 1024]); b1b = AR.alloc([128, 1024]); brb = AR.alloc([128, 32])
            bcast_row(g1b, ln1_g, 1024, "lng"); bcast_row(b1b, ln1_b, 1024, "lng"); bcast_row(brb, b_router, 32, "brb")
            AR_t["st"] = AR.alloc([128, 12]); AR_t["mv"] = AR.alloc([128, 2]); AR_t["rs"] = AR.alloc([128, 1])
            mixT = [AR.alloc([128, 8, 128], BF16) for _ in range(2)]
            xt_ = [AR.alloc([128, 1024]) for _ in range(2)]
            pre = AR.alloc([128, 1024]); x1 = [AR.alloc([128, 1024]) for _ in range(2)]
            x1Tf = AR.alloc([128, 8, 128]); x1Tb = [AR.alloc([128, 8, 128], BF16) for _ in range(2)]
            lg = AR.alloc([128, 32]); mx8 = AR.alloc([128, 8]); msk = AR.alloc([128, 32]); ex = AR.alloc([128, 32]); nmx = AR.alloc([128, 1]); ssum = AR.alloc([128, 1])
            CUT = int(os.environ.get('KCUT', '99'))
            for t in range(int(os.environ.get('KNT3', '32'))):
                b_ = t % 2
                P.dma(I("dma_start", out=mixT[b_], in_=MIXT[:, :, t * 128:(t + 1) * 128].rearrange("k p t -> p k t")), r=["MIXT"], w=["mixT%d" % b_])
                P.dma(I("dma_start", out=xt_[b_], in_=xc[LO + t * 128:LO + (t + 1) * 128, :]), w=["xt%d" % b_])
                if CUT < 2: continue
                pm = bank(0, 2)
                for dh in range(2):
                    for kt in range(8):
                        P.pe(I("matmul", pm[:, dh * 512:(dh + 1) * 512], lhsT=mixT[b_][:, kt, :], rhs=wout[:, kt, dh * 512:(dh + 1) * 512], start=(kt == 0), stop=(kt == 7)), r=["mixT%d" % b_, "wout"], w=["pm"])
                P.dve(I("scalar_tensor_tensor", out=pre, in0=xt_[b_], scalar=ALPHA, in1=pm, op0=ALU.mult, op1=ALU.add), r=["xt%d" % b_, "pm"], w=["pre"])
                if CUT < 3: continue
                layer_norm_tile(pre, g1b, b1b, x1[b_], "pre")
                if CUT < 4: continue
                P.dma(I("dma_start", out=X1[t * 128:(t + 1) * 128, :], in_=x1[b_]), r=["preo"], w=["X1"])
                if CUT < 5: continue
                ptr = bank(2, 2).rearrange("p (a b) -> p a b", a=8)
                for kt in range(8):
                    P.pe(I("transpose", out=ptr[:, kt, :], in_=x1[b_][:, kt * 128:(kt + 1) * 128], identity=ident), r=["preo", "ident"], w=["ptr"])
                KS = os.environ.get('KSUB', 'abc')
                if 'a' in KS:
                    P.act(I("activation", out=x1Tf, in_=ptr, func=AF.Copy), r=["ptr"], w=["x1Tf"])
                if 'b' in KS:
                    P.act(I("activation", out=x1Tb[b_], in_=ptr, func=AF.Copy), r=["ptr"], w=["x1Tb%d" % b_])
                if 'c' in KS:
                    P.dma(I("dma_start", out=X1T[:, :, t * 128:(t + 1) * 128].rearrange("k p t -> p k t"), in_=x1Tb[b_]), r=["x1Tb%d" % b_], w=["X1T"])
                if CUT < 6: continue
                pl = bank(4)[:, 0:32]
                for kt in range(8):
                    P.pe(I("matmul", pl, lhsT=x1Tf[:, kt, :], rhs=wr[:, kt, :], start=(kt == 0), stop=(kt == 7)), r=["x1Tf", "wr"], w=["pl"])
                if CUT < 7: continue
                P.dve(I("tensor_tensor", out=lg, in0=pl, in1=brb, op=ALU.add), r=["pl", "brb"], w=["lg"])
                P.dve(I("max", out=mx8, in_=lg), r=["lg"], w=["mx8"])
                P.dve(I("tensor_scalar", out=msk, in0=lg, scalar1=mx8[:, 3:4], scalar2=1e-7, op0=ALU.subtract, op1=ALU.add), r=["lg", "mx8"], w=["msk"])
                P.dve(I("tensor_scalar", out=msk, in0=msk, scalar1=1e10, scalar2=0.0, op0=ALU.mult, op1=ALU.max), r=["msk"], w=["msk"])
                P.dve(I("tensor_scalar", out=msk, in0=msk, scalar1=1.0, scalar2=None, op0=ALU.min), r=["msk"], w=["msk"])
                P.dve(I("tensor_scalar", out=nmx, in0=mx8[:, 0:1], scalar1=-1.0, scalar2=None, op0=ALU.mult), r=["mx8"], w=["nmx"])
                P.act(I("activation", out=ex, in_=lg, func=AF.Exp, bias=nmx[:, 0:1]), r=["lg", "nmx"], w=["ex"])
                P.dve(I("tensor_tensor", out=ex, in0=ex, in1=msk, op=ALU.mult), r=["ex", "msk"], w=["ex"])
                P.dve(I("reduce_sum", out=ssum, in_=ex, axis=AX.X), r=["ex"], w=["ssum"])
                P.dve(I("reciprocal", out=ssum, in_=ssum), r=["ssum"], w=["ssum"])
                P.dve(I("tensor_scalar", out=G[:, t, :], in0=ex, scalar1=ssum[:, 0:1], scalar2=None, op0=ALU.mult), r=["ex", "ssum"], w=["G"])
            P.barrier()
            AR.reset(gmark)

        if "4" in phases:
            Wgu = [AR.alloc([128, 8, 2, 1024], BF16) for _ in range(2)]
            Wd = [AR.alloc([128, 8, 1024], BF16) for _ in range(2)]
            stage = [AR.alloc([128, 2048]) for _ in range(2)]
            X1Tc = AR.alloc([128, 8, 1024], BF16); acc = AR.alloc([128, 8, 1024]); actT = AR.alloc([128, 8, 512], BF16)
            tgs = [AR.alloc([128, 512]) for _ in range(2)]; tsgs = [AR.alloc([128, 512]) for _ in range(2)]; tls = [AR.alloc([128, 512]) for _ in range(2)]
            BGU = AR.alloc([128, 8, 2, 32]); bd = AR.alloc([32, 1024]); GTc = AR.alloc([32, 8, 128])
            bgn = stage[0][0:32, :]
            P.dma(I("dma_start", out=bgn, in_=b_gu), w=["stage0"])
            bgv = bgn.rearrange("e (ft p two) -> e ft two p", p=128, two=2)
            pbg = bank(7).rearrange("p (a b c) -> p a b c", a=8, b=2)
            for ft in range(8):
                for two in range(2):
                    P.pe(I("transpose", out=pbg[:, ft, two, :], in_=bgv[:, ft, two, :], identity=ident[0:32, 0:32]), r=["stage0", "ident"], w=["B7"])
            P.dve(I("tensor_copy", out=BGU, in_=pbg), r=["B7"], w=["BGU"])
            P.dma(I("dma_start", out=bd, in_=b_dn), w=["bd"])
            stn = [0]

            def load_steps(e):
                b_ = e % 2
                steps = []
                for kt in range(8):
                    def st_(kt=kt):
                        sb_ = stn[0] % 2; stn[0] += 1
                        P.dma(I("dma_start", out=stage[sb_], in_=w_gu[e, kt * 128:(kt + 1) * 128, :]), w=["stage%d" % sb_])
                        src = stage[sb_].rearrange("p (f two) -> p two f", two=2)
                        if kt % 2 == 0:
                            P.act(I("activation", out=Wgu[b_][:, kt, :, :], in_=src, func=AF.Copy), r=["stage%d" % sb_], w=["Wgu%d" % b_])
                        else:
                            P.pool(I("tensor_copy", out=Wgu[b_][:, kt, :, :], in_=src), r=["stage%d" % sb_], w=["Wgu%d" % b_])
                    steps.append(st_)
                for k2 in range(4):
                    def st2_(k2=k2):
                        sb_ = stn[0] % 2; stn[0] += 1
                        P.dma(I("dma_start", out=stage[sb_].rearrange("p (k f) -> p k f", k=2), in_=w_dn[e, k2 * 256:(k2 + 1) * 256, :].rearrange("(k p) f -> p k f", p=128)), w=["stage%d" % sb_])
                        src = stage[sb_].rearrange("p (k f) -> p k f", k=2)
                        if k2 % 2 == 0:
                            P.act(I("activation", out=Wd[b_][:, 2 * k2:2 * k2 + 2, :], in_=src, func=AF.Copy), r=["stage%d" % sb_], w=["Wd%d" % b_])
                        else:
                            P.pool(I("tensor_copy", out=Wd[b_][:, 2 * k2:2 * k2 + 2, :], in_=src), r=["stage%d" % sb_], w=["Wd%d" % b_])
                    steps.append(st2_)
                return steps

            def load_expert(e):
                for f_ in load_steps(e):
                    f_()

            for c in range(NCH):
                P.dma(I("dma_start", out=X1Tc, in_=X1T[:, :, c * 1024:(c + 1) * 1024].rearrange("k p t -> p k t")), r=["X1T"], w=["X1Tc"])
                pg = bank(6)[0:32, :].rearrange("p (a b) -> p a b", a=4)
                for half in range(2):
                    for i_ in range(4):
                        tl_ = half * 4 + i_
                        P.pe(I("transpose", out=pg[:, i_, :], in_=G[:, c * 8 + tl_, :], identity=ident), r=["G", "ident"], w=["pg"])
                    P.dve(I("tensor_copy", out=GTc[:, half * 4:half * 4 + 4, :], in_=pg), r=["pg"], w=["GTc"])
                for tl_ in range(8):
                    pa = bank(0, 2)
                    for dh in range(2):
                        P.pe(I("matmul", pa[:, dh * 512:(dh + 1) * 512], lhsT=GTc[:, tl_, :], rhs=bd[:, dh * 512:(dh + 1) * 512], start=True, stop=True), r=["GTc", "bd"], w=["pa"])
                    P.dve(I("tensor_copy", out=acc[:, tl_, :], in_=pa), r=["pa"], w=["acc"])
                load_expert(0)
                for e in range(NE):
                    pend = load_steps(e + 1) if e + 1 < NE else []
                    b_ = e % 2
                    for tc in range(2):
                        for ft in range(8):
                            pgl = bank((ft % 2) * 2); pln = bank((ft % 2) * 2 + 1); gk = "pgl%d" % (ft % 2)
                            tg = tgs[ft % 2]; tsg = tsgs[ft % 2]; tl = tls[ft % 2]; kg = "tg%d" % (ft % 2); ksg = "tsg%d" % (ft % 2); kl_ = "tl%d" % (ft % 2)
                            for kt in range(8):
                                P.pe(I("matmul", pgl, lhsT=Wgu[b_][:, kt, 0, ft * 128:(ft + 1) * 128], rhs=X1Tc[:, kt, tc * 512:(tc + 1) * 512], start=(kt == 0), stop=(kt == 7)), r=["Wgu%d" % b_, "X1Tc"], w=[gk])
                            for kt in range(8):
                                P.pe(I("matmul", pln, lhsT=Wgu[b_][:, kt, 1, ft * 128:(ft + 1) * 128], rhs=X1Tc[:, kt, tc * 512:(tc + 1) * 512], start=(kt == 0), stop=(kt == 7)), r=["Wgu%d" % b_, "X1Tc"], w=[gk + "l"])
                            P.dve(I("tensor_scalar", out=tg, in0=pgl, scalar1=BGU[:, ft, 0, e:e + 1], scalar2=7.0, op0=ALU.add, op1=ALU.min), r=[gk, "BGU"], w=[kg])
                            P.act(I("activation", out=tsg, in_=tg, func=AF.Sigmoid, scale=1.702), r=[kg], w=[ksg])
                            P.dve(I("tensor_scalar", out=tl, in0=pln, scalar1=BGU[:, ft, 1, e:e + 1], scalar2=7.0, op0=ALU.add, op1=ALU.min), r=[gk + "l", "BGU"], w=[kl_])
                            P.pool(I("tensor_scalar", out=tl, in0=tl, scalar1=-7.0, scalar2=1.0, op0=ALU.max, op1=ALU.add), r=[kl_], w=[kl_])
                            P.pool(I("tensor_tensor", out=tg, in0=tg, in1=tsg, op=ALU.mult), r=[kg, ksg], w=[kg])
                            P.pool(I("tensor_tensor", out=actT[:, ft, :], in0=tg, in1=tl, op=ALU.mult), r=[kg, kl_], w=["actT"])
                            if pend:
                                pend.pop(0)()
                        for ti in range(4):
                            tl_ = tc * 4 + ti
                            for dh in range(2):
                                pdn = bank(4 + (ti * 2 + dh) % 4); dk = "pdn%d" % ((ti * 2 + dh) % 4)
                                for ft in range(8):
                                    P.pe(I("matmul", pdn, lhsT=actT[:, ft, ti * 128:(ti + 1) * 128], rhs=Wd[b_][:, ft, dh * 512:(dh + 1) * 512], start=(ft == 0), stop=(ft == 7)), r=["actT", "Wd%d" % b_], w=[dk])
                                P.dve(I("scalar_tensor_tensor", out=acc[:, tl_, dh * 512:(dh + 1) * 512], in0=pdn, scalar=G[:, c * 8 + tl_, e:e + 1], in1=acc[:, tl_, dh * 512:(dh + 1) * 512], op0=ALU.mult, op1=ALU.add), r=[dk, "G", "acc"], w=["acc"])
                    while pend:
                        pend.pop(0)()
                for tl_ in range(8):
                    t = c * 8 + tl_
                    sb_ = stn[0] % 2; stn[0] += 1
                    xs = stage[sb_][:, 0:1024]
                    P.dma(I("dma_start", out=xs, in_=X1[t * 128:(t + 1) * 128, :]), r=["X1"], w=["stage%d" % sb_])
                    P.dve(I("scalar_tensor_tensor", out=xs, in0=xs, scalar=ALPHA, in1=acc[:, tl_, :], op0=ALU.mult, op1=ALU.add), r=["stage%d" % sb_, "acc"], w=["stage%d" % sb_])
                    P.dma(I("dma_start", out=RR[t * 128:(t + 1) * 128, :], in_=xs), r=["stage%d" % sb_], w=["RR"])
            P.barrier()
            AR.reset(gmark)

        if "5" in phases:
            wpg = AR.alloc([128, 8, 1024], BF16); wpp = AR.alloc([128, 2, 1024], BF16)
            for kt in range(8):
                P.dma(I("dma_start", out=wpg[:, kt, :], in_=w_pg[kt * 128:(kt + 1) * 128, :]), w=["wpg"], q="gpsimd")
            for kt in range(2):
                P.dma(I("dma_start", out=wpp[:, kt, :], in_=w_pp[kt * 128:(kt + 1) * 128, :]), w=["wpp"], q="gpsimd")
            g2b = AR.alloc([128, 1024]); b2b = AR.alloc([128, 1024])
            bcast_row(g2b, ln2_g, 1024, "lng"); bcast_row(b2b, ln2_b, 1024, "lng")
            AR_t["st"] = AR.alloc([128, 12]); AR_t["mv"] = AR.alloc([128, 2]); AR_t["rs"] = AR.alloc([128, 1])
            rt = [AR.alloc([128, 1024]) for _ in range(2)]; ptl = [AR.alloc([128, 256]) for _ in range(2)]
            rT = AR.alloc([128, 8, 128], BF16); pT = AR.alloc([128, 2, 128], BF16)
            sgt = AR.alloc([128, 1024]); yv = AR.alloc([128, 1024]); yo = [AR.alloc([128, 1024]) for _ in range(2)]
            for t in range(32):
                b_ = t % 2
                P.dma(I("dma_start", out=rt[b_], in_=RR[t * 128:(t + 1) * 128, :]), r=["RR"], w=["rt%d" % b_])
                P.dma(I("dma_start", out=ptl[b_], in_=pc[t * 128:(t + 1) * 128, :]), w=["ptl%d" % b_])
                ptr = bank(4, 2).rearrange("p (a b) -> p a b", a=8)
                for kt in range(8):
                    P.pe(I("transpose", out=ptr[:, kt, :], in_=rt[b_][:, kt * 128:(kt + 1) * 128], identity=ident), r=["rt%d" % b_, "ident"], w=["ptr5"])
                P.act(I("activation", out=rT, in_=ptr, func=AF.Copy), r=["ptr5"], w=["rT"])
                ptp = bank(6).rearrange("p (a b) -> p a b", a=4)
                for kt in range(2):
                    P.pe(I("transpose", out=ptp[:, kt, :], in_=ptl[b_][:, kt * 128:(kt + 1) * 128], identity=ident), r=["ptl%d" % b_, "ident"], w=["ptp"])
                P.dve(I("tensor_copy", out=pT, in_=ptp[:, 0:2, :]), r=["ptp"], w=["pT"])
                pgt = bank(0, 2); ppp = bank(2, 2)
                for dh in range(2):
                    for kt in range(8):
                        P.pe(I("matmul", pgt[:, dh * 512:(dh + 1) * 512], lhsT=rT[:, kt, :], rhs=wpg[:, kt, dh * 512:(dh + 1) * 512], start=(kt == 0), stop=(kt == 7)), r=["rT", "wpg"], w=["pgt"])
                    for kt in range(2):
                        P.pe(I("matmul", ppp[:, dh * 512:(dh + 1) * 512], lhsT=pT[:, kt, :], rhs=wpp[:, kt, dh * 512:(dh + 1) * 512], start=(kt == 0), stop=(kt == 1)), r=["pT", "wpp"], w=["ppp"])
                P.act(I("activation", out=sgt, in_=pgt, func=AF.Sigmoid), r=["pgt"], w=["sgt"])
                P.dve(I("tensor_tensor", out=sgt, in0=sgt, in1=ppp, op=ALU.mult), r=["sgt", "ppp"], w=["sgt"])
                P.pool(I("tensor_tensor", out=yv, in0=sgt, in1=rt[b_], op=ALU.add), r=["sgt", "rt%d" % b_], w=["yv"])
                layer_norm_tile(yv, g2b, b2b, yo[b_], "yv")
                P.dma(I("dma_start", out=out[t * 128:(t + 1) * 128, :], in_=yo[b_]), r=["yvo"], w=["out"])
            P.barrier()

        P.emit(st)
    return nc


_NC_CACHE = {}


def _prep_inputs(inputs):
    sq = lambda k: np.ascontiguousarray(np.asarray(inputs[k])[0])
    x = np.asarray(inputs["x"]); p = np.asarray(inputs["p"])[0]
    shared = {
        "w_in": sq("w_in"), "lambda_q1": np.asarray(inputs["lambda_q1"]), "lambda_k1": np.asarray(inputs["lambda_k1"]),
        "lambda_q2": np.asarray(inputs["lambda_q2"]), "lambda_k2": np.asarray(inputs["lambda_k2"]),
        "subln_g": np.ascontiguousarray(np.asarray(inputs["subln_g"]).reshape(64, 1)),
        "ssm_a_re": sq("ssm_a_re"), "ssm_a_im": sq("ssm_a_im"), "ssm_log_dt": np.asarray(inputs["ssm_log_dt"]),
        "ssm_b_re": sq("ssm_b_re"), "ssm_b_im": sq("ssm_b_im"), "ssm_c_re": sq("ssm_c_re"), "ssm_c_im": sq("ssm_c_im"),
        "ssm_d": sq("ssm_d"), "w_glu": sq("w_glu"), "ssm_norm_g": sq("ssm_norm_g"), "w_out": sq("w_out"),
        "ln1_g": np.asarray(inputs["ln1_g"]), "ln1_b": np.asarray(inputs["ln1_b"]),
        "w_router": sq("w_router"), "b_router": np.asarray(inputs["b_router"]),
        "w_gate_up": sq("w_gate_up")[:NE], "b_gate_up": sq("b_gate_up"), "w_down": sq("w_down")[:NE], "b_down": sq("b_down"),
        "w_ple_gate": sq("w_ple_gate"), "w_ple_proj": sq("w_ple_proj"),
        "ln2_g": np.asarray(inputs["ln2_g"]), "ln2_b": np.asarray(inputs["ln2_b"]),
    }
    shared = {k: np.ascontiguousarray(v, dtype=np.float32) for k, v in shared.items()}
    maps = []
    for c in range(8):
        b, h = c // 2, c % 2
        if h == 0:
            xcore = np.concatenate([np.zeros((LO, DM), np.float32), x[b, :LO]], axis=0)
        else:
            xcore = np.ascontiguousarray(x[b])
        m = dict(shared)
        m["xc"] = np.ascontiguousarray(xcore, dtype=np.float32)
        m["pc"] = np.ascontiguousarray(p[b, h * LO:(h + 1) * LO], dtype=np.float32)
        m["pref"] = np.full((128, 1), NEG if h == 0 else 0.0, np.float32)
        maps.append(m)
    return maps


def kernel(**inputs):
    dbg = KDEBUG
    if dbg not in _NC_CACHE:
        _NC_CACHE[dbg] = build_program(dbg)
    nc = _NC_CACHE[dbg]
    maps = _prep_inputs(inputs)
    res = run_bass_kernel_spmd(nc, maps, core_ids=list(range(8)))
    if dbg:
        return res.results
    outp = np.zeros((4, L, DM), np.float32)
    for c in range(8):
        b, h = c // 2, c % 2
        outp[b, h * LO:(h + 1) * LO] = res.results[c]["out"]
    return outp
```

```python
import math
import os
from contextlib import ExitStack

import numpy as np
import concourse.bass as bass
import concourse.mybir as mybir
from concourse.bass_utils import run_bass_kernel_spmd

F32 = mybir.dt.float32
BF16 = mybir.dt.bfloat16
AF = mybir.ActivationFunctionType
ALU = mybir.AluOpType
AX = mybir.AxisListType

ENGINES = ("tensor", "vector", "scalar", "gpsimd", "sync")
SAME_ENGINE_SYNC = True
KDEBUG = os.environ.get("KDEBUG", "")

L = 8192
LO = 4096
DM = 1024
ALPHA = 2.0 ** 0.25
LAMBDA_INIT = 0.2
ATT_TH = 64.0
NEG = -30000.0
NE = int(os.environ.get('KNE', '32'))
NCH = int(os.environ.get('KNC', '4'))


KEYMAP = {
    "pb0": ["B0"], "pb1": ["B1"], "pbC": ["B2"], "pbu_re": ["B2"], "pbu_im": ["B3"], "py0": ["B4"], "py1": ["B5"], "pms": ["B6"],
    "sc0": ["B0", "B1"], "sc1": ["B2", "B3"], "Ooff": ["B4", "B5"], "Odg": ["B6", "B7"],
    "pm": ["B0", "B1"], "ptr": ["B2", "B3"], "pl": ["B4"],
    "pa": ["B0", "B1"], "pg": ["B6"], "pgl0": ["B0"], "pgl0l": ["B1"], "pgl1": ["B2"], "pgl1l": ["B3"],
    "pdn0": ["B4"], "pdn1": ["B5"], "pdn2": ["B6"], "pdn3": ["B7"],
    "ptr5": ["B4", "B5"], "ptp": ["B6"], "pgt": ["B0", "B1"], "ppp": ["B2", "B3"],
}


def _mapkeys(ks):
    out = []
    for k in ks:
        out.extend(KEYMAP.get(k, [k]))
    return out


class Prog:
    def __init__(self, nc, n_dma_sems=24):
        self.nc = nc
        self.ops = []
        self.last_w = {}
        self.readers = {}
        self.n_dma_sems = n_dma_sems
        self.last_of = {e: None for e in ENGINES}
        self.recent_dma = {e: [] for e in ENGINES}
        self.pe_sync = False

    def op(self, eng, fn, r=(), w=(), dma=False, extra=()):
        idx = len(self.ops)
        deps = set(extra)
        r = _mapkeys(r); w = _mapkeys(w)
        for k in r:
            if k in self.last_w:
                deps.add(self.last_w[k])
        for k in w:
            if k in self.last_w:
                deps.add(self.last_w[k])
            for x in self.readers.get(k, ()):
                deps.add(x)
        for k in w:
            self.last_w[k] = idx
            self.readers[k] = []
        for k in r:
            if k not in w:
                self.readers.setdefault(k, []).append(idx)
        deps.discard(idx)
        self.ops.append(dict(eng=eng, fn=fn, deps=deps, dma=dma, idx=idx, pesync=self.pe_sync))
        self.last_of[eng] = idx
        if dma:
            self.recent_dma[eng].append(idx)
            self.recent_dma[eng] = self.recent_dma[eng][-self.n_dma_sems:]
        return idx

    def pe(self, fn, r=(), w=(), tiled=False):
        extra = ()
        sv = self.pe_sync
        if tiled:
            self.pe_sync = True
            self.pe_was_tiled = True
        elif getattr(self, "pe_was_tiled", False):
            extra = (self.last_of["tensor"],)
            self.pe_sync = True
            self.pe_was_tiled = False
        i = self.op("tensor", fn, r, w, extra=extra)
        self.pe_sync = sv
        return i
    def dve(self, fn, r=(), w=()): return self.op("vector", fn, r, w)
    def act(self, fn, r=(), w=()): return self.op("scalar", fn, r, w)
    def pool(self, fn, r=(), w=()): return self.op("gpsimd", fn, r, w)
    def dma(self, fn, r=(), w=(), q="sync"): return self.op(q, fn, r, w, dma=True)

    def barrier(self):
        deps = set()
        for e in ENGINES:
            if self.last_of[e] is not None:
                deps.add(self.last_of[e])
            deps.update(self.recent_dma[e])
        for e in ENGINES:
            self.op(e, None, extra=tuple(deps))
        self.last_w = {}
        self.readers = {}

    def emit(self, stack):
        nc = self.nc
        ops = self.ops
        needed = set()
        for o in ops:
            for d in o["deps"]:
                if o["eng"] == "tensor" and ops[d]["eng"] == "tensor" and o["fn"] is not None and not o["pesync"]:
                    continue
                needed.add(d)
        for e in ENGINES:
            if self.last_of[e] is not None:
                needed.add(self.last_of[e])
        csem = {e: stack.enter_context(nc.semaphore(f"c_{e}")) for e in ENGINES}
        ccount = {e: 0 for e in ENGINES}
        dsems, dcount = {}, {}
        dnext = {e: 0 for e in ENGINES}
        for e in ("sync", "gpsimd", "scalar"):
            dsems[e] = [stack.enter_context(nc.semaphore(f"d_{e}_{i}")) for i in range(self.n_dma_sems)]
            dcount[e] = [0] * self.n_dma_sems
        for o in ops:
            e = o["eng"]
            if o["fn"] is None:
                o["sig"] = None
            elif o["dma"]:
                j = dnext[e]
                dnext[e] = (j + 1) % self.n_dma_sems
                o["dma_prev"] = (dsems[e][j], dcount[e][j])
                dcount[e][j] += 16
                o["sig"] = (dsems[e][j], dcount[e][j])
            elif o["idx"] in needed:
                ccount[e] += 1
                o["sig"] = (csem[e], ccount[e])
            else:
                o["sig"] = None
        def resolve(d, seen):
            od = ops[d]
            if od["fn"] is not None:
                return [d]
            out = []
            for dd in od["deps"]:
                if dd not in seen:
                    seen.add(dd)
                    out.extend(resolve(dd, seen))
            return out
        waited = {e: {} for e in ENGINES}
        per_eng = {e: [o for o in ops if o["eng"] == e] for e in ENGINES}
        block = stack.enter_context(nc.Block())

        def make(e):
            def body(eng):
                wd = waited[e]

                def wait(sem, val):
                    if val <= 0:
                        return
                    key = id(sem)
                    if wd.get(key, 0) >= val:
                        return
                    eng.wait_ge(sem, val)
                    wd[key] = val
                for o in per_eng[e]:
                    alld = []
                    seen = set()
                    for d in sorted(o["deps"]):
                        alld.extend(resolve(d, seen))
                    for d in sorted(set(alld)):
                        od = ops[d]
                        if od["sig"] is None:
                            continue
                        if (not od["dma"]) and od["eng"] == e and (not SAME_ENGINE_SYNC or (e == "tensor" and not o["pesync"])):
                            continue
                        wait(*od["sig"])
                    if o["fn"] is None:
                        continue
                    if o["dma"]:
                        wait(*o["dma_prev"])
                    ins = o["fn"](eng)
                    if o["sig"] is not None:
                        ins.then_inc(o["sig"][0], 16 if o["dma"] else 1)
                if e == "sync":
                    for q in dsems:
                        for j in range(self.n_dma_sems):
                            wait(dsems[q][j], dcount[q][j])
                    for e2 in ENGINES:
                        if e2 != "sync":
                            wait(csem[e2], ccount[e2])
            return body

        block.tensor(make("tensor"))
        block.vector(make("vector"))
        block.scalar(make("scalar"))
        block.gpsimd(make("gpsimd"))
        block.sync(make("sync"))


def I(name, *a, **k):
    return lambda e: getattr(e, name)(*a, **k)


class Arena:
    def __init__(self, ap_f32, nbytes):
        self.ap = ap_f32
        self.n = nbytes
        self.off = 0

    def mark(self): return self.off
    def reset(self, m): self.off = m

    def alloc(self, shape, dt=F32, parts=128):
        esz = 4 if dt == F32 else 2
        n = int(np.prod(shape[1:])) * esz
        n4 = (n + 3) // 4
        assert self.off + n4 * 4 <= self.n, f"arena overflow {self.off + n4 * 4} > {self.n}"
        a = self.ap[0:shape[0], self.off // 4:self.off // 4 + n4]
        self.off += n4 * 4
        if dt != F32:
            a = a.bitcast(dt)
        if len(shape) == 3:
            a = a.rearrange("p (a b) -> p a b", a=shape[1])
        elif len(shape) == 4:
            a = a.rearrange("p (a b c) -> p a b c", a=shape[1], b=shape[2])
        return a


def _qkey_lo(q0, slope):
    kmin = q0 - ATT_TH / slope
    return max(0, int(math.floor(kmin / 128.0)))


def build_program(dbg=""):
    nc = bass.Bass("TRN2", target_bir_lowering=False)
    D = {}

    def din(name, shape, dt=F32):
        D[name] = nc.dram_tensor(name, list(shape), dt, kind="ExternalInput").ap()
        return D[name]

    def dscr(name, shape, dt):
        kind = "ExternalOutput" if name in dbg.split(",") else "Internal"
        D[name] = nc.dram_tensor(name, list(shape), dt, kind=kind).ap()
        return D[name]

    xc = din("xc", [L, DM]); pc = din("pc", [LO, 256]); pref = din("pref", [128, 1])
    w_in = din("w_in", [1024, 2048])
    lq1 = din("lambda_q1", [1, 32]); lk1 = din("lambda_k1", [1, 32]); lq2 = din("lambda_q2", [1, 32]); lk2 = din("lambda_k2", [1, 32])
    subln_g = din("subln_g", [64, 1])
    a_re = din("ssm_a_re", [32, 64]); a_im = din("ssm_a_im", [32, 64]); log_dt = din("ssm_log_dt", [1, 32])
    b_re = din("ssm_b_re", [32, 64, 16]); b_im = din("ssm_b_im", [32, 64, 16])
    c_re = din("ssm_c_re", [32, 16, 64]); c_im = din("ssm_c_im", [32, 16, 64])
    ssm_d = din("ssm_d", [512]); w_glu = din("w_glu", [512, 1024]); ssm_norm_g = din("ssm_norm_g", [512])
    w_out = din("w_out", [1024, 1024]); ln1_g = din("ln1_g", [1, 1024]); ln1_b = din("ln1_b", [1, 1024])
    w_router = din("w_router", [1024, 32]); b_router = din("b_router", [1, 32])
    phases = os.environ.get("KPHASES", "12345")
    if "4" in phases:
        w_gu = din("w_gate_up", [NE, 1024, 2048]); b_gu = din("b_gate_up", [32, 2048])
        w_dn = din("w_down", [NE, 1024, 1024]); b_dn = din("b_down", [32, 1024])
    w_pg = din("w_ple_gate", [1024, 1024]); w_pp = din("w_ple_proj", [256, 1024])
    ln2_g = din("ln2_g", [1, 1024]); ln2_b = din("ln2_b", [1, 1024])
    out = nc.dram_tensor("out", [LO, DM], F32, kind="ExternalOutput").ap()

    KT = dscr("KT", [4, 128, L], BF16); QT = dscr("QT", [4, 128, LO], BF16)
    VV = dscr("VV", [64, 128, 520], BF16)
    MIXT = dscr("MIXT", [8, 128, LO], BF16)
    X1 = dscr("X1", [LO, DM], F32); X1T = dscr("X1T", [8, 128, LO], BF16)
    RR = dscr("RR", [LO, DM], F32)

    with ExitStack() as st:
        st.enter_context(nc.allow_non_contiguous_dma(reason="layout"))
        ARENA_B = 206 * 1024
        arena_t = st.enter_context(nc.sbuf_tensor("arena", [128, ARENA_B // 4], F32))
        psum_t = st.enter_context(nc.psum_tensor("psum", [128, 4096], F32))
        AR = Arena(arena_t, ARENA_B)
        P = Prog(nc)

        def bank(i, n=1):
            return psum_t[:, 512 * i:512 * (i + n)]

        ident = AR.alloc([128, 128]); ones = AR.alloc([128, 128])
        P.pool(I("memset", ident, 1.0), w=["ident"])
        P.pool(I("affine_select", out=ident, in_=ident, pattern=[[-1, 128]], compare_op=ALU.is_equal,
                                         fill=0.0, base=0, channel_multiplier=1), r=["ident"], w=["ident"])
        P.pool(I("memset", ones, 1.0), w=["ones"])
        gmark = AR.mark()

        if "1" in phases:
            P.pe_sync = False
            win = AR.alloc([128, 8, 2048], BF16)
            for kt in range(8):
                P.dma(I("dma_start", out=win[:, kt, :], in_=w_in[kt * 128:(kt + 1) * 128, :]), w=["win"], q="gpsimd")
            wglu = AR.alloc([128, 4, 1024], BF16)
            for kt in range(4):
                P.dma(I("dma_start", out=wglu[:, kt, :], in_=w_glu[kt * 128:(kt + 1) * 128, :]), w=["wglu"], q="gpsimd")
            sm = lambda: AR.alloc([128, 16])
            are, aim, dtt, rho, th, cth, sth, Are, Aim, t0, t1, t2, Fre, Fim, d2 = [sm() for _ in range(15)]
            P.dma(I("dma_start", out=are, in_=a_re.rearrange("(gp g2) p -> (g2 p) gp", g2=2)), w=["are"])
            P.dma(I("dma_start", out=aim, in_=a_im.rearrange("(gp g2) p -> (g2 p) gp", g2=2)), w=["aim"])
            ldv = log_dt.rearrange("o (gp g2) -> o g2 gp", g2=2)
            for g2 in range(2):
                P.dma(I("dma_start", out=dtt[64 * g2:64 * g2 + 64, :], in_=ldv[:, g2, :].to_broadcast([64, 16])), w=["dtt"])
            P.act(I("activation", out=dtt, in_=dtt, func=AF.Exp), r=["dtt"], w=["dtt"])
            P.dve(I("tensor_tensor", out=t0, in0=are, in1=dtt, op=ALU.mult), r=["are", "dtt"], w=["t0"])
            P.act(I("activation", out=rho, in_=t0, func=AF.Exp), r=["t0"], w=["rho"])
            P.dve(I("tensor_tensor", out=th, in0=aim, in1=dtt, op=ALU.mult), r=["aim", "dtt"], w=["th"])
            P.dve(I("memset", t1, 0.0), w=["t1"])
            for kk in range(6):
                thr = (2 * kk + 1) * math.pi
                P.dve(I("tensor_scalar", out=t2, in0=th, scalar1=-thr, scalar2=1e6, op0=ALU.add, op1=ALU.mult), r=["th"], w=["t2"])
                P.dve(I("tensor_scalar", out=t2, in0=t2, scalar1=0.0, scalar2=1.0, op0=ALU.max, op1=ALU.min), r=["t2"], w=["t2"])
                P.dve(I("tensor_tensor", out=t1, in0=t1, in1=t2, op=ALU.add), r=["t1", "t2"], w=["t1"])
            P.dve(I("scalar_tensor_tensor", out=th, in0=t1, scalar=-2.0 * math.pi, in1=th, op0=ALU.mult, op1=ALU.add), r=["t1", "th"], w=["th"])
            P.act(I("activation", out=sth, in_=th, func=AF.Sin), r=["th"], w=["sth"])
            P.dve(I("tensor_scalar", out=t2, in0=th, scalar1=-1.0, scalar2=None, op0=ALU.mult), r=["th"], w=["t2"])
            P.dve(I("tensor_tensor", out=t2, in0=t2, in1=th, op=ALU.max), r=["t2", "th"], w=["t2"])
            P.dve(I("tensor_scalar", out=t2, in0=t2, scalar1=-1.0, scalar2=math.pi / 2, op0=ALU.mult, op1=ALU.add), r=["t2"], w=["t2"])
            P.act(I("activation", out=cth, in_=t2, func=AF.Sin), r=["t2"], w=["cth"])
            P.dve(I("tensor_tensor", out=Are, in0=rho, in1=cth, op=ALU.mult), r=["rho", "cth"], w=["Are"])
            P.dve(I("tensor_tensor", out=Aim, in0=rho, in1=sth, op=ALU.mult), r=["rho", "sth"], w=["Aim"])
            P.dve(I("tensor_scalar", out=t0, in0=Are, scalar1=-1.0, scalar2=None, op0=ALU.add), r=["Are"], w=["t0"])
            P.dve(I("tensor_tensor", out=d2, in0=are, in1=are, op=ALU.mult), r=["are"], w=["d2"])
            P.dve(I("tensor_tensor", out=t1, in0=aim, in1=aim, op=ALU.mult), r=["aim"], w=["t1"])
            P.dve(I("tensor_tensor", out=d2, in0=d2, in1=t1, op=ALU.add), r=["d2", "t1"], w=["d2"])
            P.dve(I("reciprocal", out=d2, in_=d2), r=["d2"], w=["d2"])
            P.dve(I("tensor_tensor", out=t1, in0=t0, in1=are, op=ALU.mult), r=["t0", "are"], w=["t1"])
            P.dve(I("tensor_tensor", out=t2, in0=Aim, in1=aim, op=ALU.mult), r=["Aim", "aim"], w=["t2"])
            P.dve(I("tensor_tensor", out=t1, in0=t1, in1=t2, op=ALU.add), r=["t1", "t2"], w=["t1"])
            P.dve(I("tensor_tensor", out=Fre, in0=t1, in1=d2, op=ALU.mult), r=["t1", "d2"], w=["Fre"])
            P.dve(I("tensor_tensor", out=t1, in0=Aim, in1=are, op=ALU.mult), r=["Aim", "are"], w=["t1"])
            P.dve(I("tensor_tensor", out=t2, in0=t0, in1=aim, op=ALU.mult), r=["t0", "aim"], w=["t2"])
            P.dve(I("tensor_tensor", out=t1, in0=t1, in1=t2, op=ALU.subtract), r=["t1", "t2"], w=["t1"])
            P.dve(I("tensor_tensor", out=Fim, in0=t1, in1=d2, op=ALU.mult), r=["t1", "d2"], w=["Fim"])
            Bre = AR.alloc([128, 16, 16]); Bim = AR.alloc([128, 16, 16]); Bbr = AR.alloc([128, 16, 16]); Bbi = AR.alloc([128, 16, 16]); Bt = AR.alloc([128, 16, 16])
            P.dma(I("dma_start", out=Bre, in_=b_re.rearrange("(gp g2) p c -> (g2 p) gp c", g2=2)), w=["Bre"])
            P.dma(I("dma_start", out=Bim, in_=b_im.rearrange("(gp g2) p c -> (g2 p) gp c", g2=2)), w=["Bim"])
            bc = lambda a: a.unsqueeze(2).to_broadcast([128, 16, 16])
            P.dve(I("tensor_tensor", out=Bbr, in0=Bre, in1=bc(Fre), op=ALU.mult), r=["Bre", "Fre"], w=["Bbr"])
            P.dve(I("tensor_tensor", out=Bt, in0=Bim, in1=bc(Fim), op=ALU.mult), r=["Bim", "Fim"], w=["Bt"])
            P.dve(I("tensor_tensor", out=Bbr, in0=Bbr, in1=Bt, op=ALU.subtract), r=["Bbr", "Bt"], w=["Bbr"])
            P.dve(I("tensor_tensor", out=Bbi, in0=Bim, in1=bc(Fre), op=ALU.mult), r=["Bim", "Fre"], w=["Bbi"])
            P.dve(I("tensor_tensor", out=Bt, in0=Bre, in1=bc(Fim), op=ALU.mult), r=["Bre", "Fim"], w=["Bt"])
            P.dve(I("tensor_tensor", out=Bbi, in0=Bbi, in1=Bt, op=ALU.add), r=["Bbi", "Bt"], w=["Bbi"])
            Bp = [AR.alloc([128, 16, 128], BF16), AR.alloc([128, 16, 128], BF16)]
            m1 = AR.mark()
            Lx = AR.alloc([128, 16, 128])
            for ri, Bb in enumerate((Bbr, Bbi)):
                P.dve(I("memset", Lx, 0.0), w=["Lx"])
                Lv = Lx.rearrange("q gp (l g c) -> q gp l g c", l=4, g=2)
                for g2 in range(2):
                    P.dve(I("tensor_copy",
                        out=Lv[64 * g2:64 * g2 + 64, :, :, g2, :],
                        in_=Bb[64 * g2:64 * g2 + 64].unsqueeze(2).to_broadcast([64, 16, 4, 16])), r=["Bbr", "Bbi"], w=["Lx"])
                for g4 in range(4):
                    pb = bank(g4 % 2).rearrange("p (a b) -> p a b", a=4)
                    for j in range(4):
                        gp = g4 * 4 + j
                        P.pe(I("transpose", out=pb[:, j, :], in_=Lx[:, gp, :], identity=ident), r=["Lx", "ident"], w=["pb%d" % (g4 % 2)])
                    P.dve(I("tensor_copy", out=Bp[ri][:, g4 * 4:g4 * 4 + 4, :], in_=pb), r=["pb%d" % (g4 % 2)], w=["Bp"])
            AR.reset(m1)
            Cr = AR.alloc([128, 16, 32]); Cni = AR.alloc([128, 16, 32])
            m1 = AR.mark()
            Cx = AR.alloc([128, 16, 128])
            for ri, csrc in enumerate((c_re, c_im)):
                P.dve(I("memset", Cx[0:32], 0.0), w=["Cx"])
                cv = csrc.rearrange("(gp g2) c p -> g2 c gp p", g2=2)
                for g2 in range(2):
                    P.dma(I("dma_start", out=Cx[16 * g2:16 * g2 + 16, :, 64 * g2:64 * g2 + 64], in_=cv[g2]), r=["Cx"], w=["Cx"])
                pb = bank(2).rearrange("p (a b) -> p a b", a=16)
                for gp in range(16):
                    P.pe(I("transpose", out=pb[:, gp, :], in_=Cx[0:32, gp, :], identity=ident[0:32, 0:32]), r=["Cx", "ident"], w=["pbC"])
                if ri == 0:
                    P.dve(I("tensor_copy", out=Cr, in_=pb), r=["pbC"], w=["Cr"])
                else:
                    P.dve(I("tensor_scalar", out=Cni, in0=pb, scalar1=-1.0, scalar2=None, op0=ALU.mult), r=["pbC"], w=["Cni"])
            AR.reset(m1)
            cn = AR.alloc([128, 16, 128]); sn = AR.alloc([128, 16, 128]); tA = AR.alloc([128, 16, 64]); tB = AR.alloc([128, 16, 64])
            P.dve(I("tensor_copy", out=cn[:, :, 0:1], in_=cth.unsqueeze(2)), r=["cth"], w=["cn"])
            P.dve(I("tensor_copy", out=sn[:, :, 0:1], in_=sth.unsqueeze(2)), r=["sth"], w=["sn"])
            m = 1
            while m < 128:
                cm = cn[:, :, m - 1:m].to_broadcast([128, 16, m]); smm = sn[:, :, m - 1:m].to_broadcast([128, 16, m])
                ta = tA[:, :, 0:m]; tb = tB[:, :, 0:m]
                P.dve(I("tensor_tensor", out=ta, in0=cn[:, :, 0:m], in1=cm, op=ALU.mult), r=["cn"], w=["tA"])
                P.dve(I("tensor_tensor", out=tb, in0=sn[:, :, 0:m], in1=smm, op=ALU.mult), r=["sn"], w=["tB"])
                P.dve(I("tensor_tensor", out=ta, in0=ta, in1=tb, op=ALU.subtract), r=["tA", "tB"], w=["tA"])
                P.dve(I("tensor_tensor", out=tb, in0=cn[:, :, 0:m], in1=smm, op=ALU.mult), r=["cn", "sn"], w=["tB"])
                P.dve(I("tensor_copy", out=cn[:, :, m:2 * m], in_=ta), r=["tA"], w=["cn"])
                P.dve(I("tensor_tensor", out=ta, in0=sn[:, :, 0:m], in1=cm, op=ALU.mult), r=["sn", "cn"], w=["tA"])
                P.dve(I("tensor_tensor", out=sn[:, :, m:2 * m], in0=ta, in1=tb, op=ALU.add), r=["tA", "tB"], w=["sn"])
                m *= 2
            dcol = AR.alloc([128, 4]); gncol = AR.alloc([128, 4])
            P.dma(I("dma_start", out=dcol, in_=ssm_d.rearrange("(ut r) -> r ut", r=128)), w=["dcol"])
            P.dma(I("dma_start", out=gncol, in_=ssm_norm_g.rearrange("(ut r) -> r ut", r=128)), w=["gncol"])
            Sre = AR.alloc([128, 16]); Sim = AR.alloc([128, 16])
            P.dve(I("memset", Sre, 0.0), w=["Sre"]); P.dve(I("memset", Sim, 0.0), w=["Sim"])
            xin = [AR.alloc([128, 1024]) for _ in range(2)]
            xT = [AR.alloc([128, 8, 512], BF16) for _ in range(2)]
            kst = AR.alloc([128, 4, 512], BF16); qst = AR.alloc([128, 4, 512], BF16)
            vst = [AR.alloc([128, 8, 65], BF16) for _ in range(2)]
            for b_ in range(2):
                P.dve(I("memset", vst[b_], 1.0), w=["vst%d" % b_])
            uT = AR.alloc([128, 4, 512], BF16)
            vre = AR.alloc([128, 4, 128]); vim = AR.alloc([128, 4, 128]); e1 = AR.alloc([128, 4, 128]); e2 = AR.alloc([128, 4, 128])
            wre = AR.alloc([128, 4, 128]); wim = AR.alloc([128, 4, 128]); sre = AR.alloc([128, 4, 128]); sim = AR.alloc([128, 4, 128])
            yd = AR.alloc([128, 512]); g1 = AR.alloc([128, 512]); g2t = AR.alloc([128, 512])
            glT = AR.alloc([128, 4, 512], BF16)
            y2 = AR.alloc([128, 4, 512]); sq = AR.alloc([128, 512]); sig = AR.alloc([128, 512]); rstd = AR.alloc([128, 512])
            soT = AR.alloc([128, 4, 512], BF16)
            pbu_re = bank(2).rearrange("p (a b) -> p a b", a=4); pbu_im = bank(3).rearrange("p (a b) -> p a b", a=4)
            for s in range(16):
                own = s >= 8
                xt = xT[s % 2]; xk = "xT%d" % (s % 2)
                for ti in range(4):
                    xb = xin[ti % 2]; xbk = "xin%d" % (ti % 2)
                    r0 = s * 512 + ti * 128
                    P.dma(I("dma_start", out=xb, in_=xc[r0:r0 + 128, :]), w=[xbk])
                    for half in range(2):
                        pb = bank(half).rearrange("p (a b) -> p a b", a=4)
                        for j in range(4):
                            kt = half * 4 + j
                            P.pe(I("transpose", out=pb[:, j, :], in_=xb[:, kt * 128:(kt + 1) * 128], identity=ident), r=[xbk, "ident"], w=["pb%d" % half])
                        if half == 0:
                            P.dve(I("tensor_copy", out=xt[:, 0:4, ti * 128:(ti + 1) * 128], in_=pb), r=["pb0"], w=[xk])
                        else:
                            P.act(I("activation", out=xt[:, 4:8, ti * 128:(ti + 1) * 128], in_=pb, func=AF.Copy), r=["pb1"], w=[xk])
                def proj_fm(col0, dst, dk, n_m=4):
                    for mt in range(n_m):
                        pb = bank(mt % 2); pk = "pb%d" % (mt % 2)
                        for kt in range(8):
                            P.pe(I("matmul", pb, lhsT=win[:, kt, col0 + mt * 128:col0 + (mt + 1) * 128], rhs=xt[:, kt, :], start=(kt == 0), stop=(kt == 7)), r=["win", xk], w=[pk])
                        if mt % 2 == 0:
                            P.dve(I("tensor_copy", out=dst[:, mt, :], in_=pb), r=[pk], w=[dk])
                        else:
                            P.act(I("activation", out=dst[:, mt, :], in_=pb, func=AF.Copy), r=[pk], w=[dk])
                proj_fm(512, kst, "kst")
                P.dma(I("dma_start", out=KT[:, :, s * 512:(s + 1) * 512].rearrange("h p t -> p h t"), in_=kst), r=["kst"], w=["KT"])
                if own:
                    proj_fm(0, qst, "qst")
                    P.dma(I("dma_start", out=QT[:, :, (s - 8) * 512:(s - 7) * 512].rearrange("h p t -> p h t"), in_=qst), r=["qst"], w=["QT"])
                for ti in range(4):
                    pb = bank(ti % 2); pk = "pb%d" % (ti % 2)
                    vb = vst[ti % 2]; vk = "vst%d" % (ti % 2)
                    for kt in range(8):
                        P.pe(I("matmul", pb, lhsT=xt[:, kt, ti * 128:(ti + 1) * 128], rhs=win[:, kt, 1024:1536], start=(kt == 0), stop=(kt == 7)), r=["win", xk], w=[pk])
                    P.dve(I("tensor_copy", out=vb[:, :, 0:64], in_=pb.rearrange("p (h d) -> p h d", h=8)), r=[pk], w=[vk])
                    P.dma(I("dma_start", out=VV[s * 4 + ti], in_=vb.rearrange("p h d -> p (h d)")), r=[vk], w=["VV"])
                proj_fm(1536, uT, "uT")
                for ut in range(4):
                    py = bank(4 + ut % 2); pyk = "py%d" % (ut % 2)
                    for un in range(4):
                        tsl = slice(un * 128, (un + 1) * 128)
                        for gl in range(4):
                            gp = ut * 4 + gl
                            P.pe(I("matmul", pbu_re[:, gl, :], lhsT=Bp[0][32 * gl:32 * gl + 32, gp, :], rhs=uT[32 * gl:32 * gl + 32, ut, tsl], start=True, stop=True, tile_position=(32 * gl, 0)), r=["Bp", "uT"], w=["pbu_re"], tiled=True)
                            P.pe(I("matmul", pbu_im[:, gl, :], lhsT=Bp[1][32 * gl:32 * gl + 32, gp, :], rhs=uT[32 * gl:32 * gl + 32, ut, tsl], start=True, stop=True, tile_position=(32 * gl, 0)), r=["Bp", "uT"], w=["pbu_im"], tiled=True)
                        g4 = slice(ut * 4, ut * 4 + 4)
                        cnv = cn[:, g4, :]; snv = sn[:, g4, :]
                        P.dve(I("tensor_tensor", out=vre, in0=pbu_re, in1=cnv, op=ALU.mult), r=["pbu_re", "cn"], w=["vre"])
                        P.dve(I("tensor_tensor", out=e1, in0=pbu_im, in1=snv, op=ALU.mult), r=["pbu_im", "sn"], w=["e1"])
                        P.dve(I("tensor_tensor", out=vim, in0=pbu_im, in1=cnv, op=ALU.mult), r=["pbu_im", "cn"], w=["vim"])
                        P.dve(I("tensor_tensor", out=e2, in0=pbu_re, in1=snv, op=ALU.mult), r=["pbu_re", "sn"], w=["e2"])
                        P.pool(I("tensor_tensor", out=vre, in0=vre, in1=e1, op=ALU.add), r=["vre", "e1"], w=["vre"])
                        P.pool(I("tensor_tensor", out=vim, in0=vim, in1=e2, op=ALU.subtract), r=["vim", "e2"], w=["vim"])
                        for gl in range(4):
                            gp = ut * 4 + gl
                            P.dve(I("tensor_tensor_scan", out=wre[:, gl, :], data0=rho[:, gp:gp + 1].to_broadcast([128, 128]), data1=vre[:, gl, :], initial=Sre[:, gp:gp + 1], op0=ALU.mult, op1=ALU.add), r=["vre", "rho", "Sre"], w=["wre"])
                            P.dve(I("tensor_tensor_scan", out=wim[:, gl, :], data0=rho[:, gp:gp + 1].to_broadcast([128, 128]), data1=vim[:, gl, :], initial=Sim[:, gp:gp + 1], op0=ALU.mult, op1=ALU.add), r=["vim", "rho", "Sim"], w=["wim"])
                        if own:
                            cs, ws = slice(0, 128), slice(0, 4)
                        else:
                            cs, ws = slice(127, 128), slice(0, 4)
                        P.pool(I("tensor_tensor", out=sre[:, :, cs], in0=wre[:, :, cs], in1=cnv[:, :, cs], op=ALU.mult), r=["wre", "cn"], w=["sre"])
                        P.pool(I("tensor_tensor", out=e1[:, :, cs], in0=wim[:, :, cs], in1=snv[:, :, cs], op=ALU.mult), r=["wim", "sn"], w=["e1"])
                        P.pool(I("tensor_tensor", out=sre[:, :, cs], in0=sre[:, :, cs], in1=e1[:, :, cs], op=ALU.subtract), r=["sre", "e1"], w=["sre"])
                        P.pool(I("tensor_tensor", out=sim[:, :, cs], in0=wim[:, :, cs], in1=cnv[:, :, cs], op=ALU.mult), r=["wim", "cn"], w=["sim"])
                        P.pool(I("tensor_tensor", out=e2[:, :, cs], in0=wre[:, :, cs], in1=snv[:, :, cs], op=ALU.mult), r=["wre", "sn"], w=["e2"])
                        P.pool(I("tensor_tensor", out=sim[:, :, cs], in0=sim[:, :, cs], in1=e2[:, :, cs], op=ALU.add), r=["sim", "e2"], w=["sim"])
                        P.dve(I("tensor_copy", out=Sre[:, g4], in_=sre[:, :, 127]), r=["sre"], w=["Sre"])
                        P.dve(I("tensor_copy", out=Sim[:, g4], in_=sim[:, :, 127]), r=["sim"], w=["Sim"])
                        if own:
                            for gl in range(4):
                                gp = ut * 4 + gl
                                P.pe(I("matmul", py[32 * gl:32 * gl + 32, tsl], lhsT=Cr[:, gp, :], rhs=sre[:, gl, :], start=True, stop=False, tile_position=(0, 32 * gl)), r=["Cr", "sre"], w=[pyk], tiled=True)
                                P.pe(I("matmul", py[32 * gl:32 * gl + 32, tsl], lhsT=Cni[:, gp, :], rhs=sim[:, gl, :], start=False, stop=True, tile_position=(0, 32 * gl)), r=["Cni", "sim"], w=[pyk], tiled=True)
                    if own:
                        P.dve(I("scalar_tensor_tensor", out=yd, in0=uT[:, ut, :], scalar=dcol[:, ut:ut + 1], in1=py, op0=ALU.mult, op1=ALU.add), r=["uT", "dcol", pyk], w=["yd"])
                        P.pool(I("tensor_tensor", out=g1, in0=yd, in1=yd, op=ALU.mult), r=["yd"], w=["g1"])
                        P.pool(I("tensor_scalar", out=g1, in0=g1, scalar1=0.044715, scalar2=1.0, op0=ALU.mult, op1=ALU.add), r=["g1"], w=["g1"])
                        P.pool(I("tensor_tensor", out=g1, in0=g1, in1=yd, op=ALU.mult), r=["g1", "yd"], w=["g1"])
                        P.act(I("activation", out=g2t, in_=g1, func=AF.Sigmoid, scale=2.0 * math.sqrt(2.0 / math.pi)), r=["g1"], w=["g2t"])
                        P.pool(I("tensor_tensor", out=glT[:, ut, :], in0=yd, in1=g2t, op=ALU.mult), r=["yd", "g2t"], w=["glT"])
                if own:
                    for mo in range(4):
                        plo = bank(0); phi = bank(1)
                        for kt in range(4):
                            P.pe(I("matmul", plo, lhsT=wglu[:, kt, mo * 128:(mo + 1) * 128], rhs=glT[:, kt, :], start=(kt == 0), stop=(kt == 3)), r=["wglu", "glT"], w=["pb0"])
                        for kt in range(4):
                            P.pe(I("matmul", phi, lhsT=wglu[:, kt, 512 + mo * 128:512 + (mo + 1) * 128], rhs=glT[:, kt, :], start=(kt == 0), stop=(kt == 3)), r=["wglu", "glT"], w=["pb1"])
                        P.act(I("activation", out=sig, in_=phi, func=AF.Sigmoid), r=["pb1"], w=["sig"])
                        P.dve(I("tensor_tensor", out=y2[:, mo, :], in0=plo, in1=sig, op=ALU.mult), r=["pb0", "sig"], w=["y2"])
                    pms = bank(6)
                    for mo in range(4):
                        P.pool(I("tensor_tensor", out=sq, in0=y2[:, mo, :], in1=y2[:, mo, :], op=ALU.mult), r=["y2"], w=["sq"])
                        P.pe(I("matmul", pms, lhsT=ones, rhs=sq, start=(mo == 0), stop=(mo == 3)), r=["ones", "sq"], w=["pms"])
                    P.act(I("activation", out=rstd, in_=pms, func=AF.Ln, scale=1.0 / 512.0, bias=1e-5), r=["pms"], w=["rstd"])
                    P.act(I("activation", out=rstd, in_=rstd, func=AF.Exp, scale=-0.5), r=["rstd"], w=["rstd"])
                    for mo in range(4):
                        P.dve(I("scalar_tensor_tensor", out=soT[:, mo, :], in0=y2[:, mo, :], scalar=gncol[:, mo:mo + 1], in1=rstd, op0=ALU.mult, op1=ALU.mult), r=["y2", "gncol", "rstd"], w=["soT"])
                    P.dma(I("dma_start", out=MIXT[4:8, :, (s - 8) * 512:(s - 7) * 512].rearrange("h p t -> p h t"), in_=soT), r=["soT"], w=["MIXT"])
            P.barrier()
            AR.reset(gmark)


        P.pe_sync = False
        G = AR.alloc([128, 32, 32])
        gmark = AR.mark()
        SC = 1.0 / math.sqrt(32.0)

        def layer_norm_tile(pre, gb, bb, dst, tg):
            stt = AR_t["st"]; mv = AR_t["mv"]; rs = AR_t["rs"]
            P.dve(I("bn_stats", out=stt[:, 0:6], in_=pre[:, 0:512]), r=[tg], w=["lnst"])
            P.dve(I("bn_stats", out=stt[:, 6:12], in_=pre[:, 512:1024]), r=[tg], w=["lnst2"])
            P.dve(I("bn_aggr", out=mv, in_=stt), r=["lnst", "lnst2"], w=["lnmv"])
            P.act(I("activation", out=rs, in_=mv[:, 1:2], func=AF.Ln, bias=1e-5), r=["lnmv"], w=["lnrs"])
            P.act(I("activation", out=rs, in_=rs, func=AF.Exp, scale=-0.5), r=["lnrs"], w=["lnrs"])
            P.dve(I("tensor_scalar", out=dst, in0=pre, scalar1=mv[:, 0:1], scalar2=rs[:, 0:1], op0=ALU.subtract, op1=ALU.mult), r=[tg, "lnmv", "lnrs"], w=[tg + "o"])
            P.pool(I("tensor_tensor", out=dst, in0=dst, in1=gb, op=ALU.mult), r=[tg + "o", "lng"], w=[tg + "o"])
            P.pool(I("tensor_tensor", out=dst, in0=dst, in1=bb, op=ALU.add), r=[tg + "o", "lng"], w=[tg + "o"])
        AR_t = {}

        if "2" in phases:
            slopes = [2.0 ** (-(h + 1)) for h in range(8)]
            tri = AR.alloc([128, 128]); trib = AR.alloc([128, 128], BF16); U = AR.alloc([128, 128]); kl = AR.alloc([128, 1])
            P.pool(I("memset", tri, 1.0), w=["tri"])
            P.pool(I("affine_select", out=tri, in_=tri, pattern=[[1, 128]], compare_op=ALU.is_ge, fill=0.0, base=0, channel_multiplier=-1), r=["tri"], w=["tri"])
            P.dve(I("tensor_copy", out=trib, in_=tri), r=["tri"], w=["trib"])
            P.pool(I("memset", U, 1.0), w=["U"])
            P.pool(I("affine_select", out=U, in_=U, pattern=[[1, 128]], compare_op=ALU.is_gt, fill=0.0, base=0, channel_multiplier=-1), r=["U"], w=["U"])
            P.pe(I("matmul", bank(0)[:, 0:1], lhsT=U, rhs=ones[:, 0:1], start=True, stop=True), r=["U", "ones"], w=["pb0"])
            P.dve(I("tensor_copy", out=kl, in_=bank(0)[:, 0:1]), r=["pb0"], w=["kl"])
            ND = 68
            bt = AR.alloc([128, 8, ND]); btp = AR.alloc([128, 8, ND]); prefc = AR.alloc([128, 1])
            P.dma(I("dma_start", out=prefc, in_=pref), w=["prefc"])
            for h in range(8):
                for Dd in range(ND):
                    fn = P.dve if (Dd % 2 == 0) else P.pool
                    fn(I("tensor_scalar", out=bt[:, h, Dd:Dd + 1], in0=kl, scalar1=slopes[h], scalar2=-slopes[h] * 128.0 * Dd, op0=ALU.mult, op1=ALU.add), r=["kl"], w=["bt%d" % (Dd % 2)])
            P.dve(I("tensor_scalar", out=btp, in0=bt, scalar1=prefc[:, 0:1], scalar2=None, op0=ALU.add), r=["bt0", "bt1", "prefc"], w=["btp"])
            lv = [AR.alloc([64, 32]) for _ in range(4)]; lt = AR.alloc([64, 32]); l1 = AR.alloc([64, 1]); l2 = AR.alloc([64, 1]); neglam = AR.alloc([64, 1]); gcol = AR.alloc([64, 1])
            for i_, src in enumerate((lq1, lk1, lq2, lk2)):
                P.dma(I("dma_start", out=lv[i_], in_=src.to_broadcast([64, 32])), w=["lv%d" % i_])
            P.dve(I("tensor_tensor", out=lt, in0=lv[0], in1=lv[1], op=ALU.mult), r=["lv0", "lv1"], w=["lt"])
            P.dve(I("reduce_sum", out=l1, in_=lt, axis=AX.X), r=["lt"], w=["l1"])
            P.dve(I("tensor_tensor", out=lt, in0=lv[2], in1=lv[3], op=ALU.mult), r=["lv2", "lv3", "l1"], w=["lt"])
            P.dve(I("reduce_sum", out=l2, in_=lt, axis=AX.X), r=["lt"], w=["l2"])
            P.act(I("activation", out=l1, in_=l1, func=AF.Exp), r=["l1"], w=["l1"])
            P.act(I("activation", out=l2, in_=l2, func=AF.Exp), r=["l2"], w=["l2"])
            P.dve(I("tensor_tensor", out=neglam, in0=l2, in1=l1, op=ALU.subtract), r=["l1", "l2"], w=["neglam"])
            P.dve(I("tensor_scalar", out=neglam, in0=neglam, scalar1=-LAMBDA_INIT, scalar2=None, op0=ALU.add), r=["neglam"], w=["neglam"])
            P.dma(I("dma_start", out=gcol, in_=subln_g), w=["gcol"])
            P.dve(I("tensor_scalar", out=gcol, in0=gcol, scalar1=1.0 - LAMBDA_INIT, scalar2=None, op0=ALU.mult), r=["gcol"], w=["gcol"])
            sel = AR.alloc([65, 64])
            P.dve(I("memset", sel, 0.0), w=["sel"])
            P.dve(I("memset", sel[64:65, :], 1.0), r=["sel"], w=["sel"])
            KTh = AR.alloc([128, L], BF16); QTh = AR.alloc([128, LO], BF16); Vh = AR.alloc([128, 64, 130], BF16)
            Pt = [AR.alloc([128, 2, 512], BF16) for _ in range(2)]
            Osb = AR.alloc([65, 2, 512]); Od = AR.alloc([65, 2, 512]); rL = AR.alloc([64, 2, 512])
            a1 = AR.alloc([64, 512]); a2 = AR.alloc([64, 512]); dif = AR.alloc([64, 512]); sq2 = AR.alloc([64, 512]); rs2 = AR.alloc([64, 512])
            oT = AR.alloc([64, 512], BF16)
            scb = [bank(0, 2).rearrange("p (a b) -> p a b", a=2), bank(2, 2).rearrange("p (a b) -> p a b", a=2)]
            Ooff = bank(4, 2).rearrange("p (a b) -> p a b", a=2); Odg = bank(6, 2).rearrange("p (a b) -> p a b", a=2)
            it = 0
            for hp in range(4):
                P.dma(I("dma_start", out=KTh, in_=KT[hp]), r=["KT"], w=["KTh"])
                P.dma(I("dma_start", out=QTh, in_=QT[hp]), r=["QT"], w=["QTh"])
                P.dma(I("dma_start", out=Vh, in_=VV[:, :, hp * 130:(hp + 1) * 130].rearrange("b p c -> p b c")), r=["VV"], w=["Vh"])
                for j in range(8):
                    for hl in range(2):
                        h = 2 * hp + hl; sl = slopes[h]
                        q0 = LO + 512 * j; kd0 = q0 // 128; klo = _qkey_lo(q0, sl)
                        offs = list(range(klo, kd0))
                        for ii, kb in enumerate(offs):
                            sc = scb[it % 2]; sk = "sc%d" % (it % 2); pt = Pt[it % 2]; pk = "Pt%d" % (it % 2); it += 1
                            for m_ in range(2):
                                pr = 32 * (2 * hl + m_)
                                P.pe(I("matmul", sc[:, m_, :], lhsT=KTh[pr:pr + 32, kb * 128:(kb + 1) * 128], rhs=QTh[pr:pr + 32, j * 512:(j + 1) * 512], start=True, stop=True, tile_position=(pr, 0)), r=["KTh", "QTh"], w=[sk])
                            btab = btp if kb < 32 else bt
                            P.act(I("activation", out=pt, in_=sc, func=AF.Exp, bias=btab[:, h, kd0 - kb:kd0 - kb + 1], scale=SC), r=[sk, "btp", "bt0", "bt1"], w=[pk])
                            for m_ in range(2):
                                P.pe(I("matmul", Ooff[0:65, m_, :], lhsT=Vh[:, kb, hl * 65:(hl + 1) * 65], rhs=pt[:, m_, :], start=(ii == 0), stop=(ii == len(offs) - 1)), r=["Vh", pk], w=["Ooff"])
                        for r_ in range(4):
                            kb = kd0 + r_
                            sc = scb[it % 2]; sk = "sc%d" % (it % 2); pt = Pt[it % 2]; pk = "Pt%d" % (it % 2); it += 1
                            c0 = 128 * r_
                            for m_ in range(2):
                                pr = 32 * (2 * hl + m_)
                                P.pe(I("matmul", sc[:, m_, c0:512], lhsT=KTh[pr:pr + 32, kb * 128:(kb + 1) * 128], rhs=QTh[pr:pr + 32, j * 512 + c0:(j + 1) * 512], start=True, stop=True, tile_position=(pr, 0)), r=["KTh", "QTh"], w=[sk])
                            for qs in range(r_, 4):
                                P.act(I("activation", out=pt[:, :, 128 * qs:128 * qs + 128], in_=sc[:, :, 128 * qs:128 * qs + 128], func=AF.Exp, bias=bt[:, h, qs - r_:qs - r_ + 1], scale=SC), r=[sk, "bt0", "bt1"], w=[pk])
                            P.pool(I("tensor_tensor", out=pt[:, :, c0:c0 + 128], in0=pt[:, :, c0:c0 + 128], in1=trib.unsqueeze(1).to_broadcast([128, 2, 128]), op=ALU.mult), r=[pk, "trib"], w=[pk])
                            for m_ in range(2):
                                P.pe(I("matmul", Odg[0:65, m_, c0:512], lhsT=Vh[:, kb, hl * 65:(hl + 1) * 65], rhs=pt[:, m_, c0:512], start=(r_ == 0), stop=(r_ == 3)), r=["Vh", pk], w=["Odg"])
                        P.act(I("activation", out=Od, in_=Odg[0:65], func=AF.Copy), r=["Odg"], w=["Od"])
                        for qs in range(4):
                            f = math.exp(-sl * 128.0 * qs)
                            cs = slice(128 * qs, 128 * qs + 128)
                            P.dve(I("scalar_tensor_tensor", out=Osb[:, :, cs], in0=Ooff[0:65, :, cs], scalar=f, in1=Od[:, :, cs], op0=ALU.mult, op1=ALU.add), r=["Ooff", "Od"], w=["Osb"])
                        for m_ in range(2):
                            P.pe(I("matmul", Odg[0:64, m_, :], lhsT=sel, rhs=Osb[:, m_, :], start=True, stop=True), r=["sel", "Osb", "Od"], w=["Odg"])
                        P.dve(I("reciprocal", out=rL, in_=Odg[0:64]), r=["Odg"], w=["rL"])
                        P.pool(I("tensor_tensor", out=a1, in0=Osb[0:64, 0, :], in1=rL[:, 0, :], op=ALU.mult), r=["Osb", "rL"], w=["a1"])
                        P.pool(I("tensor_tensor", out=a2, in0=Osb[0:64, 1, :], in1=rL[:, 1, :], op=ALU.mult), r=["Osb", "rL"], w=["a2"])
                        P.dve(I("scalar_tensor_tensor", out=dif, in0=a2, scalar=neglam[:, 0:1], in1=a1, op0=ALU.mult, op1=ALU.add), r=["a1", "a2", "neglam"], w=["dif"])
                        P.pool(I("tensor_tensor", out=sq2, in0=dif, in1=dif, op=ALU.mult), r=["dif"], w=["sq2"])
                        P.pe(I("matmul", Odg[0:64, 0, :], lhsT=ones[0:64, 0:64], rhs=sq2, start=True, stop=True), r=["ones", "sq2", "rL"], w=["Odg"])
                        P.act(I("activation", out=rs2, in_=Odg[0:64, 0, :], func=AF.Ln, scale=1.0 / 64.0, bias=1e-5), r=["Odg"], w=["rs2"])
                        P.act(I("activation", out=rs2, in_=rs2, func=AF.Exp, scale=-0.5), r=["rs2"], w=["rs2"])
                        P.dve(I("scalar_tensor_tensor", out=oT, in0=dif, scalar=gcol[:, 0:1], in1=rs2, op0=ALU.mult, op1=ALU.mult), r=["dif", "gcol", "rs2"], w=["oT"])
                        P.dma(I("dma_start", out=MIXT[h // 2, (h % 2) * 64:(h % 2) * 64 + 64, j * 512:(j + 1) * 512], in_=oT), r=["oT"], w=["MIXT"])
            P.barrier()
            AR.reset(gmark)

        def bcast_row(dst, src, n, key):
            P.dma(I("dma_start", out=dst, in_=src.to_broadcast([128, n])), w=[key])

        if "3" in phases:
            if os.environ.get('KZERO'):
                zt = AR.alloc([128, 8, 512], BF16)
                P.dve(I("memset", zt, 0.0), w=["zt"])
                for cc in range(8):
                    P.dma(I("dma_start", out=MIXT[:, :, cc * 512:(cc + 1) * 512].rearrange("k p t -> p k t"), in_=zt), r=["zt"], w=["MIXT"])
            wout = AR.alloc([128, 8, 1024], BF16)
            for kt in range(8):
                P.dma(I("dma_start", out=wout[:, kt, :], in_=w_out[kt * 128:(kt + 1) * 128, :]), w=["wout"], q="gpsimd")
            wr = AR.alloc([128, 8, 32])
            P.dma(I("dma_start", out=wr, in_=w_router.rearrange("(k p) e -> p k e", p=128)), w=["wr"])
            g1b = AR.alloc([128, 1024]); b1b = AR.alloc([128, 1024]); brb = AR.alloc([128, 32])
            bcast_row(g1b, ln1_g, 1024, "lng"); bcast_row(b1b, ln1_b, 1024, "lng"); bcast_row(brb, b_router, 32, "brb")
            AR_t["st"] = AR.alloc([128, 12]); AR_t["mv"] = AR.alloc([128, 2]); AR_t["rs"] = AR.alloc([128, 1])
            mixT = [AR.alloc([128, 8, 128], BF16) for _ in range(2)]
            xt_ = [AR.alloc([128, 1024]) for _ in range(2)]
            pre = AR.alloc([128, 1024]); x1 = [AR.alloc([128, 1024]) for _ in range(2)]
            x1Tf = AR.alloc([128, 8, 128]); x1Tb = [AR.alloc([128, 8, 128], BF16) for _ in range(2)]
            lg = AR.alloc([128, 32]); mx8 = AR.alloc([128, 8]); msk = AR.alloc([128, 32]); ex = AR.alloc([128, 32]); nmx = AR.alloc([128, 1]); ssum = AR.alloc([128, 1])
            CUT = int(os.environ.get('KCUT', '99'))
            for t in range(int(os.environ.get('KNT3', '32'))):
                b_ = t % 2
                P.dma(I("dma_start", out=mixT[b_], in_=MIXT[:, :, t * 128:(t + 1) * 128].rearrange("k p t -> p k t")), r=["MIXT"], w=["mixT%d" % b_])
                P.dma(I("dma_start", out=xt_[b_], in_=xc[LO + t * 128:LO + (t + 1) * 128, :]), w=["xt%d" % b_])
                if CUT < 2: continue
                pm = bank(0, 2)
                for dh in range(2):
                    for kt in range(8):
                        P.pe(I("matmul", pm[:, dh * 512:(dh + 1) * 512], lhsT=mixT[b_][:, kt, :], rhs=wout[:, kt, dh * 512:(dh + 1) * 512], start=(kt == 0), stop=(kt == 7)), r=["mixT%d" % b_, "wout"], w=["pm"])
                P.dve(I("scalar_tensor_tensor", out=pre, in0=xt_[b_], scalar=ALPHA, in1=pm, op0=ALU.mult, op1=ALU.add), r=["xt%d" % b_, "pm"], w=["pre"])
                if CUT < 3: continue
                layer_norm_tile(pre, g1b, b1b, x1[b_], "pre")
                if CUT < 4: continue
                P.dma(I("dma_start", out=X1[t * 128:(t + 1) * 128, :], in_=x1[b_]), r=["preo"], w=["X1"])
                if CUT < 5: continue
                ptr = bank(2, 2).rearrange("p (a b) -> p a b", a=8)
                for kt in range(8):
                    P.pe(I("transpose", out=ptr[:, kt, :], in_=x1[b_][:, kt * 128:(kt + 1) * 128], identity=ident), r=["preo", "ident"], w=["ptr"])
                KS = os.environ.get('KSUB', 'abc')
                if 'a' in KS:
                    P.act(I("activation", out=x1Tf, in_=ptr, func=AF.Copy), r=["ptr"], w=["x1Tf"])
                if 'b' in KS:
                    P.act(I("activation", out=x1Tb[b_], in_=ptr, func=AF.Copy), r=["ptr"], w=["x1Tb%d" % b_])
                if 'c' in KS:
                    P.dma(I("dma_start", out=X1T[:, :, t * 128:(t + 1) * 128].rearrange("k p t -> p k t"), in_=x1Tb[b_]), r=["x1Tb%d" % b_], w=["X1T"])
                if CUT < 6: continue
                pl = bank(4)[:, 0:32]
                for kt in range(8):
                    P.pe(I("matmul", pl, lhsT=x1Tf[:, kt, :], rhs=wr[:, kt, :], start=(kt == 0), stop=(kt == 7)), r=["x1Tf", "wr"], w=["pl"])
                if CUT < 7: continue
                P.dve(I("tensor_tensor", out=lg, in0=pl, in1=brb, op=ALU.add), r=["pl", "brb"], w=["lg"])
                P.dve(I("max", out=mx8, in_=lg), r=["lg"], w=["mx8"])
                P.dve(I("tensor_scalar", out=msk, in0=lg, scalar1=mx8[:, 3:4], scalar2=1e-7, op0=ALU.subtract, op1=ALU.add), r=["lg", "mx8"], w=["msk"])
                P.dve(I("tensor_scalar", out=msk, in0=msk, scalar1=1e10, scalar2=0.0, op0=ALU.mult, op1=ALU.max), r=["msk"], w=["msk"])
                P.dve(I("tensor_scalar", out=msk, in0=msk, scalar1=1.0, scalar2=None, op0=ALU.min), r=["msk"], w=["msk"])
                P.dve(I("tensor_scalar", out=nmx, in0=mx8[:, 0:1], scalar1=-1.0, scalar2=None, op0=ALU.mult), r=["mx8"], w=["nmx"])
                P.act(I("activation", out=ex, in_=lg, func=AF.Exp, bias=nmx[:, 0:1]), r=["lg", "nmx"], w=["ex"])
                P.dve(I("tensor_tensor", out=ex, in0=ex, in1=msk, op=ALU.mult), r=["ex", "msk"], w=["ex"])
                P.dve(I("reduce_sum", out=ssum, in_=ex, axis=AX.X), r=["ex"], w=["ssum"])
                P.dve(I("reciprocal", out=ssum, in_=ssum), r=["ssum"], w=["ssum"])
                P.dve(I("tensor_scalar", out=G[:, t, :], in0=ex, scalar1=ssum[:, 0:1], scalar2=None, op0=ALU.mult), r=["ex", "ssum"], w=["G"])
            P.barrier()
            AR.reset(gmark)

        if "4" in phases:
            Wgu = [AR.alloc([128, 8, 2, 1024], BF16) for _ in range(2)]
            Wd = [AR.alloc([128, 8, 1024], BF16) for _ in range(2)]
            stage = [AR.alloc([128, 2048]) for _ in range(3)]
            X1Tc = AR.alloc([128, 8, 1024], BF16); acc = AR.alloc([128, 8, 1024]); actT = AR.alloc([128, 8, 512], BF16)
            tgs = [AR.alloc([128, 512]) for _ in range(2)]; tsgs = [AR.alloc([128, 512]) for _ in range(2)]; tls = [AR.alloc([128, 512]) for _ in range(2)]
            BGU = AR.alloc([128, 8, 2, 32]); bd = AR.alloc([32, 1024]); GTc = AR.alloc([32, 8, 128])
            bgn = stage[0][0:32, :]
            P.dma(I("dma_start", out=bgn, in_=b_gu), w=["stage0"])
            bgv = bgn.rearrange("e (ft p two) -> e ft two p", p=128, two=2)
            pbg = bank(7).rearrange("p (a b c) -> p a b c", a=8, b=2)
            for ft in range(8):
                for two in range(2):
                    P.pe(I("transpose", out=pbg[:, ft, two, :], in_=bgv[:, ft, two, :], identity=ident[0:32, 0:32]), r=["stage0", "ident"], w=["B7"])
            P.dve(I("tensor_copy", out=BGU, in_=pbg), r=["B7"], w=["BGU"])
            P.dma(I("dma_start", out=bd, in_=b_dn), w=["bd"])
            stn = [0]

            def load_steps(e):
                b_ = e % 2
                steps = []
                for kt in range(8):
                    def st_(kt=kt):
                        sb_ = stn[0] % 3; stn[0] += 1
                        P.dma(I("dma_start", out=stage[sb_], in_=w_gu[e, kt * 128:(kt + 1) * 128, :]), w=["stage%d" % sb_])
                        src = stage[sb_].rearrange("p (f two) -> p two f", two=2)
                        if kt % 2 == 0:
                            P.act(I("activation", out=Wgu[b_][:, kt, :, :], in_=src, func=AF.Copy), r=["stage%d" % sb_], w=["Wgu%d" % b_])
                        else:
                            P.pool(I("tensor_copy", out=Wgu[b_][:, kt, :, :], in_=src), r=["stage%d" % sb_], w=["Wgu%d" % b_])
                    steps.append(st_)
                for k2 in range(4):
                    def st2_(k2=k2):
                        sb_ = stn[0] % 3; stn[0] += 1
                        P.dma(I("dma_start", out=stage[sb_].rearrange("p (k f) -> p k f", k=2), in_=w_dn[e, k2 * 256:(k2 + 1) * 256, :].rearrange("(k p) f -> p k f", p=128)), w=["stage%d" % sb_])
                        src = stage[sb_].rearrange("p (k f) -> p k f", k=2)
                        if k2 % 2 == 0:
                            P.act(I("activation", out=Wd[b_][:, 2 * k2:2 * k2 + 2, :], in_=src, func=AF.Copy), r=["stage%d" % sb_], w=["Wd%d" % b_])
                        else:
                            P.pool(I("tensor_copy", out=Wd[b_][:, 2 * k2:2 * k2 + 2, :], in_=src), r=["stage%d" % sb_], w=["Wd%d" % b_])
                    steps.append(st2_)
                return steps

            def load_expert(e):
                for f_ in load_steps(e):
                    f_()

            for c in range(NCH):
                P.dma(I("dma_start", out=X1Tc, in_=X1T[:, :, c * 1024:(c + 1) * 1024].rearrange("k p t -> p k t")), r=["X1T"], w=["X1Tc"])
                pg = bank(6)[0:32, :].rearrange("p (a b) -> p a b", a=4)
                for half in range(2):
                    for i_ in range(4):
                        tl_ = half * 4 + i_
                        P.pe(I("transpose", out=pg[:, i_, :], in_=G[:, c * 8 + tl_, :], identity=ident), r=["G", "ident"], w=["pg"])
                    P.dve(I("tensor_copy", out=GTc[:, half * 4:half * 4 + 4, :], in_=pg), r=["pg"], w=["GTc"])
                for tl_ in range(8):
                    pa = bank(0, 2)
                    for dh in range(2):
                        P.pe(I("matmul", pa[:, dh * 512:(dh + 1) * 512], lhsT=GTc[:, tl_, :], rhs=bd[:, dh * 512:(dh + 1) * 512], start=True, stop=True), r=["GTc", "bd"], w=["pa"])
                    P.dve(I("tensor_copy", out=acc[:, tl_, :], in_=pa), r=["pa"], w=["acc"])
                load_expert(0)
                for e in range(NE):
                    pend = load_steps(e + 1) if e + 1 < NE else []
                    b_ = e % 2
                    for tc in range(2):
                        for ft in range(8):
                            pgl = bank((ft % 2) * 2); pln = bank((ft % 2) * 2 + 1); gk = "pgl%d" % (ft % 2)
                            tg = tgs[ft % 2]; tsg = tsgs[ft % 2]; tl = tls[ft % 2]; kg = "tg%d" % (ft % 2); ksg = "tsg%d" % (ft % 2); kl_ = "tl%d" % (ft % 2)
                            for kt in range(8):
                                P.pe(I("matmul", pgl, lhsT=Wgu[b_][:, kt, 0, ft * 128:(ft + 1) * 128], rhs=X1Tc[:, kt, tc * 512:(tc + 1) * 512], start=(kt == 0), stop=(kt == 7)), r=["Wgu%d" % b_, "X1Tc"], w=[gk])
                            for kt in range(8):
                                P.pe(I("matmul", pln, lhsT=Wgu[b_][:, kt, 1, ft * 128:(ft + 1) * 128], rhs=X1Tc[:, kt, tc * 512:(tc + 1) * 512], start=(kt == 0), stop=(kt == 7)), r=["Wgu%d" % b_, "X1Tc"], w=[gk + "l"])
                            P.dve(I("tensor_scalar", out=tg, in0=pgl, scalar1=BGU[:, ft, 0, e:e + 1], scalar2=7.0, op0=ALU.add, op1=ALU.min), r=[gk, "BGU"], w=[kg])
                            P.act(I("activation", out=tsg, in_=tg, func=AF.Sigmoid, scale=1.702), r=[kg], w=[ksg])
                            P.dve(I("tensor_scalar", out=tl, in0=pln, scalar1=BGU[:, ft, 1, e:e + 1], scalar2=7.0, op0=ALU.add, op1=ALU.min), r=[gk + "l", "BGU"], w=[kl_])
                            P.pool(I("tensor_scalar", out=tl, in0=tl, scalar1=-7.0, scalar2=1.0, op0=ALU.max, op1=ALU.add), r=[kl_], w=[kl_])
                            P.pool(I("tensor_tensor", out=tg, in0=tg, in1=tsg, op=ALU.mult), r=[kg, ksg], w=[kg])
                            P.pool(I("tensor_tensor", out=actT[:, ft, :], in0=tg, in1=tl, op=ALU.mult), r=[kg, kl_], w=["actT"])
                            if pend:
                                pend.pop(0)()
                        for ti in range(4):
                            tl_ = tc * 4 + ti
                            for dh in range(2):
                                pdn = bank(4 + (ti * 2 + dh) % 4); dk = "pdn%d" % ((ti * 2 + dh) % 4)
                                for ft in range(8):
                                    P.pe(I("matmul", pdn, lhsT=actT[:, ft, ti * 128:(ti + 1) * 128], rhs=Wd[b_][:, ft, dh * 512:(dh + 1) * 512], start=(ft == 0), stop=(ft == 7)), r=["actT", "Wd%d" % b_], w=[dk])
                                P.dve(I("scalar_tensor_tensor", out=acc[:, tl_, dh * 512:(dh + 1) * 512], in0=pdn, scalar=G[:, c * 8 + tl_, e:e + 1], in1=acc[:, tl_, dh * 512:(dh + 1) * 512], op0=ALU.mult, op1=ALU.add), r=[dk, "G", "acc"], w=["acc"])
                    while pend:
                        pend.pop(0)()
                for tl_ in range(8):
                    t = c * 8 + tl_
                    sb_ = stn[0] % 3; stn[0] += 1
                    xs = stage[sb_][:, 0:1024]
                    P.dma(I("dma_start", out=xs, in_=X1[t * 128:(t + 1) * 128, :]), r=["X1"], w=["stage%d" % sb_])
                    P.dve(I("scalar_tensor_tensor", out=xs, in0=xs, scalar=ALPHA, in1=acc[:, tl_, :], op0=ALU.mult, op1=ALU.add), r=["stage%d" % sb_, "acc"], w=["stage%d" % sb_])
                    P.dma(I("dma_start", out=RR[t * 128:(t + 1) * 128, :], in_=xs), r=["stage%d" % sb_], w=["RR"])
            P.barrier()
            AR.reset(gmark)

        if "5" in phases:
            wpg = AR.alloc([128, 8, 1024], BF16); wpp = AR.alloc([128, 2, 1024], BF16)
            for kt in range(8):
                P.dma(I("dma_start", out=wpg[:, kt, :], in_=w_pg[kt * 128:(kt + 1) * 128, :]), w=["wpg"], q="gpsimd")
            for kt in range(2):
                P.dma(I("dma_start", out=wpp[:, kt, :], in_=w_pp[kt * 128:(kt + 1) * 128, :]), w=["wpp"], q="gpsimd")
            g2b = AR.alloc([128, 1024]); b2b = AR.alloc([128, 1024])
            bcast_row(g2b, ln2_g, 1024, "lng"); bcast_row(b2b, ln2_b, 1024, "lng")
            AR_t["st"] = AR.alloc([128, 12]); AR_t["mv"] = AR.alloc([128, 2]); AR_t["rs"] = AR.alloc([128, 1])
            rt = [AR.alloc([128, 1024]) for _ in range(2)]; ptl = [AR.alloc([128, 256]) for _ in range(2)]
            rT = AR.alloc([128, 8, 128], BF16); pT = AR.alloc([128, 2, 128], BF16)
            sgt = AR.alloc([128, 1024]); yv = AR.alloc([128, 1024]); yo = [AR.alloc([128, 1024]) for _ in range(2)]
            for t in range(32):
                b_ = t % 2
                P.dma(I("dma_start", out=rt[b_], in_=RR[t * 128:(t + 1) * 128, :]), r=["RR"], w=["rt%d" % b_])
                P.dma(I("dma_start", out=ptl[b_], in_=pc[t * 128:(t + 1) * 128, :]), w=["ptl%d" % b_])
                ptr = bank(4, 2).rearrange("p (a b) -> p a b", a=8)
                for kt in range(8):
                    P.pe(I("transpose", out=ptr[:, kt, :], in_=rt[b_][:, kt * 128:(kt + 1) * 128], identity=ident), r=["rt%d" % b_, "ident"], w=["ptr5"])
                P.act(I("activation", out=rT, in_=ptr, func=AF.Copy), r=["ptr5"], w=["rT"])
                ptp = bank(6).rearrange("p (a b) -> p a b", a=4)
                for kt in range(2):
                    P.pe(I("transpose", out=ptp[:, kt, :], in_=ptl[b_][:, kt * 128:(kt + 1) * 128], identity=ident), r=["ptl%d" % b_, "ident"], w=["ptp"])
                P.dve(I("tensor_copy", out=pT, in_=ptp[:, 0:2, :]), r=["ptp"], w=["pT"])
                pgt = bank(0, 2); ppp = bank(2, 2)
                for dh in range(2):
                    for kt in range(8):
                        P.pe(I("matmul", pgt[:, dh * 512:(dh + 1) * 512], lhsT=rT[:, kt, :], rhs=wpg[:, kt, dh * 512:(dh + 1) * 512], start=(kt == 0), stop=(kt == 7)), r=["rT", "wpg"], w=["pgt"])
                    for kt in range(2):
                        P.pe(I("matmul", ppp[:, dh * 512:(dh + 1) * 512], lhsT=pT[:, kt, :], rhs=wpp[:, kt, dh * 512:(dh + 1) * 512], start=(kt == 0), stop=(kt == 1)), r=["pT", "wpp"], w=["ppp"])
                P.act(I("activation", out=sgt, in_=pgt, func=AF.Sigmoid), r=["pgt"], w=["sgt"])
                P.dve(I("tensor_tensor", out=sgt, in0=sgt, in1=ppp, op=ALU.mult), r=["sgt", "ppp"], w=["sgt"])
                P.pool(I("tensor_tensor", out=yv, in0=sgt, in1=rt[b_], op=ALU.add), r=["sgt", "rt%d" % b_], w=["yv"])
                layer_norm_tile(yv, g2b, b2b, yo[b_], "yv")
                P.dma(I("dma_start", out=out[t * 128:(t + 1) * 128, :], in_=yo[b_]), r=["yvo"], w=["out"])
            P.barrier()

        P.emit(st)
    return nc


_NC_CACHE = {}


def _prep_inputs(inputs):
    sq = lambda k: np.ascontiguousarray(np.asarray(inputs[k])[0])
    x = np.asarray(inputs["x"]); p = np.asarray(inputs["p"])[0]
    shared = {
        "w_in": sq("w_in"), "lambda_q1": np.asarray(inputs["lambda_q1"]), "lambda_k1": np.asarray(inputs["lambda_k1"]),
        "lambda_q2": np.asarray(inputs["lambda_q2"]), "lambda_k2": np.asarray(inputs["lambda_k2"]),
        "subln_g": np.ascontiguousarray(np.asarray(inputs["subln_g"]).reshape(64, 1)),
        "ssm_a_re": sq("ssm_a_re"), "ssm_a_im": sq("ssm_a_im"), "ssm_log_dt": np.asarray(inputs["ssm_log_dt"]),
        "ssm_b_re": sq("ssm_b_re"), "ssm_b_im": sq("ssm_b_im"), "ssm_c_re": sq("ssm_c_re"), "ssm_c_im": sq("ssm_c_im"),
        "ssm_d": sq("ssm_d"), "w_glu": sq("w_glu"), "ssm_norm_g": sq("ssm_norm_g"), "w_out": sq("w_out"),
        "ln1_g": np.asarray(inputs["ln1_g"]), "ln1_b": np.asarray(inputs["ln1_b"]),
        "w_router": sq("w_router"), "b_router": np.asarray(inputs["b_router"]),
        "w_gate_up": sq("w_gate_up")[:NE], "b_gate_up": sq("b_gate_up"), "w_down": sq("w_down")[:NE], "b_down": sq("b_down"),
        "w_ple_gate": sq("w_ple_gate"), "w_ple_proj": sq("w_ple_proj"),
        "ln2_g": np.asarray(inputs["ln2_g"]), "ln2_b": np.asarray(inputs["ln2_b"]),
    }
    shared = {k: np.ascontiguousarray(v, dtype=np.float32) for k, v in shared.items()}
    maps = []
    for c in range(8):
        b, h = c // 2, c % 2
        if h == 0:
            xcore = np.concatenate([np.zeros((LO, DM), np.float32), x[b, :LO]], axis=0)
        else:
            xcore = np.ascontiguousarray(x[b])
        m = dict(shared)
        m["xc"] = np.ascontiguousarray(xcore, dtype=np.float32)
        m["pc"] = np.ascontiguousarray(p[b, h * LO:(h + 1) * LO], dtype=np.float32)
        m["pref"] = np.full((128, 1), NEG if h == 0 else 0.0, np.float32)
        maps.append(m)
    return maps


def kernel(**inputs):
    dbg = KDEBUG
    if dbg not in _NC_CACHE:
        _NC_CACHE[dbg] = build_program(dbg)
    nc = _NC_CACHE[dbg]
    maps = _prep_inputs(inputs)
    res = run_bass_kernel_spmd(nc, maps, core_ids=list(range(8)))
    if dbg:
        return res.results
    outp = np.zeros((4, L, DM), np.float32)
    for c in range(8):
        b, h = c // 2, c % 2
        outp[b, h * LO:(h + 1) * LO] = res.results[c]["out"]
    return outp
```
